# Optimizing a Trainium2 kernel written in Bass

```python
import jax
import jax.numpy as jnp
from jax import lax
import numpy as np

D_MODEL = 2048
BATCH = 2
SEQ = 8192
DEPTH = 1

D_MIX = D_MODEL
CONV_W = D_MIX // 2
CONV_TAPS = 3
HEAD_DIM = 128
ATTN_W = D_MIX - CONV_W
N_HEADS = ATTN_W // HEAD_DIM
N_KV_HEADS = 2
GROUP = N_HEADS // N_KV_HEADS
KV_W = N_KV_HEADS * HEAD_DIM
CMP_BLOCK = 32
CMP_STRIDE = 16
SEL_BLOCK = 64
N_SELECT = 16
WINDOW = 512
Q_BLOCK = 128
N_BRANCH = 3
ROPE_THETA = 10000.0
RMS_EPS = 1e-6
FFN_HIDDEN = -(-(8 * D_MODEL) // (3 * 256)) * 256
PROJ_SIZES = (CONV_W, CONV_W, CONV_W, ATTN_W) + (KV_W,) * 6 + (N_HEADS * N_BRANCH,)
D_PROJ = sum(PROJ_SIZES)

kernel_name = "hybrid_shortconv_nsa_swiglu"


def rms_norm(x, g):
    xf = x.astype(jnp.float32)
    y = xf * lax.rsqrt(jnp.mean(xf * xf, axis=-1, keepdims=True) + RMS_EPS)
    return (y * g.astype(jnp.float32)).astype(x.dtype)


def rope(x, positions):
    d = x.shape[-1]
    half = d // 2
    inv = 1.0 / (ROPE_THETA ** (jnp.arange(half, dtype=jnp.float32) / half))
    ang = positions.astype(jnp.float32)[:, None] * inv[None, :]
    cos = jnp.cos(ang)[None, :, None, :]
    sin = jnp.sin(ang)[None, :, None, :]
    xf = x.astype(jnp.float32)
    x1, x2 = xf[..., :half], xf[..., half:]
    return jnp.concatenate([x1 * cos - x2 * sin, x2 * cos + x1 * sin], axis=-1).astype(x.dtype)


def masked_softmax(s, mask):
    s = jnp.where(mask, s.astype(jnp.float32), -jnp.inf)
    m = jnp.max(s, axis=-1, keepdims=True)
    m = jnp.where(jnp.isfinite(m), m, 0.0)
    e = jnp.exp(s - m)
    return e / jnp.maximum(jnp.sum(e, axis=-1, keepdims=True), 1e-30)


def causal_depthwise_conv(u, w):
    S = u.shape[1]
    up = jnp.pad(u, ((0, 0), (CONV_TAPS - 1, 0), (0, 0)))
    y = up[:, 0:S] * w[0]
    for k in range(1, CONV_TAPS):
        y = y + up[:, k:k + S] * w[k]
    return y


def compress(kv, pe, w1, w2):
    B, S, Hk, d = kv.shape
    n_cmp = (S - CMP_BLOCK) // CMP_STRIDE + 1
    idx = jnp.arange(n_cmp)[:, None] * CMP_STRIDE + jnp.arange(CMP_BLOCK)[None, :]
    blocks = kv[:, idx] + pe[:, None, :]
    flat = blocks.transpose(0, 3, 1, 2, 4).reshape(B, Hk, n_cmp, CMP_BLOCK * d)
    return jax.nn.silu(flat @ w1) @ w2


def nsa_attention(q, kc, vc, ks, vs, kw, vw):
    B, Hk, G, S, d = q.shape
    scale = d ** -0.5
    n_cmp = kc.shape[2]
    n_sel = S // SEL_BLOCK
    k_top = min(N_SELECT, n_sel)
    cmp_start = jnp.arange(n_cmp) * CMP_STRIDE
    cmp_end = cmp_start + CMP_BLOCK - 1
    sel_ids = jnp.arange(n_sel)
    sel_start = sel_ids * SEL_BLOCK
    overlap = ((cmp_start[:, None] < sel_start[None, :] + SEL_BLOCK)
               & (cmp_start[:, None] + CMP_BLOCK > sel_start[None, :])).astype(jnp.float32)
    ks_blk = ks.reshape(B, Hk, n_sel, SEL_BLOCK, d)
    vs_blk = vs.reshape(B, Hk, n_sel, SEL_BLOCK, d)
    kw_pad = jnp.pad(kw, ((0, 0), (0, 0), (WINDOW, 0), (0, 0)))
    vw_pad = jnp.pad(vw, ((0, 0), (0, 0), (WINDOW, 0), (0, 0)))
    b_ix = jnp.arange(B)[:, None, None, None]
    h_ix = jnp.arange(Hk)[None, :, None, None]

    def block_fn(q0):
        t = q0 + jnp.arange(Q_BLOCK)
        qb = lax.dynamic_slice_in_dim(q, q0, Q_BLOCK, axis=3)
        s_c = jnp.einsum('bhgqd,bhnd->bhgqn', qb, kc) * scale
        p_c = masked_softmax(s_c, cmp_end[None, :] <= t[:, None])
        o_c = jnp.einsum('bhgqn,bhnd->bhgqd', p_c.astype(vc.dtype), vc)
        imp = jnp.einsum('bhgqn,ns->bhqs', p_c, overlap)
        cur = t // SEL_BLOCK
        future = sel_ids[None, :] > cur[:, None]
        forced = ((sel_ids[None, :] == 0) | (sel_ids[None, :] == cur[:, None])
                  | (sel_ids[None, :] == cur[:, None] - 1))
        imp = jnp.where(future, -jnp.inf, jnp.where(forced, jnp.inf, imp))
        _, top_idx = lax.top_k(imp, k_top)
        top_ok = top_idx <= cur[None, None, :, None]
        kg = ks_blk[b_ix, h_ix, top_idx]
        vg = vs_blk[b_ix, h_ix, top_idx]
        s_s = jnp.einsum('bhgqd,bhqkld->bhgqkl', qb, kg) * scale
        pos_s = top_idx[..., None] * SEL_BLOCK + jnp.arange(SEL_BLOCK)
        m_s = (pos_s <= t[None, None, :, None, None]) & top_ok[..., None]
        p_s = masked_softmax(s_s.reshape(B, Hk, G, Q_BLOCK, k_top * SEL_BLOCK),
                             m_s.reshape(B, Hk, 1, Q_BLOCK, k_top * SEL_BLOCK)).reshape(s_s.shape)
        o_s = jnp.einsum('bhgqkl,bhqkld->bhgqd', p_s.astype(vg.dtype), vg)
        kwb = lax.dynamic_slice_in_dim(kw_pad, q0, WINDOW + Q_BLOCK, axis=2)
        vwb = lax.dynamic_slice_in_dim(vw_pad, q0, WINDOW + Q_BLOCK, axis=2)
        pos_w = q0 - WINDOW + jnp.arange(WINDOW + Q_BLOCK)
        m_w = ((pos_w[None, :] <= t[:, None]) & (pos_w[None, :] > t[:, None] - WINDOW)
               & (pos_w[None, :] >= 0))
        s_w = jnp.einsum('bhgqd,bhkd->bhgqk', qb, kwb) * scale
        o_w = jnp.einsum('bhgqk,bhkd->bhgqd', masked_softmax(s_w, m_w).astype(vwb.dtype), vwb)
        return o_c, o_s, o_w

    starts = jnp.arange(S // Q_BLOCK) * Q_BLOCK
    o_c, o_s, o_w = lax.map(block_fn, starts)

    def to_bshd(o):
        return o.transpose(1, 0, 4, 2, 3, 5).reshape(B, S, Hk * G, d)

    return to_bshd(o_c), to_bshd(o_s), to_bshd(o_w)


def setup_inputs(seed: int = 0) -> dict:
    key = jax.random.key(seed)
    ks = jax.random.split(key, 20)

    def nrm(k, shape, scale):
        return jax.random.normal(k, shape, jnp.float32) * scale

    def gain(k, shape):
        return 1.0 + nrm(k, shape, 0.02)

    return {
        "x": nrm(ks[0], (BATCH, SEQ, D_MODEL), 1.0),
        "norm_mix": gain(ks[1], (DEPTH, D_MODEL)),
        "w_in": nrm(ks[2], (DEPTH, D_MODEL, D_PROJ), D_MODEL ** -0.5),
        "conv_w": nrm(ks[3], (DEPTH, CONV_TAPS, CONV_W), CONV_TAPS ** -0.5),
        "cmp_pe_k": nrm(ks[4], (DEPTH, CMP_BLOCK, HEAD_DIM), 0.02),
        "cmp_w1_k": nrm(ks[5], (DEPTH, CMP_BLOCK * HEAD_DIM, HEAD_DIM), (CMP_BLOCK * HEAD_DIM) ** -0.5),
        "cmp_w2_k": nrm(ks[6], (DEPTH, HEAD_DIM, HEAD_DIM), HEAD_DIM ** -0.5),
        "cmp_pe_v": nrm(ks[7], (DEPTH, CMP_BLOCK, HEAD_DIM), 0.02),
        "cmp_w1_v": nrm(ks[8], (DEPTH, CMP_BLOCK * HEAD_DIM, HEAD_DIM), (CMP_BLOCK * HEAD_DIM) ** -0.5),
        "cmp_w2_v": nrm(ks[9], (DEPTH, HEAD_DIM, HEAD_DIM), HEAD_DIM ** -0.5),
        "norm_conv_out": gain(ks[10], (DEPTH, CONV_W)),
        "norm_attn_out": gain(ks[11], (DEPTH, ATTN_W)),
        "w_out": nrm(ks[12], (DEPTH, D_MIX, D_MODEL), D_MIX ** -0.5),
        "norm_ffn": gain(ks[13], (DEPTH, D_MODEL)),
        "w_gate": nrm(ks[14], (DEPTH, D_MODEL, FFN_HIDDEN), D_MODEL ** -0.5),
        "w_up": nrm(ks[15], (DEPTH, D_MODEL, FFN_HIDDEN), D_MODEL ** -0.5),
        "w_down": nrm(ks[16], (DEPTH, FFN_HIDDEN, D_MODEL), FFN_HIDDEN ** -0.5),
        "norm_final": gain(ks[17], (D_MODEL,)),
    }


def reference(x, norm_mix, w_in, conv_w, cmp_pe_k, cmp_w1_k, cmp_w2_k, cmp_pe_v, cmp_w1_v,
              cmp_w2_v, norm_conv_out, norm_attn_out, w_out, norm_ffn, w_gate, w_up, w_down,
              norm_final):
    B, S, _ = x.shape
    positions = jnp.arange(S)
    split_idx = np.cumsum(PROJ_SIZES)[:-1].tolist()
    h = x
    for l in range(DEPTH):
        a = rms_norm(h, norm_mix[l])
        z = a @ w_in[l]
        (cb, cc, ch, zq, zkc, zvc, zks, zvs, zkw, zvw, zg) = jnp.split(z, split_idx, axis=-1)

        y_conv = cb * causal_depthwise_conv(cc * ch, conv_w[l])

        q = rope(zq.reshape(B, S, N_HEADS, HEAD_DIM), positions)
        q = q.reshape(B, S, N_KV_HEADS, GROUP, HEAD_DIM).transpose(0, 2, 3, 1, 4)

        def kv_heads(t):
            return t.reshape(B, S, N_KV_HEADS, HEAD_DIM)

        k_cmp = compress(rope(kv_heads(zkc), positions), cmp_pe_k[l], cmp_w1_k[l], cmp_w2_k[l])
        v_cmp = compress(kv_heads(zvc), cmp_pe_v[l], cmp_w1_v[l], cmp_w2_v[l])
        k_sel = rope(kv_heads(zks), positions).transpose(0, 2, 1, 3)
        v_sel = kv_heads(zvs).transpose(0, 2, 1, 3)
        k_win = rope(kv_heads(zkw), positions).transpose(0, 2, 1, 3)
        v_win = kv_heads(zvw).transpose(0, 2, 1, 3)
        o_c, o_s, o_w = nsa_attention(q, k_cmp, v_cmp, k_sel, v_sel, k_win, v_win)
        g = jax.nn.sigmoid(zg.reshape(B, S, N_HEADS, N_BRANCH))
        y_attn = (g[..., 0:1] * o_c + g[..., 1:2] * o_s + g[..., 2:3] * o_w).reshape(B, S, ATTN_W)

        y_mix = jnp.concatenate([rms_norm(y_conv, norm_conv_out[l]),
                                 rms_norm(y_attn, norm_attn_out[l])], axis=-1)
        h = h + y_mix @ w_out[l]

        f = rms_norm(h, norm_ffn[l])
        h = h + (jax.nn.silu(f @ w_gate[l]) * (f @ w_up[l])) @ w_down[l]
    return rms_norm(h, norm_final)
```

```python
import os
import numpy as np
import concourse.bass as bass
import concourse.mybir as mybir
from concourse.bass_utils import run_bass_kernel_spmd
from contextlib import ExitStack

F32 = mybir.dt.float32
BF16 = mybir.dt.bfloat16
U8 = mybir.dt.uint8
AF = mybir.ActivationFunctionType
ALU = mybir.AluOpType
AX = mybir.AxisListType

D = 2048
S = 8192
DP = 5656
FF = 5632
NEG = -30000.0
SCALE = 128 ** -0.5
EPS = 1e-6
ENGS = ["tensor", "vector", "scalar", "gpsimd", "sync"]
DEBUG = bool(int(os.environ.get("KDEBUG", "0")))
KSTOP = os.environ.get("KSTOP", "")
KCORES = int(os.environ.get("KCORES", "8"))
KS = int(os.environ.get("KS", "8192"))
KSKIP = set(os.environ.get("KSKIP", "").split(","))
_UA = {"xall", "w_in", "norm_mix", "cs_all", "ident"}
_UA2 = _UA | {"cmp_pe_k", "cmp_w1_k", "cmp_w2_k", "cmp_pe_v", "cmp_w1_v", "cmp_w2_v"}
_U0 = _UA2 | {"xown", "xhalo", "conv_w", "norm_conv_out", "cs_own", "Sh", "Hh"}
_UB = _U0 | {"cb_sel", "cb_win", "cb_cmp", "cbT_cmp", "sel_force", "sel_future", "E_all", "norm_attn_out"}
_USED = {"A": _UA, "A2": _UA2, "0": _U0, "B": _UB}
SHRINK_KEEP = _USED.get(KSTOP)


class Op:
    __slots__ = ("eng", "fn", "deps", "needs_inc", "count", "grp", "is_nop")


class DmaGroup:
    __slots__ = ("stream", "n", "final", "last")


class Prog:
    def __init__(self):
        self.q = {e: [] for e in ENGS}
        self.keys = {}
        self.streams = {}

    def dma_group(self, stream):
        g = DmaGroup()
        g.stream = stream
        g.n = 0
        g.final = None
        g.last = None
        self.streams.setdefault(stream, []).append(g)
        return g

    def op(self, eng, fn, reads=(), writes=(), grp=None, extra=()):
        o = Op()
        o.eng = eng
        o.fn = fn
        o.needs_inc = False
        o.count = None
        o.grp = grp
        o.is_nop = False
        if grp is not None:
            grp.n += 1
            grp.last = o
        ident = eng if grp is None else ("dma", id(grp))
        deps = set(extra)
        writes = list(writes) + [k for k in reads if isinstance(k, tuple) and k[0] == "ps" and k not in writes]
        reads = [k for k in reads if not (isinstance(k, tuple) and k[0] == "ps")]
        for k in reads:
            st = self.keys.setdefault(k, ({}, {}))
            deps.update(st[0].values())
            st[1][ident] = o
        for k in writes:
            st = self.keys.setdefault(k, ({}, {}))
            deps.update(st[0].values())
            deps.update(st[1].values())
            if st[1]:
                st[0].clear()
                st[1].clear()
            st[0][ident] = o
        deps.discard(o)
        o.deps = deps
        self.q[eng].append(o)
        return o

    def barrier(self):
        arr = []
        for e in ENGS:
            for o in reversed(self.q[e]):
                if o.grp is None and not getattr(o, "is_nop", False):
                    arr.append(o)
                    break
        lasts = [g[-1].last for g in self.streams.values() if g and g[-1].last is not None]
        for e in ENGS:
            o = self.op(e, lambda eng: eng.nop(), extra=arr + lasts)
            o.is_nop = True
        self.keys = {}

    def emit(self, nc, ctx):
        engsem = {e: ctx.enter_context(nc.semaphore("es_" + e)) for e in ENGS}
        ssem = {}
        for s, groups in self.streams.items():
            ssem[s] = ctx.enter_context(nc.semaphore("ds_" + str(s)))
            cum = 0
            for g in groups:
                cum += 16 * g.n
                g.final = cum
        for e in ENGS:
            for o in self.q[e]:
                for d in o.deps:
                    if d.grp is None and not (d.eng == e and e == "tensor"):
                        d.needs_inc = True
        for e in ENGS:
            c = 0
            for o in self.q[e]:
                if o.grp is None and o.needs_inc:
                    c += 1
                    o.count = c
        block = ctx.enter_context(nc.Block())
        prog = self

        def run(e):
            def body(eng):
                waited = {}
                for o in prog.q[e]:
                    needs = {}
                    for d in o.deps:
                        if d.grp is not None:
                            s, v, key = ssem[d.grp.stream], d.grp.final, ("s", d.grp.stream)
                        else:
                            if d.eng == e and e == "tensor":
                                continue
                            s, v, key = engsem[d.eng], d.count, ("e", d.eng)
                        if key not in needs or needs[key][1] < v:
                            needs[key] = (s, v)
                    for key, (s, v) in needs.items():
                        if waited.get(key, 0) < v:
                            eng.wait_ge(s, v)
                            waited[key] = v
                    ins = o.fn(eng)
                    if o.grp is not None:
                        ins.then_inc(ssem[o.grp.stream], 16)
                    elif o.needs_inc:
                        ins.then_inc(engsem[e], 1)
            return body

        block.tensor(run("tensor"))
        block.vector(run("vector"))
        block.scalar(run("scalar"))
        block.gpsimd(run("gpsimd"))
        block.sync(run("sync"))


class Arena:
    def __init__(self, t, size):
        self.t = t
        self.size = size
        self.off = 0

    def reset(self):
        self.off = 0

    def alloc(self, shape, dt):
        esz = 4 if dt == F32 else (2 if dt == BF16 else 1)
        n = 1
        for s in shape[1:]:
            n *= s
        nb = (n * esz + 63) // 64 * 64
        assert self.off + nb <= self.size, ("arena overflow", self.off, nb, self.size)
        ap = self.t[0:shape[0], self.off:self.off + n * esz].bitcast(dt)
        self.off += nb
        if len(shape) == 3:
            ap = ap.rearrange("p (a b) -> p a b", a=shape[1])
        elif len(shape) == 4:
            ap = ap.rearrange("p (a b c) -> p a b c", a=shape[1], b=shape[2])
        elif len(shape) == 5:
            ap = ap.rearrange("p (a b c d) -> p a b c d", a=shape[1], b=shape[2], c=shape[3])
        return ap


def build_nc():
    nc = bass.Bass("TRN2", target_bir_lowering=False)

    def din(name, shape):
        if SHRINK_KEEP is not None and name not in SHRINK_KEEP:
            shape = [1, 1]
        return nc.dram_tensor(name, list(shape), F32, kind="ExternalInput").ap()

    xall = din("xall", [KS, D])
    xown = din("xown", [2048, D])
    xhalo = din("xhalo", [32, D])
    w_in = din("w_in", [D, DP])
    w_out = din("w_out", [D, D])
    w_gate = din("w_gate", [D, FF])
    w_up = din("w_up", [D, FF])
    w_down = din("w_down", [FF, D])
    conv_w = din("conv_w", [3, 1024])
    pe_k = din("cmp_pe_k", [32, 128])
    w1_k = din("cmp_w1_k", [4096, 128])
    w2_k = din("cmp_w2_k", [128, 128])
    pe_v = din("cmp_pe_v", [32, 128])
    w1_v = din("cmp_w1_v", [4096, 128])
    w2_v = din("cmp_w2_v", [128, 128])
    g_mix = din("norm_mix", [D])
    g_conv = din("norm_conv_out", [1024])
    g_attn = din("norm_attn_out", [1024])
    g_ffn = din("norm_ffn", [D])
    g_fin = din("norm_final", [D])
    cs_all = din("cs_all", [KS, 128])
    cs_own = din("cs_own", [2048, 128])
    cb_sel_d = din("cb_sel", [128, 4 * 128])
    cb_win_d = din("cb_win", [128, 8 * 128])
    cb_cmp_d = din("cb_cmp", [16, 128, 2 * 128])
    cbT_cmp_d = din("cbT_cmp", [16, 128, 64])
    self_force_d = din("sel_force", [128, 9])
    self_future_d = din("sel_future", [128, 9])
    E_all_d = din("E_all", [128, 64 * 128])
    Sh_d = din("Sh", [128, 2 * 128])
    Hh_d = din("Hh", [32, 272])
    ident_d = din("ident", [128, 128])
    y = nc.dram_tensor("y", [2048, D], F32, kind="ExternalOutput").ap()

    skind = dict(kind="ExternalOutput") if DEBUG else {}
    KTs = nc.dram_tensor("KTs", [4, 2, 128, KS], BF16, **skind).ap()
    VSc = nc.dram_tensor("VSc", [2, KS, 2, 130], BF16, **skind).ap()
    qTs = nc.dram_tensor("qTs", [16, 128, 8, 128], BF16, **skind).ap()
    ymixT = nc.dram_tensor("ymixT", [16, 128, 16, 128], BF16, **skind).ap()
    if DEBUG:
        dbg_kc = nc.dram_tensor("dbg_kc", [128, 2 * 512], BF16, kind="ExternalOutput").ap()
        dbg_vc = nc.dram_tensor("dbg_vc", [128, 4 * 2 * 130], BF16, kind="ExternalOutput").ap()
        dbg_gates = nc.dram_tensor("dbg_gates", [128, 16 * 24], F32, kind="ExternalOutput").ap()

    ctx = ExitStack()
    with ctx:
        P = Prog()
        AR_SIZE = 176 * 1024
        arena_t = ctx.enter_context(nc.sbuf_tensor("arena", [128, AR_SIZE], U8))
        AR = Arena(arena_t, AR_SIZE)

        def sbt(name, shape, dt):
            return ctx.enter_context(nc.sbuf_tensor(name, shape, dt))

        ps = ctx.enter_context(nc.psum_tensor("ps", [128, 8, 512], F32))

        def bank(i):
            return ps[:, i, :]

        def bank_bf(i, n=1):
            return ps[:, i:i + n, :].bitcast(BF16)

        identF = sbt("identF", [128, 128], F32)
        identB = sbt("identB", [128, 128], BF16)
        eps_t = sbt("eps_t", [128, 1], F32)
        kcT = sbt("kcT", [128, 2, 512], BF16)
        vcc = sbt("vcc", [128, 4, 2, 130], BF16)
        gates = sbt("gates", [128, 16, 24], F32)
        u_halo = sbt("u_halo", [32, 1024], F32)
        aT_halo = sbt("aT_halo", [128, 16, 32], BF16)
        junk = sbt("junk", [128, 2048], BF16)
        stat = sbt("stat", [128, 64], F32)

        def dma(queue, out, in_, reads, writes, stream, grp=None):
            g = grp if grp is not None else P.dma_group(stream)
            P.op(queue, lambda e: e.dma_start(out=out, in_=in_), reads, writes, grp=g)
            return g

        def mm(out, lhsT, rhs, start, stop, reads, writes, skip=False):
            P.op("tensor", lambda e: e.matmul(out, lhsT=lhsT, rhs=rhs, start=start, stop=stop,
                                              skip_group_check=skip), reads, writes)

        def tr(out, in_, ident, reads, writes):
            P.op("tensor", lambda e: e.transpose(out=out, in_=in_, identity=ident), reads, writes)

        def act(out, in_, func, reads, writes, **kw):
            P.op("scalar", lambda e: e.activation(out=out, in_=in_, func=func, **kw), reads, writes)

        def vtt(out, in0, in1, op, reads, writes):
            P.op("vector", lambda e: e.tensor_tensor(out=out, in0=in0, in1=in1, op=op), reads, writes)

        def vts(out, in0, s1, s2, op0, op1, reads, writes):
            if op1 is None:
                P.op("vector", lambda e: e.tensor_scalar(out=out, in0=in0, scalar1=s1, scalar2=None, op0=op0),
                     reads, writes)
            else:
                P.op("vector", lambda e: e.tensor_scalar(out=out, in0=in0, scalar1=s1, scalar2=s2, op0=op0, op1=op1),
                     reads, writes)

        def vstt(out, in0, scalar, in1, op0, op1, reads, writes):
            P.op("vector", lambda e: e.scalar_tensor_tensor(out=out, in0=in0, scalar=scalar, in1=in1,
                                                            op0=op0, op1=op1), reads, writes)

        def vcopy(out, in_, reads, writes):
            P.op("vector", lambda e: e.tensor_copy(out=out, in_=in_), reads, writes)

        def vrecip(out, in_, reads, writes):
            P.op("vector", lambda e: e.reciprocal(out=out, in_=in_), reads, writes)

        def vmemset(ap, val, writes):
            P.op("vector", lambda e: e.memset(ap, val), (), writes)

        def bc_load(dst, src_row, key, stream):
            dma("sync", dst, src_row.partition_broadcast(128), (), [key], stream)

        def rms_rstd(src, n, col, kin, np_=128):
            ss = stat[0:np_, col:col + 1]
            rt = stat[0:np_, col + 1:col + 2]
            rs = stat[0:np_, col + 2:col + 3]
            act(junk[0:np_, 0:n], src, AF.Square, [kin], ["junk", ("stat", col)], accum_out=ss)
            act(rt, ss, AF.Sqrt, [("stat", col)], [("stat", col + 1)], scale=1.0 / n, bias=eps_t[0:np_, :])
            vrecip(rs, rt, [("stat", col + 1)], [("stat", col + 2)])
            return rs, ("stat", col + 2)

        def norm_transpose(src, ksrc, gain_bc, kgain, xb, kxb, pbank, dst, kdst, col, np_=128):
            rs, krs = rms_rstd(src, 2048, col, ksrc, np_)
            vstt(xb[0:np_, :], src, rs, gain_bc[0:np_, :], ALU.mult, ALU.mult, [ksrc, krs, kgain], [kxb])
            pt = bank_bf(pbank, 2).rearrange("p a (c t) -> p (a c) t", t=128)
            idn = identB[0:np_, 0:np_]
            for k in range(16):
                tr(pt[:, k, 0:np_], xb[0:np_, k * 128:(k + 1) * 128], idn, [kxb, "identB"],
                   [("ps", pbank), ("ps", pbank + 1)])
            act(dst, pt[:, :, 0:np_], AF.Copy, [("ps", pbank), ("ps", pbank + 1)], [kdst])

        def load_w(dst, src_cols, key, stream):
            dma("gpsimd", dst, src_cols.rearrange("(kc p) n -> p kc n", p=128), (), [key], stream)

        dma("sync", identF[:], ident_d, (), ["identF"], "c_id")
        vcopy(identB[:], identF[:], ["identF"], ["identB"])
        vmemset(eps_t[:], EPS, ["eps"])
        P.barrier()
        for _once in (0,):

            AR.reset()
            wkv = AR.alloc([128, 16, 1536], BF16)
            gmix = AR.alloc([128, 2048], F32)
            xs = [AR.alloc([128, 2048], F32) for _ in range(2)]
            xb = AR.alloc([128, 2048], BF16)
            aT = [AR.alloc([128, 16, 128], BF16) for _ in range(2)]
            cs = [AR.alloc([128, 2, 64], F32) for _ in range(2)]
            rt1 = AR.alloc([128, 3, 2, 64], F32)
            rt2 = AR.alloc([128, 3, 2, 64], F32)
            krope = AR.alloc([128, 3, 2, 2, 64], BF16)
            vcb = AR.alloc([128, 2, 128], BF16)
            vst = [AR.alloc([128, 2, 2, 130], BF16) for _ in range(2)]
            kts = [AR.alloc([128, 8, 128], BF16) for _ in range(2)]

            for g in range(3):
                load_w(wkv[:, :, g * 512:(g + 1) * 512], w_in[:, 4096 + g * 512:4096 + (g + 1) * 512], ("wkv", g), f"wkv{g}")
            bc_load(gmix, g_mix, "gmix", "c_gmix")
            for s_ in range(2):
                if "ms" not in KSKIP:
                    vmemset(vst[s_][:, :, :, 128:130], 1.0, [("vst1", s_)])
            NB_A = int(os.environ.get("KNBA", "64"))
            dma("sync", xs[0], xall[0:128, :], (), [("xs", 0)], "xs0")
            for i in range(NB_A):
                sl = i % 2
                if i + 1 < NB_A:
                    dma("sync", xs[1 - sl], xall[(i + 1) * 128:(i + 2) * 128, :], (), [("xs", 1 - sl)], f"xs{1 - sl}")
                dma("sync", cs[sl], cs_all[i * 128:(i + 1) * 128, :].rearrange("p (a b) -> p a b", a=2), (), [("cs", sl)], f"cs{sl}")
                pb = 0 if sl == 0 else 6
                norm_transpose(xs[sl], ("xs", sl), gmix, "gmix", xb, "xb", pb, aT[sl], ("aT", sl), 0)
                for g in range(3 if "mm" not in KSKIP else 0):
                    for k in range(16):
                        mm(bank(2 + g), aT[sl][:, k, :], wkv[:, k, g * 512:(g + 1) * 512], k == 0, k == 15,
                           [("aT", sl), ("wkv", g)], [("ps", 2 + g)])
                if "rope" not in KSKIP:
                    z = ps[:, 2:5, 0:256].rearrange("p t (h f x) -> p t h f x", h=2, f=2)
                    cosb = cs[sl][:, 0, :].unsqueeze(1).unsqueeze(1).to_broadcast([128, 3, 2, 64])
                    sinb = cs[sl][:, 1, :].unsqueeze(1).unsqueeze(1).to_broadcast([128, 3, 2, 64])
                    pk = [("ps", 2), ("ps", 3), ("ps", 4)]
                    vtt(rt1, z[:, :, :, 0, :], cosb, ALU.mult, pk + [("cs", sl)], ["rt1"])
                    vtt(rt2, z[:, :, :, 1, :], sinb, ALU.mult, pk + [("cs", sl)], ["rt2"])
                    vtt(krope[:, :, :, 0, :], rt1, rt2, ALU.subtract, ["rt1", "rt2"], ["krope0"])
                    vtt(rt1, z[:, :, :, 1, :], cosb, ALU.mult, pk + [("cs", sl)], ["rt1"])
                    vtt(rt2, z[:, :, :, 0, :], sinb, ALU.mult, pk + [("cs", sl)], ["rt2"])
                    vtt(krope[:, :, :, 1, :], rt1, rt2, ALU.add, ["rt1", "rt2"], ["krope1"])
                if "vcopy" not in KSKIP:
                    act(vcb, ps[:, 2, 256:512].rearrange("p (h x) -> p h x", h=2), AF.Copy, [("ps", 2)], ["vcb"])
                    for t_ in range(2):
                        act(vst[sl][:, t_, :, 0:128], ps[:, 3 + t_, 256:512].rearrange("p (h x) -> p h x", h=2), AF.Copy,
                            [("ps", 3 + t_)], [("vst", sl)])
                if "tr5" not in KSKIP:
                    pk5 = bank_bf(5).rearrange("p a (c t) -> p (a c) t", t=128)
                    for t in range(3):
                        for hk in range(2):
                            tr(pk5[:, t * 2 + hk, :], krope[:, t, hk, :, :].rearrange("p f x -> p (f x)"), identB[:],
                               ["krope0", "krope1", "identB"], [("ps", 5)])
                    for hk in range(2):
                        tr(pk5[:, 6 + hk, :], vcb[:, hk, :], identB[:], ["vcb", "identB"], [("ps", 5)])
                    vcopy(kts[sl], pk5, [("ps", 5)], [("kts", sl)])
                if "st" not in KSKIP:
                    dma("sync", KTs[:, :, :, i * 128:(i + 1) * 128].rearrange("t h d s -> d (t h) s"), kts[sl],
                        [("kts", sl)], [("KTs", i)], f"kst{sl}")
                    dma("sync", VSc[:, i * 128:(i + 1) * 128, :, :].rearrange("t s h c -> s t (h c)"),
                        vst[sl].rearrange("p t h c -> p t (h c)"), [("vst", sl), ("vst1", sl)], [("VSc", i)], f"vst{sl}")
            P.barrier()
            if KSTOP == "A":
                break

            AR.reset()
            w1b = [AR.alloc([128, 32, 128], BF16) for _ in range(2)]
            w2b = [AR.alloc([128, 128], BF16) for _ in range(2)]
            pe_f = [AR.alloc([32, 128], F32) for _ in range(2)]
            peT = [AR.alloc([128, 32], BF16) for _ in range(2)]
            c0 = [AR.alloc([128, 1], F32) for _ in range(2)]
            raw = [AR.alloc([128, S], BF16) for _ in range(2)]
            h1T = AR.alloc([128, 512], BF16)
            for ti, (w1d, w2d, ped) in enumerate([(w1_k, w2_k, pe_k), (w1_v, w2_v, pe_v)]):
                dma("gpsimd", w1b[ti], w1d.rearrange("(l d) e -> d l e", d=128), (), [("w1b", ti)], f"w1b{ti}")
                dma("gpsimd", w2b[ti], w2d, (), [("w2b", ti)], f"w2b{ti}")
                dma("sync", pe_f[ti], ped, (), [("pe_f", ti)], f"pef{ti}")
                tr(ps[:, 0, 0:32], pe_f[ti], identF[0:32, 0:32], [("pe_f", ti), "identF"], [("ps", 0)])
                vcopy(peT[ti], ps[:, 0, 0:32], [("ps", 0)], [("peT", ti)])
                for l in range(32):
                    mm(ps[:, 1, 0:1], w1b[ti][:, l, :], peT[ti][:, l:l + 1], l == 0, l == 31,
                       [("w1b", ti), ("peT", ti)], [("ps", 1)])
                vcopy(c0[ti], ps[:, 1, 0:1], [("ps", 1)], [("c0", ti)])
            vmemset(vcc[:, :, :, 128:130], 1.0, ["vcc1"])
            it = 0
            for ti in range(2):
                for hk in range(2):
                    sl = it % 2
                    it += 1
                    dma("sync", raw[sl], KTs[0 if ti == 0 else 3, hk, :, :], (), [("raw", sl)], f"raw{sl}")
                    r16 = raw[sl].rearrange("p (n s) -> p n s", s=16)
                    pb = 2 + sl
                    for l in range(32):
                        rhs = r16[:, 0:511, l] if l < 16 else r16[:, 1:512, l - 16]
                        mm(ps[:, pb, 0:511], w1b[ti][:, l, :], rhs, l == 0, l == 31, [("w1b", ti), ("raw", sl)], [("ps", pb)])
                    vmemset(h1T[:, 511:512], 0.0, ["h1T"])
                    act(h1T[:, 0:511], ps[:, pb, 0:511], AF.Silu, [("ps", pb), ("c0", ti)], ["h1T"], bias=c0[ti])
                    if ti == 0:
                        mm(bank(4), w2b[0], h1T, True, True, [("w2b", 0), "h1T"], [("ps", 4)])
                        vcopy(kcT[:, hk, :], bank(4), [("ps", 4)], ["kcT"])
                    else:
                        for nb in range(4):
                            mm(ps[:, 5, nb * 128:(nb + 1) * 128], h1T[:, nb * 128:(nb + 1) * 128], w2b[1], True, True,
                               [("w2b", 1), "h1T"], [("ps", 5)])
                        vcopy(vcc[:, :, hk, 0:128], ps[:, 5, :].rearrange("p (n x) -> p n x", n=4), [("ps", 5)], ["vcc"])
            if DEBUG:
                dma("sync", dbg_kc, kcT[:].rearrange("p a b -> p (a b)"), ["kcT"], ["dbg_kc"], "dbg1")
                dma("sync", dbg_vc, vcc[:].rearrange("p a b c -> p (a b c)"), ["vcc", "vcc1"], ["dbg_vc"], "dbg2")
            P.barrier()
            if KSTOP == "A2":
                break

            AR.reset()
            gmix = AR.alloc([128, 2048], F32)
            gconv = AR.alloc([128, 1024], F32)
            wcv = AR.alloc([128, 3, 1024], F32)
            shf = AR.alloc([128, 2, 128], F32)
            hfix = AR.alloc([32, 2, 128], F32)
            hmask = AR.alloc([32, 16], F32)
            uh = [AR.alloc([32, 256], F32) for _ in range(2)]
            xs = [AR.alloc([128, 2048], F32) for _ in range(2)]
            xb = AR.alloc([128, 2048], BF16)
            aTo = AR.alloc([128, 16, 1024], BF16)
            wch = [AR.alloc([128, 16, 256], BF16) for _ in range(4)]
            wgt = AR.alloc([128, 16, 24], BF16)
            ycb = AR.alloc([128, 8, 1024], BF16)
            cso = AR.alloc([128, 8, 2, 64], F32)
            ccs = [AR.alloc([128, 256], F32) for _ in range(2)]
            uu = [AR.alloc([128, 256], F32) for _ in range(2)]
            aa = [AR.alloc([128, 256], F32) for _ in range(2)]
            tb_ = [AR.alloc([128, 256], F32) for _ in range(2)]
            qr1 = AR.alloc([128, 2, 64], F32)
            qr2 = AR.alloc([128, 2, 64], F32)
            qrope = [AR.alloc([128, 2, 2, 64], BF16) for _ in range(2)]
            qts = [AR.alloc([128, 2, 128], BF16) for _ in range(2)]
            ycn = AR.alloc([128, 1024], BF16)
            ymT = [AR.alloc([128, 8, 128], BF16) for _ in range(2)]
            ssq = AR.alloc([128, 8, 4], F32)

            bc_load(gmix, g_mix, "gmix", "c_gmix")
            bc_load(gconv, g_conv, "gconv", "c_gconv")
            for kk in range(3):
                bc_load(wcv[:, kk, :], conv_w[kk, :], ("wcv", kk), f"c_wcv{kk}")
            dma("sync", shf, Sh_d.rearrange("p (a b) -> p a b", a=2), (), ["shf"], "c_sh")
            dma("sync", hfix, Hh_d[:, 0:256].rearrange("p (a b) -> p a b", a=2), (), ["hfix"], "c_hh")
            dma("sync", hmask, Hh_d[:, 256:272], (), ["hmask"], "c_hm")
            wcnt = [0]

            def next_w(src_cols, ncols=256):
                i = wcnt[0] % 4
                wcnt[0] += 1
                load_w(wch[i][:, :, 0:ncols], src_cols, ("wch", i), f"wch{i}")
                return wch[i], ("wch", i)

            for hf in range(2):
                if hf == 0:
                    dma("sync", xs[1][0:32, :], xhalo, (), [("xs", 1)], "xs1")
                    norm_transpose(xs[1][0:32, :], ("xs", 1), gmix, "gmix", xb, "xb", 0, aT_halo[:], "aT_halo", 0, np_=32)
                for mi in range(8):
                    m = hf * 8 + mi
                    sl = mi % 2
                    dma("sync", xs[sl], xown[m * 128:(m + 1) * 128, :], (), [("xs", sl)], f"xs{sl}")
                    norm_transpose(xs[sl], ("xs", sl), gmix, "gmix", xb, "xb", 0 if sl == 0 else 6,
                                   aTo[:, :, mi * 128:(mi + 1) * 128], ("aTo", mi), 0)
                dma("sync", cso, cs_own[hf * 1024:(hf + 1) * 1024, :].rearrange("(m p) (a b) -> p m a b", p=128, a=2),
                    (), ["cso"], "cso")
                for cg in range(4):
                    c0_, c1_ = cg * 256, (cg + 1) * 256
                    Wb, kWb = next_w(w_in[:, c0_:c1_])
                    Wc, kWc = next_w(w_in[:, 1024 + c0_:1024 + c1_])
                    Wh, kWh = next_w(w_in[:, 2048 + c0_:2048 + c1_])
                    if hf == 0:
                        for k in range(16):
                            mm(ps[0:32, 2, 0:256], aT_halo[:, k, :], Wc[:, k, :], k == 0, k == 15, ["aT_halo", kWc], [("ps", 2)])
                        for k in range(16):
                            mm(ps[0:32, 2, 256:512], aT_halo[:, k, :], Wh[:, k, :], k == 0, k == 15, ["aT_halo", kWh], [("ps", 2)])
                        act(ccs[0][0:32, :], ps[0:32, 2, 0:256], AF.Copy, [("ps", 2)], [("ccs", 0)])
                        vtt(u_halo[:, c0_:c1_], ps[0:32, 2, 256:512], ccs[0][0:32, :], ALU.mult, [("ps", 2), ("ccs", 0)], ["u_halo"])
                    for mi in range(8):
                        m = hf * 8 + mi
                        sl = mi % 2
                        bA, bB, bC = (2, 3, 4) if sl == 0 else (5, 6, 7)
                        at = aTo[:, :, mi * 128:(mi + 1) * 128]
                        for k in range(16):
                            mm(ps[:, bA, 0:256], at[:, k, :], Wc[:, k, :], k == 0, k == 15, [("aTo", mi), kWc], [("ps", bA)])
                        for k in range(16):
                            mm(ps[:, bA, 256:512], at[:, k, :], Wh[:, k, :], k == 0, k == 15, [("aTo", mi), kWh], [("ps", bA)])
                        for k in range(16):
                            mm(ps[:, bB, 0:256], at[:, k, :], Wb[:, k, :], k == 0, k == 15, [("aTo", mi), kWb], [("ps", bB)])
                        act(ccs[sl], ps[:, bA, 0:256], AF.Copy, [("ps", bA)], [("ccs", sl)])
                        vtt(uu[sl], ps[:, bA, 256:512], ccs[sl], ALU.mult, [("ps", bA), ("ccs", sl)], [("uu", sl)])
                        vts(uh[sl], u_halo[:, c0_:c1_], hmask[:, m:m + 1], None, ALU.mult, None, ["u_halo", "hmask"], [("uh", sl)])
                        mm(ps[:, bC, 0:256], shf[:, 0, :], uu[sl], True, False, ["shf", ("uu", sl)], [("ps", bC)])
                        mm(ps[:, bC, 0:256], hfix[:, 0, :], uh[sl], False, True, ["hfix", ("uh", sl)], [("ps", bC)])
                        mm(ps[:, bC, 256:512], shf[:, 1, :], uu[sl], True, False, ["shf", ("uu", sl)], [("ps", bC)])
                        mm(ps[:, bC, 256:512], hfix[:, 1, :], uh[sl], False, True, ["hfix", ("uh", sl)], [("ps", bC)])
                        vtt(aa[sl], uu[sl], wcv[:, 2, c0_:c1_], ALU.mult, [("uu", sl), ("wcv", 2)], [("aa", sl)])
                        vtt(tb_[sl], ps[:, bC, 0:256], wcv[:, 1, c0_:c1_], ALU.mult, [("ps", bC), ("wcv", 1)], [("tb", sl)])
                        vtt(aa[sl], aa[sl], tb_[sl], ALU.add, [("aa", sl), ("tb", sl)], [("aa", sl)])
                        vtt(tb_[sl], ps[:, bC, 256:512], wcv[:, 0, c0_:c1_], ALU.mult, [("ps", bC), ("wcv", 0)], [("tb", sl)])
                        vtt(aa[sl], aa[sl], tb_[sl], ALU.add, [("aa", sl), ("tb", sl)], [("aa", sl)])
                        vtt(ycb[:, mi, c0_:c1_], ps[:, bB, 0:256], aa[sl], ALU.mult, [("ps", bB), ("aa", sl)], [("ycb", mi, cg)])
                        act(junk[:, 0:256], ycb[:, mi, c0_:c1_], AF.Square, [("ycb", mi, cg)], ["junk", ("ssq", mi, cg)],
                            accum_out=ssq[:, mi, cg:cg + 1])
                for qc in range(4):
                    Wq, kWq = next_w(w_in[:, 3072 + qc * 256:3072 + (qc + 1) * 256])
                    for mi in range(8):
                        m = hf * 8 + mi
                        sl = mi % 2
                        bq = 2 if sl == 0 else 5
                        at = aTo[:, :, mi * 128:(mi + 1) * 128]
                        for k in range(16):
                            mm(ps[:, bq, 0:256], at[:, k, :], Wq[:, k, :], k == 0, k == 15, [("aTo", mi), kWq], [("ps", bq)])
                        z = ps[:, bq, 0:256].rearrange("p (h f x) -> p h f x", h=2, f=2)
                        cosb = cso[:, mi, 0, :].unsqueeze(1).to_broadcast([128, 2, 64])
                        sinb = cso[:, mi, 1, :].unsqueeze(1).to_broadcast([128, 2, 64])
                        kz = [("ps", bq), "cso"]
                        vtt(qr1, z[:, :, 0, :], cosb, ALU.mult, kz, ["qr1"])
                        vtt(qr2, z[:, :, 1, :], sinb, ALU.mult, kz, ["qr2"])
                        vtt(qrope[sl][:, :, 0, :], qr1, qr2, ALU.subtract, ["qr1", "qr2"], [("qrope0", sl)])
                        vtt(qr1, z[:, :, 1, :], cosb, ALU.mult, kz, ["qr1"])
                        vtt(qr2, z[:, :, 0, :], sinb, ALU.mult, kz, ["qr2"])
                        vtt(qrope[sl][:, :, 1, :], qr1, qr2, ALU.add, ["qr1", "qr2"], [("qrope1", sl)])
                        bt = 3 if sl == 0 else 6
                        ptq = bank_bf(bt)[:, 0, 0:256].rearrange("p (h t) -> p h t", h=2)
                        for hh in range(2):
                            tr(ptq[:, hh, :], qrope[sl][:, hh, :, :].rearrange("p f x -> p (f x)"), identB[:],
                               [("qrope0", sl), ("qrope1", sl), "identB"], [("ps", bt)])
                        act(qts[sl], ptq, AF.Copy, [("ps", bt)], [("qts", sl)])
                        dma("sync", qTs[m, :, qc * 2:qc * 2 + 2, :], qts[sl], [("qts", sl)], [("qTs", m, qc)], f"qts{sl}")
                load_w(wgt, w_in[:, 5632:5656], "wgt", "wgt")
                for mi in range(8):
                    m = hf * 8 + mi
                    bq = 4 if mi % 2 == 0 else 7
                    at = aTo[:, :, mi * 128:(mi + 1) * 128]
                    for k in range(16):
                        mm(ps[:, bq, 0:24], at[:, k, :], wgt[:, k, :], k == 0, k == 15, [("aTo", mi), "wgt"], [("ps", bq)])
                    act(gates[:, m, :], ps[:, bq, 0:24], AF.Sigmoid, [("ps", bq)], [("gates", m)])
                for mi in range(8):
                    m = hf * 8 + mi
                    sl = mi % 2
                    P.op("vector", lambda e, mi=mi: e.tensor_reduce(out=stat[:, 8:9], in_=ssq[:, mi, :], axis=AX.X, op=ALU.add),
                         [("ssq", mi, c) for c in range(4)], [("stat", 8)])
                    act(stat[:, 9:10], stat[:, 8:9], AF.Sqrt, [("stat", 8)], [("stat", 9)], scale=1.0 / 1024, bias=eps_t[:])
                    vrecip(stat[:, 10:11], stat[:, 9:10], [("stat", 9)], [("stat", 10)])
                    vstt(ycn, ycb[:, mi, :], stat[:, 10:11], gconv, ALU.mult, ALU.mult,
                         [("ycb", mi, c) for c in range(4)] + [("stat", 10), "gconv"], ["ycn"])
                    bt = 3 if sl == 0 else 6
                    pty = bank_bf(bt).rearrange("p a (c t) -> p (a c) t", t=128)
                    for c in range(8):
                        tr(pty[:, c, :], ycn[:, c * 128:(c + 1) * 128], identB[:], ["ycn", "identB"], [("ps", bt)])
                    act(ymT[sl], pty, AF.Copy, [("ps", bt)], [("ymT", sl)])
                    dma("sync", ymixT[m, :, 0:8, :], ymT[sl], [("ymT", sl)], [("ymixT", m, 0)], f"ymT{sl}")
            if DEBUG:
                dma("sync", dbg_gates, gates[:].rearrange("p a b -> p (a b)"), [("gates", m) for m in range(16)], ["dbg_g"], "dbg3")
            P.barrier()
            if KSTOP == "0":
                break

            AR.reset()
            ksT = AR.alloc([128, 2, S], BF16)
            vsA = AR.alloc([128, 64, 2, 130], BF16)
            Eall = AR.alloc([128, 64, 128], BF16)
            cbs = AR.alloc([128, 4, 4, 128], BF16)
            cbw = AR.alloc([128, 8, 4, 128], BF16)
            cbs_f = AR.alloc([128, 8, 128], F32)
            gattn = AR.alloc([128, 1024], F32)
            sfz = AR.alloc([128, 9], F32)
            suz = AR.alloc([128, 9], F32)
            qT = [AR.alloc([128, 8, 128], BF16) for _ in range(2)]
            kwT = [AR.alloc([128, 2, 1024], BF16) for _ in range(2)]
            vwA = [AR.alloc([128, 8, 2, 130], BF16) for _ in range(2)]
            cbc_f = [AR.alloc([128, 2, 128], F32) for _ in range(2)]
            cbc = [AR.alloc([128, 2, 4, 128], BF16) for _ in range(2)]
            cbT = [AR.alloc([128, 64], F32) for _ in range(2)]
            ee = [AR.alloc([128, 512], F32) for _ in range(2)]
            psum_ = AR.alloc([128, 512], F32)
            imp = AR.alloc([128, 128], F32)
            imp2 = AR.alloc([128, 128], F32)
            m8 = AR.alloc([128, 16], F32)
            selb = AR.alloc([128, 128], F32)
            sbT = [AR.alloc([128, 4, 128], BF16) for _ in range(2)]
            pT = [AR.alloc([128, 512], BF16) for _ in range(3)]
            yat = AR.alloc([128, 8, 128], F32)
            ytmp = AR.alloc([128, 8, 128], F32)
            coef = AR.alloc([128, 8], F32)
            dden = AR.alloc([128, 8], F32)
            yan = AR.alloc([128, 1024], BF16)
            ymT2 = [AR.alloc([128, 8, 128], BF16) for _ in range(2)]
            Dh = AR.alloc([128, 8], F32)

            for hk in range(2):
                dma("sync", ksT[:, hk, :], KTs[1, hk, :, :], (), [("ksT", hk)], f"ksT{hk}")
            for qd in range(4):
                dma("sync", vsA[:, qd * 16:(qd + 1) * 16, :, :].rearrange("p b h c -> p b (h c)"),
                    VSc[0, qd * 2048:(qd + 1) * 2048, :, :].rearrange("(b p) h c -> p b (h c)", p=128), (), [("vsA", qd)], f"vsA{qd}")
            for qd in range(8):
                dma("gpsimd", Eall[:, qd * 8:(qd + 1) * 8, :], E_all_d[:, qd * 1024:(qd + 1) * 1024].rearrange("p (a b) -> p a b", a=8),
                    (), [("Eall", qd // 2)], f"Eall{qd}")
            dma("sync", cbs_f[:, 0:4, :], cb_sel_d.rearrange("p (a b) -> p a b", a=4), (), ["cbs_f"], "c_cbs")
            vcopy(cbs, cbs_f[:, 0:4, :].unsqueeze(2).to_broadcast([128, 4, 4, 128]), ["cbs_f"], ["cbs"])
            dma("sync", cbs_f, cb_win_d.rearrange("p (a b) -> p a b", a=8), ["cbs_f"], ["cbs_f"], "c_cbs")
            vcopy(cbw, cbs_f.unsqueeze(2).to_broadcast([128, 8, 4, 128]), ["cbs_f"], ["cbw"])
            bc_load(gattn, g_attn, "gattn", "c_gattn")
            dma("sync", sfz, self_force_d, (), ["sfz"], "c_sfz")
            dma("sync", suz, self_future_d, (), ["suz"], "c_suz")
            vmemset(imp, -1.0, ["imp"])

            sctr = [0]
            pctr = [0]
            S_BANKS = [0, 1]
            O_BANK0 = 2

            def o_view(h):
                return ps[:, O_BANK0 + h // 2, (h % 2) * 256:(h % 2) * 256 + 129]

            def attend(m, hk, visits, o_started):
                qsl = m % 2
                qrhs = qT[qsl][:, 4 * hk:4 * hk + 4, :].rearrange("p g q -> p (g q)")
                for vi, (kT_ap, kkeys, v_ap, vkeys, biases) in enumerate(visits):
                    last = vi == len(visits) - 1
                    sb_ = S_BANKS[sctr[0] % 2]
                    sctr[0] += 1
                    pi = pctr[0] % 3
                    pctr[0] += 1
                    nb = len(biases)
                    mm(bank(sb_), kT_ap, qrhs, True, nb == 0, kkeys + [("qT", qsl)], [("ps", sb_)])
                    for bi, (bl, br, bk) in enumerate(biases):
                        mm(bank(sb_), bl, br, False, bi == nb - 1, bk, [("ps", sb_)])
                    act(pT[pi], bank(sb_), AF.Exp, [("ps", sb_)], [("pT", pi)], scale=SCALE)
                    for g_ in range(4):
                        h = 4 * hk + g_
                        bnk = O_BANK0 + h // 2
                        st = bnk not in o_started
                        o_started.add(bnk)
                        mm(o_view(h), pT[pi][:, g_ * 128:(g_ + 1) * 128], v_ap, st, last, [("pT", pi)] + vkeys,
                           [("ps", bnk)], skip=True)

            def finalize(m, br, first):
                ov = ps[:, O_BANK0:O_BANK0 + 4, :].rearrange("p b (h x) -> p (b h) x", h=2)
                okeys = [("ps", O_BANK0 + b_) for b_ in range(4)]
                vts(dden, ov[:, :, 128], 1e-30, None, ALU.max, None, okeys, ["dden"])
                vrecip(dden, dden, ["dden"], ["dden"])
                gv = gates[:, m, :].rearrange("p (h b) -> p h b", b=3)[:, :, br]
                vtt(coef, dden, gv, ALU.mult, ["dden", ("gates", m)], ["coef"])
                cb_ = coef[:, :].unsqueeze(2).to_broadcast([128, 8, 128])
                if first:
                    vtt(yat, ov[:, :, 0:128], cb_, ALU.mult, okeys + ["coef"], ["yat"])
                else:
                    vtt(ytmp, ov[:, :, 0:128], cb_, ALU.mult, okeys + ["coef"], ["ytmp"])
                    vtt(yat, yat, ytmp, ALU.add, ["yat", "ytmp"], ["yat"])

            for m in range(16):
                qsl = m % 2
                dma("sync", qT[qsl], qTs[m], (), [("qT", qsl)], f"qT{qsl}")
                nwin0 = 4 if m == 0 else 0
                t0 = 512 * (m - 1)
                for hk in range(2):
                    if m == 0:
                        dma("sync", kwT[qsl][:, hk, 512:1024], KTs[2, hk, :, 0:512], (), [("kwT", qsl)], f"kwT{qsl}")
                    else:
                        dma("sync", kwT[qsl][:, hk, :], KTs[2, hk, :, t0:t0 + 1024], (), [("kwT", qsl)], f"kwT{qsl}")
                if m == 0:
                    dma("sync", vwA[qsl][:, 4:8, :, :].rearrange("p b h c -> p b (h c)"),
                        VSc[1, 0:512, :, :].rearrange("(b p) h c -> p b (h c)", p=128), (), [("vwA", qsl)], f"vwA{qsl}")
                else:
                    dma("sync", vwA[qsl].rearrange("p b h c -> p b (h c)"),
                        VSc[1, t0:t0 + 1024, :, :].rearrange("(b p) h c -> p b (h c)", p=128), (), [("vwA", qsl)], f"vwA{qsl}")
                dma("sync", cbc_f[qsl], cb_cmp_d[m].rearrange("p (a b) -> p a b", a=2), (), [("cbc_f", qsl)], f"cbcf{qsl}")
                vcopy(cbc[qsl], cbc_f[qsl].unsqueeze(2).to_broadcast([128, 2, 4, 128]), [("cbc_f", qsl)], [("cbc", qsl)])
                dma("sync", cbT[qsl], cbT_cmp_d[m], (), [("cbT", qsl)], f"cbT{qsl}")
                Nm = 32 * (m + 1)
                ns = 8 * (m + 1)
                NBc = m // 4 + 1
                zlo = max(0, 32 * m - 32)
                zc0 = zlo - (32 * m - 32)
                for hk in range(2):
                    for g_ in range(4):
                        h = 4 * hk + g_
                        tbk = 6 + (h % 2)
                        esl = h % 2
                        mm(ps[:, tbk, 0:Nm], qT[qsl][:, h, :], kcT[:, hk, 0:Nm], True, True, [("qT", qsl), "kcT"], [("ps", tbk)])
                        vtt(ps[:, tbk, zlo:Nm], ps[:, tbk, zlo:Nm], cbT[qsl][:, zc0:64], ALU.add,
                            [("ps", tbk), ("cbT", qsl)], [("ps", tbk)])
                        act(ee[esl][:, 0:Nm], ps[:, tbk, 0:Nm], AF.Exp, [("ps", tbk)], [("ee", esl), ("Dh", h)],
                            scale=SCALE, accum_out=Dh[:, h:h + 1])
                        vts(Dh[:, h:h + 1], Dh[:, h:h + 1], 1e-30, None, ALU.max, None, [("Dh", h)], [("Dh", h)])
                        vrecip(Dh[:, h:h + 1], Dh[:, h:h + 1], [("Dh", h)], [("Dh", h)])
                        if g_ == 0:
                            vts(psum_[:, 0:Nm], ee[esl][:, 0:Nm], Dh[:, h:h + 1], None, ALU.mult, None,
                                [("ee", esl), ("Dh", h)], ["psum"])
                        else:
                            vstt(psum_[:, 0:Nm], ee[esl][:, 0:Nm], Dh[:, h:h + 1], psum_[:, 0:Nm], ALU.mult, ALU.add,
                                 [("ee", esl), ("Dh", h), "psum"], ["psum"])
                    P.op("vector", lambda e, Nm=Nm, ns=ns: e.tensor_reduce(
                        out=imp[:, 0:ns], in_=psum_[:, 0:Nm].rearrange("p (s f) -> p s f", f=4), axis=AX.X, op=ALU.add),
                        ["psum"], ["imp"])
                    vtt(imp[:, 1:ns], imp[:, 1:ns], psum_[:, 0:Nm].rearrange("p (s f) -> p s f", f=4)[:, 0:ns - 1, 3], ALU.add,
                        ["imp", "psum"], ["imp"])
                    vmemset(imp[:, 0:1], 10.0, ["imp"])
                    if m == 0:
                        zs, zt = imp[:, 0:8], slice(1, 9)
                    else:
                        zs, zt = imp[:, 8 * m - 1:8 * m + 8], slice(0, 9)
                    vtt(zs, zs, sfz[:, zt], ALU.max, ["imp", "sfz"], ["imp"])
                    vtt(zs, zs, suz[:, zt], ALU.min, ["imp", "suz"], ["imp"])
                    P.op("vector", lambda e: e.max(out=m8[:, 0:8], in_=imp[:, :]), ["imp"], ["m8a"])
                    P.op("vector", lambda e: e.match_replace(out=imp2[:, :], in_to_replace=m8[:, 0:8], in_values=imp[:, :],
                                                             imm_value=-2.0), ["imp", "m8a"], ["imp2"])
                    P.op("vector", lambda e: e.max(out=m8[:, 8:16], in_=imp2[:, :]), ["imp2"], ["m8b"])
                    vts(selb, imp, m8[:, 15:16], 1.0, ALU.is_ge, ALU.subtract, ["imp", "m8b"], ["selb"])
                    tr(ps[:, 6, 0:128], selb, identF[:], ["selb", "identF"], [("ps", 6)])
                    act(sbT[hk], ps[:, 6, 0:128].unsqueeze(1).to_broadcast([128, 4, 128]), AF.Copy, [("ps", 6)], [("sbT", hk)],
                        scale=-NEG)
                idB = identB[:]
                o_started = set()
                for hk in range(2):
                    visits = []
                    for nb in range(NBc):
                        biases = []
                        w_ = nb - (NBc - 2)
                        if w_ >= 0:
                            biases.append((idB, cbc[qsl][:, w_, :, :].rearrange("p g q -> p (g q)"), ["identB", ("cbc", qsl)]))
                        visits.append((kcT[:, hk, nb * 128:(nb + 1) * 128], ["kcT"], vcc[:, nb, hk, 0:129], ["vcc", "vcc1"], biases))
                    attend(m, hk, visits, o_started)
                finalize(m, 0, True)
                o_started = set()
                for hk in range(2):
                    visits = []
                    for jj in range(nwin0, 8):
                        biases = [(idB, cbw[:, jj, :, :].rearrange("p g q -> p (g q)"), ["identB", "cbw"])]
                        visits.append((kwT[qsl][:, hk, jj * 128:(jj + 1) * 128], [("kwT", qsl)], vwA[qsl][:, jj, hk, 0:129],
                                       [("vwA", qsl)], biases))
                    attend(m, hk, visits, o_started)
                finalize(m, 2, False)
                o_started = set()
                for hk in range(2):
                    visits = []
                    for kb in range(4 * m + 4):
                        biases = [(Eall[:, kb, :], sbT[hk].rearrange("p g q -> p (g q)"), [("Eall", kb // 16), ("sbT", hk)])]
                        if kb >= 4 * m:
                            biases.append((idB, cbs[:, kb - 4 * m, :, :].rearrange("p g q -> p (g q)"), ["identB", "cbs"]))
                        visits.append((ksT[:, hk, kb * 128:(kb + 1) * 128], [("ksT", hk)], vsA[:, kb, hk, 0:129],
                                       [("vsA", kb // 16)], biases))
                    attend(m, hk, visits, o_started)
                finalize(m, 1, False)
                yflat = yat.rearrange("p h x -> p (h x)")
                rs, krs = rms_rstd(yflat, 1024, 16, "yat")
                vstt(yan, yflat, rs, gattn, ALU.mult, ALU.mult, ["yat", krs, "gattn"], ["yan"])
                pty = bank_bf(7).rearrange("p a (c t) -> p (a c) t", t=128)
                for c in range(8):
                    tr(pty[:, c, :], yan[:, c * 128:(c + 1) * 128], identB[:], ["yan", "identB"], [("ps", 7)])
                act(ymT2[qsl], pty, AF.Copy, [("ps", 7)], [("ymT2", qsl)])
                dma("sync", ymixT[m, :, 8:16, :], ymT2[qsl], [("ymT2", qsl)], [("ymixT", m, 1)], f"ymT2{qsl}")
            P.barrier()
            if KSTOP == "B":
                break

            AR.reset()
            gbuf = AR.alloc([128, 2048], F32)
            ymx = [AR.alloc([128, 16, 128], BF16) for _ in range(2)]
            hh_ = AR.alloc([128, 4, 2048], F32)
            xb = AR.alloc([128, 2048], BF16)
            fT = AR.alloc([128, 16, 512], BF16)
            actT = AR.alloc([128, 44, 512], BF16)
            wst = [AR.alloc([128, 16, 256], BF16) for _ in range(3)]
            stg = [AR.alloc([128, 2816], F32) for _ in range(2)]
            wdn = [AR.alloc([128, 11, 256], BF16) for _ in range(2)]
            sg = [AR.alloc([128, 512], F32) for _ in range(2)]
            wc2 = [0]

            cst = [0]

            def load_cast(dst, src, kdst, a, n):
                i = cst[0] % 2
                cst[0] += 1
                st = stg[i][:, 0:a * n].rearrange("p (a n) -> p a n", a=a)
                dma("sync", st, src, (), [("stg", i)], f"stg{i}")
                if i == 0:
                    vcopy(dst, st, [("stg", i)], [kdst])
                else:
                    act(dst, st, AF.Copy, [("stg", i)], [kdst])

            def next_w2(src_cols):
                i = wc2[0] % 3
                wc2[0] += 1
                sv = src_cols.rearrange("(kc p) n -> p kc n", p=128)
                for hf_ in range(2):
                    load_cast(wst[i][:, hf_ * 8:(hf_ + 1) * 8, :], sv[:, hf_ * 8:(hf_ + 1) * 8, :], ("wst", i), 8, 256)
                return wst[i], ("wst", i)

            dctr = [0]
            octr = [0]
            for tt in range(4):
                for tb in range(4):
                    m = tt * 4 + tb
                    dma("sync", hh_[:, tb, :], xown[m * 128:(m + 1) * 128, :], (), [("hh", tb)], f"hh{tb}")
                for oc in range(8):
                    Wo, kWo = next_w2(w_out[:, oc * 256:(oc + 1) * 256])
                    for tb in range(4):
                        m = tt * 4 + tb
                        sl = tb % 2
                        dma("sync", ymx[sl], ymixT[m], (), [("ymx", sl)], f"ymx{sl}")
                        ob = octr[0] % 2
                        octr[0] += 1
                        for k in range(16):
                            mm(ps[:, ob, 0:256], ymx[sl][:, k, :], Wo[:, k, :], k == 0, k == 15, [("ymx", sl), kWo], [("ps", ob)])
                        vtt(hh_[:, tb, oc * 256:(oc + 1) * 256], ps[:, ob, 0:256], hh_[:, tb, oc * 256:(oc + 1) * 256], ALU.add,
                            [("ps", ob), ("hh", tb)], [("hh", tb)])
                bc_load(gbuf, g_ffn, "gbuf", "c_gbuf")
                for tb in range(4):
                    norm_transpose(hh_[:, tb, :], ("hh", tb), gbuf, "gbuf", xb, "xb", 2 if tb % 2 == 0 else 4,
                                   fT[:, :, tb * 128:(tb + 1) * 128], ("fT", tb), 20)
                fkeys = [("fT", tb) for tb in range(4)]
                for hp in range(22):
                    Wg, kWg = next_w2(w_gate[:, hp * 256:(hp + 1) * 256])
                    Wu, kWu = next_w2(w_up[:, hp * 256:(hp + 1) * 256])
                    for c2 in range(2):
                        hc = hp * 2 + c2
                        gb = 0 + 2 * (hc % 2)
                        ub = gb + 1
                        for k in range(16):
                            mm(bank(gb), Wg[:, k, c2 * 128:(c2 + 1) * 128], fT[:, k, :], k == 0, k == 15, [kWg] + fkeys, [("ps", gb)])
                        for k in range(16):
                            mm(bank(ub), Wu[:, k, c2 * 128:(c2 + 1) * 128], fT[:, k, :], k == 0, k == 15, [kWu] + fkeys, [("ps", ub)])
                        act(sg[hc % 2], bank(gb), AF.Silu, [("ps", gb)], [("sg", hc % 2)])
                        vtt(actT[:, hc, :], bank(ub), sg[hc % 2], ALU.mult, [("ps", ub), ("sg", hc % 2)], [("actT", hc)])
                wdv = w_down.rearrange("(hc p) n -> p hc n", p=128)
                for cg in range(8):
                    for hq in range(4):
                        di = dctr[0] % 2
                        dctr[0] += 1
                        load_cast(wdn[di], wdv[:, hq * 11:(hq + 1) * 11, cg * 256:(cg + 1) * 256], ("wdn", di), 11, 256)
                        for tb in range(4):
                            for c in range(11):
                                hc = hq * 11 + c
                                mm(ps[:, 4 + tb, 0:256], actT[:, hc, tb * 128:(tb + 1) * 128], wdn[di][:, c, :], hc == 0, hc == 43,
                                   [("actT", hc), ("wdn", di)], [("ps", 4 + tb)])
                    for tb in range(4):
                        vtt(hh_[:, tb, cg * 256:(cg + 1) * 256], ps[:, 4 + tb, 0:256], hh_[:, tb, cg * 256:(cg + 1) * 256], ALU.add,
                            [("ps", 4 + tb), ("hh", tb)], [("hh", tb)])
                bc_load(gbuf, g_fin, "gbuf", "c_gbuf")
                for tb in range(4):
                    m = tt * 4 + tb
                    rs, krs = rms_rstd(hh_[:, tb, :], 2048, 24, ("hh", tb))
                    vstt(hh_[:, tb, :], hh_[:, tb, :], rs, gbuf, ALU.mult, ALU.mult, [("hh", tb), krs, "gbuf"], [("hh", tb)])
                    dma("sync", y[m * 128:(m + 1) * 128, :], hh_[:, tb, :], [("hh", tb)], [("y", m)], f"yout{tb}")
            o_ = P.op("sync", lambda e: e.nop(), [("y", m) for m in range(16)], ())
            o_.is_nop = True
            P.barrier()
        P.emit(nc, ctx)
    return nc


def _tables(j):
    f32 = np.float32
    k = np.arange(128)[:, None]
    q = np.arange(128)[None, :]
    cb_sel = np.zeros((128, 4, 128), f32)
    for jj in range(4):
        if jj < j:
            v = np.ones((128, 128), bool)
        elif jj == j:
            v = k <= q
        else:
            v = np.zeros((128, 128), bool)
        cb_sel[:, jj, :] = np.where(v, 0.0, NEG)
    cb_win = np.zeros((128, 8, 128), f32)
    for jj in range(8):
        delta = 128 * (j + 4 - jj) + q - k
        cb_win[:, jj, :] = np.where((delta >= 0) & (delta < 512), 0.0, NEG)
    cb_cmp = np.zeros((16, 128, 2, 128), f32)
    cbT_cmp = np.zeros((16, 128, 64), f32)
    for m in range(16):
        t = 128 * (4 * m + j) + np.arange(128)
        NBc = m // 4 + 1
        for w in range(2):
            nb = NBc - 2 + w
            n = 128 * nb + np.arange(128)
            valid = (16 * n[:, None] + 31 <= t[None, :]) & (n[:, None] >= 0)
            cb_cmp[m, :, w, :] = np.where(valid, 0.0, NEG)
        n = 32 * m - 32 + np.arange(64)
        valid = 16 * n[None, :] + 31 <= t[:, None]
        cbT_cmp[m] = np.where(valid, 0.0, NEG)
    qq = np.arange(128)[:, None]
    c = np.arange(9)[None, :]
    rel = c - 1 - 2 * j - (qq >= 64)
    sel_force = np.where((rel == 0) | (rel == -1), 10.0, -1e9).astype(f32)
    sel_future = np.where(rel > 0, -1.0, 1e9).astype(f32)
    return dict(cb_sel=cb_sel.reshape(128, 512), cb_win=cb_win.reshape(128, 1024),
                cb_cmp=cb_cmp.reshape(16, 128, 256), cbT_cmp=cbT_cmp, sel_force=sel_force, sel_future=sel_future)


def _shared_tables():
    f32 = np.float32
    half = 64
    inv = 1.0 / (10000.0 ** (np.arange(half, dtype=np.float32) / half))
    ang = np.arange(S, dtype=np.float32)[:, None] * inv[None, :]
    cs_all = np.concatenate([np.cos(ang), np.sin(ang)], axis=1).astype(f32)
    s = np.arange(128)[:, None, None]
    kb = np.arange(64)[None, :, None]
    kk = np.arange(128)[None, None, :]
    E_all = (s == 2 * kb + kk // 64).astype(f32).reshape(128, 64 * 128)
    kr = np.arange(128)[:, None]
    mc = np.arange(128)[None, :]
    Sh = np.stack([(kr == mc - 1), (kr == mc - 2)], axis=1).astype(f32).reshape(128, 256)
    Hh = np.zeros((32, 272), f32)
    r = np.arange(32)
    Hh[r % 2 == 1, 0] = 1.0
    Hh[r % 2 == 0, 128 + 0] = 1.0
    Hh[r % 2 == 1, 128 + 1] = 1.0
    for m in range(16):
        Hh[2 * m:2 * m + 2, 256 + m] = 1.0
    return dict(cs_all=cs_all, E_all=E_all, Sh=Sh, Hh=Hh, ident=np.eye(128, dtype=f32))


_NC_CACHE = {}


def kernel(**inputs):
    x = np.asarray(inputs["x"], dtype=np.float32)
    sh = _shared_tables()
    base = {
        "w_in": np.ascontiguousarray(inputs["w_in"][0]),
        "w_out": np.ascontiguousarray(inputs["w_out"][0]),
        "w_gate": np.ascontiguousarray(inputs["w_gate"][0]),
        "w_up": np.ascontiguousarray(inputs["w_up"][0]),
        "w_down": np.ascontiguousarray(inputs["w_down"][0]),
        "conv_w": np.ascontiguousarray(inputs["conv_w"][0]),
        "cmp_pe_k": np.ascontiguousarray(inputs["cmp_pe_k"][0]),
        "cmp_w1_k": np.ascontiguousarray(inputs["cmp_w1_k"][0]),
        "cmp_w2_k": np.ascontiguousarray(inputs["cmp_w2_k"][0]),
        "cmp_pe_v": np.ascontiguousarray(inputs["cmp_pe_v"][0]),
        "cmp_w1_v": np.ascontiguousarray(inputs["cmp_w1_v"][0]),
        "cmp_w2_v": np.ascontiguousarray(inputs["cmp_w2_v"][0]),
        "norm_mix": np.ascontiguousarray(inputs["norm_mix"][0]),
        "norm_conv_out": np.ascontiguousarray(inputs["norm_conv_out"][0]),
        "norm_attn_out": np.ascontiguousarray(inputs["norm_attn_out"][0]),
        "norm_ffn": np.ascontiguousarray(inputs["norm_ffn"][0]),
        "norm_final": np.ascontiguousarray(inputs["norm_final"]),
    }
    base = {k: np.asarray(v, dtype=np.float32) for k, v in base.items()}
    base.update(sh)
    in_maps = []
    for c in range(8):
        b, j = c // 4, c % 4
        blocks = [4 * m + j for m in range(16)]
        xb_ = x[b]
        xown = np.concatenate([xb_[128 * qb:128 * qb + 128] for qb in blocks], axis=0)
        xhalo = np.zeros((32, D), np.float32)
        for m, qb in enumerate(blocks):
            if qb > 0:
                xhalo[2 * m:2 * m + 2] = xb_[128 * qb - 2:128 * qb]
        cs_own = np.concatenate([sh["cs_all"][128 * qb:128 * qb + 128] for qb in blocks], axis=0)
        im = dict(base)
        im.update(_tables(j))
        im.update(xall=np.ascontiguousarray(xb_[:KS]), xown=np.ascontiguousarray(xown), xhalo=xhalo,
                  cs_own=np.ascontiguousarray(cs_own))
        im["cs_all"] = np.ascontiguousarray(im["cs_all"][:KS])
        if SHRINK_KEEP is not None:
            im = {k: (v if k in SHRINK_KEEP else np.zeros((1, 1), np.float32)) for k, v in im.items()}
        in_maps.append(im)
    if "nc" not in _NC_CACHE:
        _NC_CACHE["nc"] = build_nc()
    nc = _NC_CACHE["nc"]
    res = run_bass_kernel_spmd(nc, in_maps[:KCORES], core_ids=list(range(KCORES)))
    out = np.zeros((2, S, D), np.float32)
    for c in range(KCORES):
        b, j = c // 4, c % 4
        yv = res.results[c]["y"]
        for m in range(16):
            qb = 4 * m + j
            out[b, 128 * qb:128 * qb + 128] = yv[128 * m:128 * m + 128]
    if DEBUG:
        kernel.last_results = res.results
    return out
```

```python
import os
import numpy as np
import concourse.bass as bass
import concourse.mybir as mybir
from concourse.bass_utils import run_bass_kernel_spmd
from contextlib import ExitStack

F32 = mybir.dt.float32
BF16 = mybir.dt.bfloat16
U8 = mybir.dt.uint8
AF = mybir.ActivationFunctionType
ALU = mybir.AluOpType
AX = mybir.AxisListType

D = 2048
S = 8192
DP = 5656
FF = 5632
NEG = -30000.0
SCALE = 128 ** -0.5
EPS = 1e-6
ENGS = ["tensor", "vector", "scalar", "gpsimd", "sync"]
DEBUG = bool(int(os.environ.get("KDEBUG", "0")))
KSTOP = os.environ.get("KSTOP", "")
KCORES = int(os.environ.get("KCORES", "8"))
KS = int(os.environ.get("KS", "8192"))
KSKIP = set(os.environ.get("KSKIP", "").split(","))
_UA = {"xall", "w_in", "norm_mix", "cs_all", "ident"}
_UA2 = _UA | {"cmp_pe_k", "cmp_w1_k", "cmp_w2_k", "cmp_pe_v", "cmp_w1_v", "cmp_w2_v"}
_U0 = _UA2 | {"xown", "xhalo", "conv_w", "norm_conv_out", "cs_own", "Sh", "Hh"}
_UB = _U0 | {"cb_sel", "cb_win", "cb_cmp", "cbT_cmp", "sel_force", "sel_future", "E_all", "norm_attn_out"}
_USED = {"A": _UA, "A2": _UA2, "0": _U0, "B": _UB}
SHRINK_KEEP = _USED.get(KSTOP)


class Op:
    __slots__ = ("eng", "fn", "deps", "needs_inc", "count", "grp", "is_nop")


class DmaGroup:
    __slots__ = ("stream", "n", "final", "last")


class Prog:
    def __init__(self):
        self.q = {e: [] for e in ENGS}
        self.keys = {}
        self.streams = {}

    def dma_group(self, stream):
        g = DmaGroup()
        g.stream = stream
        g.n = 0
        g.final = None
        g.last = None
        self.streams.setdefault(stream, []).append(g)
        return g

    def op(self, eng, fn, reads=(), writes=(), grp=None, extra=()):
        o = Op()
        o.eng = eng
        o.fn = fn
        o.needs_inc = False
        o.count = None
        o.grp = grp
        o.is_nop = False
        if grp is not None:
            grp.n += 1
            grp.last = o
        ident = eng if grp is None else ("dma", id(grp))
        deps = set(extra)
        writes = list(writes) + [k for k in reads if isinstance(k, tuple) and k[0] == "ps" and k not in writes]
        reads = [k for k in reads if not (isinstance(k, tuple) and k[0] == "ps")]
        for k in reads:
            st = self.keys.setdefault(k, ({}, {}))
            deps.update(st[0].values())
            st[1][ident] = o
        for k in writes:
            st = self.keys.setdefault(k, ({}, {}))
            deps.update(st[0].values())
            deps.update(st[1].values())
            if st[1]:
                st[0].clear()
                st[1].clear()
            st[0][ident] = o
        deps.discard(o)
        o.deps = deps
        self.q[eng].append(o)
        return o

    def barrier(self):
        arr = []
        for e in ENGS:
            for o in reversed(self.q[e]):
                if o.grp is None and not getattr(o, "is_nop", False):
                    arr.append(o)
                    break
        lasts = [g[-1].last for g in self.streams.values() if g and g[-1].last is not None]
        for e in ENGS:
            o = self.op(e, lambda eng: eng.nop(), extra=arr + lasts)
            o.is_nop = True
        self.keys = {}

    def emit(self, nc, ctx):
        engsem = {e: ctx.enter_context(nc.semaphore("es_" + e)) for e in ENGS}
        ssem = {}
        for s, groups in self.streams.items():
            ssem[s] = ctx.enter_context(nc.semaphore("ds_" + str(s)))
            cum = 0
            for g in groups:
                cum += 16 * g.n
                g.final = cum
        for e in ENGS:
            for o in self.q[e]:
                for d in o.deps:
                    if d.grp is None and not (d.eng == e and e == "tensor"):
                        d.needs_inc = True
        for e in ENGS:
            c = 0
            for o in self.q[e]:
                if o.grp is None and o.needs_inc:
                    c += 1
                    o.count = c
        block = ctx.enter_context(nc.Block())
        prog = self

        def run(e):
            def body(eng):
                waited = {}
                for o in prog.q[e]:
                    needs = {}
                    for d in o.deps:
                        if d.grp is not None:
                            s, v, key = ssem[d.grp.stream], d.grp.final, ("s", d.grp.stream)
                        else:
                            if d.eng == e and e == "tensor":
                                continue
                            s, v, key = engsem[d.eng], d.count, ("e", d.eng)
                        if key not in needs or needs[key][1] < v:
                            needs[key] = (s, v)
                    for key, (s, v) in needs.items():
                        if waited.get(key, 0) < v:
                            eng.wait_ge(s, v)
                            waited[key] = v
                    ins = o.fn(eng)
                    if o.grp is not None:
                        ins.then_inc(ssem[o.grp.stream], 16)
                    elif o.needs_inc:
                        ins.then_inc(engsem[e], 1)
            return body

        block.tensor(run("tensor"))
        block.vector(run("vector"))
        block.scalar(run("scalar"))
        block.gpsimd(run("gpsimd"))
        block.sync(run("sync"))


class Arena:
    def __init__(self, t, size):
        self.t = t
        self.size = size
        self.off = 0

    def reset(self):
        self.off = 0

    def alloc(self, shape, dt):
        esz = 4 if dt == F32 else (2 if dt == BF16 else 1)
        n = 1
        for s in shape[1:]:
            n *= s
        nb = (n * esz + 63) // 64 * 64
        assert self.off + nb <= self.size, ("arena overflow", self.off, nb, self.size)
        ap = self.t[0:shape[0], self.off:self.off + n * esz].bitcast(dt)
        self.off += nb
        if len(shape) == 3:
            ap = ap.rearrange("p (a b) -> p a b", a=shape[1])
        elif len(shape) == 4:
            ap = ap.rearrange("p (a b c) -> p a b c", a=shape[1], b=shape[2])
        elif len(shape) == 5:
            ap = ap.rearrange("p (a b c d) -> p a b c d", a=shape[1], b=shape[2], c=shape[3])
        return ap


def build_nc():
    nc = bass.Bass("TRN2", target_bir_lowering=False)

    def din(name, shape):
        if SHRINK_KEEP is not None and name not in SHRINK_KEEP:
            shape = [1, 1]
        return nc.dram_tensor(name, list(shape), F32, kind="ExternalInput").ap()

    xall = din("xall", [KS, D])
    xown = din("xown", [2048, D])
    xhalo = din("xhalo", [32, D])
    w_in = din("w_in", [D, DP])
    w_out = din("w_out", [D, D])
    w_gate = din("w_gate", [D, FF])
    w_up = din("w_up", [D, FF])
    w_down = din("w_down", [FF, D])
    conv_w = din("conv_w", [3, 1024])
    pe_k = din("cmp_pe_k", [32, 128])
    w1_k = din("cmp_w1_k", [4096, 128])
    w2_k = din("cmp_w2_k", [128, 128])
    pe_v = din("cmp_pe_v", [32, 128])
    w1_v = din("cmp_w1_v", [4096, 128])
    w2_v = din("cmp_w2_v", [128, 128])
    g_mix = din("norm_mix", [D])
    g_conv = din("norm_conv_out", [1024])
    g_attn = din("norm_attn_out", [1024])
    g_ffn = din("norm_ffn", [D])
    g_fin = din("norm_final", [D])
    cs_all = din("cs_all", [KS, 128])
    cs_own = din("cs_own", [2048, 128])
    cb_sel_d = din("cb_sel", [128, 4 * 128])
    cb_win_d = din("cb_win", [128, 8 * 128])
    cb_cmp_d = din("cb_cmp", [16, 128, 2 * 128])
    cbT_cmp_d = din("cbT_cmp", [16, 128, 64])
    self_force_d = din("sel_force", [128, 9])
    self_future_d = din("sel_future", [128, 9])
    E_all_d = din("E_all", [128, 64 * 128])
    Sh_d = din("Sh", [128, 2 * 128])
    Hh_d = din("Hh", [32, 272])
    ident_d = din("ident", [128, 128])
    y = nc.dram_tensor("y", [2048, D], F32, kind="ExternalOutput").ap()

    skind = dict(kind="ExternalOutput") if DEBUG else {}
    KTs = nc.dram_tensor("KTs", [4, 2, 128, KS], BF16, **skind).ap()
    VSc = nc.dram_tensor("VSc", [2, KS, 2, 130], BF16, **skind).ap()
    qTs = nc.dram_tensor("qTs", [16, 128, 8, 128], BF16, **skind).ap()
    ymixT = nc.dram_tensor("ymixT", [16, 128, 16, 128], BF16, **skind).ap()
    if DEBUG:
        dbg_kc = nc.dram_tensor("dbg_kc", [128, 2 * 512], BF16, kind="ExternalOutput").ap()
        dbg_vc = nc.dram_tensor("dbg_vc", [128, 4 * 2 * 130], BF16, kind="ExternalOutput").ap()
        dbg_gates = nc.dram_tensor("dbg_gates", [128, 16 * 24], F32, kind="ExternalOutput").ap()

    ctx = ExitStack()
    with ctx:
        P = Prog()
        AR_SIZE = 176 * 1024
        arena_t = ctx.enter_context(nc.sbuf_tensor("arena", [128, AR_SIZE], U8))
        AR = Arena(arena_t, AR_SIZE)

        def sbt(name, shape, dt):
            return ctx.enter_context(nc.sbuf_tensor(name, shape, dt))

        ps = ctx.enter_context(nc.psum_tensor("ps", [128, 8, 512], F32))

        def bank(i):
            return ps[:, i, :]

        def bank_bf(i, n=1):
            return ps[:, i:i + n, :].bitcast(BF16)

        identF = sbt("identF", [128, 128], F32)
        identB = sbt("identB", [128, 128], BF16)
        eps_t = sbt("eps_t", [128, 1], F32)
        kcT = sbt("kcT", [128, 2, 512], BF16)
        vcc = sbt("vcc", [128, 4, 2, 130], BF16)
        gates = sbt("gates", [128, 16, 24], F32)
        u_halo = sbt("u_halo", [32, 1024], F32)
        aT_halo = sbt("aT_halo", [128, 16, 32], BF16)
        junk = sbt("junk", [128, 2048], BF16)
        stat = sbt("stat", [128, 64], F32)

        def dma(queue, out, in_, reads, writes, stream, grp=None):
            g = grp if grp is not None else P.dma_group(stream)
            P.op(queue, lambda e: e.dma_start(out=out, in_=in_), reads, writes, grp=g)
            return g

        def mm(out, lhsT, rhs, start, stop, reads, writes, skip=False):
            P.op("tensor", lambda e: e.matmul(out, lhsT=lhsT, rhs=rhs, start=start, stop=stop,
                                              skip_group_check=skip), reads, writes)

        def tr(out, in_, ident, reads, writes):
            P.op("tensor", lambda e: e.transpose(out=out, in_=in_, identity=ident), reads, writes)

        def act(out, in_, func, reads, writes, **kw):
            P.op("scalar", lambda e: e.activation(out=out, in_=in_, func=func, **kw), reads, writes)

        def vtt(out, in0, in1, op, reads, writes):
            P.op("vector", lambda e: e.tensor_tensor(out=out, in0=in0, in1=in1, op=op), reads, writes)

        def vts(out, in0, s1, s2, op0, op1, reads, writes):
            if op1 is None:
                P.op("vector", lambda e: e.tensor_scalar(out=out, in0=in0, scalar1=s1, scalar2=None, op0=op0),
                     reads, writes)
            else:
                P.op("vector", lambda e: e.tensor_scalar(out=out, in0=in0, scalar1=s1, scalar2=s2, op0=op0, op1=op1),
                     reads, writes)

        def vstt(out, in0, scalar, in1, op0, op1, reads, writes):
            P.op("vector", lambda e: e.scalar_tensor_tensor(out=out, in0=in0, scalar=scalar, in1=in1,
                                                            op0=op0, op1=op1), reads, writes)

        def vcopy(out, in_, reads, writes):
            P.op("vector", lambda e: e.tensor_copy(out=out, in_=in_), reads, writes)

        def vrecip(out, in_, reads, writes):
            P.op("vector", lambda e: e.reciprocal(out=out, in_=in_), reads, writes)

        def vmemset(ap, val, writes):
            P.op("vector", lambda e: e.memset(ap, val), (), writes)

        def bc_load(dst, src_row, key, stream):
            dma("sync", dst, src_row.partition_broadcast(128), (), [key], stream)

        def rms_rstd(src, n, col, kin, np_=128):
            ss = stat[0:np_, col:col + 1]
            rt = stat[0:np_, col + 1:col + 2]
            rs = stat[0:np_, col + 2:col + 3]
            act(junk[0:np_, 0:n], src, AF.Square, [kin], ["junk", ("stat", col)], accum_out=ss)
            act(rt, ss, AF.Sqrt, [("stat", col)], [("stat", col + 1)], scale=1.0 / n, bias=eps_t[0:np_, :])
            vrecip(rs, rt, [("stat", col + 1)], [("stat", col + 2)])
            return rs, ("stat", col + 2)

        def norm_transpose(src, ksrc, gain_bc, kgain, xb, kxb, pbank, dst, kdst, col, np_=128):
            rs, krs = rms_rstd(src, 2048, col, ksrc, np_)
            vstt(xb[0:np_, :], src, rs, gain_bc[0:np_, :], ALU.mult, ALU.mult, [ksrc, krs, kgain], [kxb])
            pt = bank_bf(pbank, 2).rearrange("p a (c t) -> p (a c) t", t=128)
            idn = identB[0:np_, 0:np_]
            for k in range(16):
                tr(pt[:, k, 0:np_], xb[0:np_, k * 128:(k + 1) * 128], idn, [kxb, "identB"],
                   [("ps", pbank), ("ps", pbank + 1)])
            act(dst, pt[:, :, 0:np_], AF.Copy, [("ps", pbank), ("ps", pbank + 1)], [kdst])

        def load_w(dst, src_cols, key, stream):
            dma("gpsimd", dst, src_cols.rearrange("(kc p) n -> p kc n", p=128), (), [key], stream)

        dma("sync", identF[:], ident_d, (), ["identF"], "c_id")
        vcopy(identB[:], identF[:], ["identF"], ["identB"])
        vmemset(eps_t[:], EPS, ["eps"])
        P.barrier()
        for _once in (0,):

            AR.reset()
            wkv = AR.alloc([128, 16, 1536], BF16)
            gmix = AR.alloc([128, 2048], F32)
            xs = [AR.alloc([128, 2048], F32) for _ in range(2)]
            xb = AR.alloc([128, 2048], BF16)
            aT = [AR.alloc([128, 16, 128], BF16) for _ in range(2)]
            cs = [AR.alloc([128, 2, 64], F32) for _ in range(2)]
            rt1 = AR.alloc([128, 3, 2, 64], F32)
            rt2 = AR.alloc([128, 3, 2, 64], F32)
            krope = AR.alloc([128, 3, 2, 2, 64], BF16)
            vcb = AR.alloc([128, 2, 128], BF16)
            vst = [AR.alloc([128, 2, 2, 130], BF16) for _ in range(2)]
            kts = [AR.alloc([128, 8, 128], BF16) for _ in range(2)]

            for g in range(3):
                load_w(wkv[:, :, g * 512:(g + 1) * 512], w_in[:, 4096 + g * 512:4096 + (g + 1) * 512], ("wkv", g), f"wkv{g}")
            bc_load(gmix, g_mix, "gmix", "c_gmix")
            for s_ in range(2):
                if "ms" not in KSKIP:
                    vmemset(vst[s_][:, :, :, 128:130], 1.0, [("vst1", s_)])
            NB_A = int(os.environ.get("KNBA", "64"))
            dma("sync", xs[0], xall[0:128, :], (), [("xs", 0)], "xs0")
            if NB_A > 1:
                dma("sync", xs[1], xall[128:256, :], (), [("xs", 1)], "xs1")
            for i in range(NB_A):
                sl = i % 2
                dma("sync", cs[sl], cs_all[i * 128:(i + 1) * 128, :].rearrange("p (a b) -> p a b", a=2), (), [("cs", sl)], f"cs{sl}")
                if i == 0:
                    norm_transpose(xs[0], ("xs", 0), gmix, "gmix", xb, "xb", 0, aT[0], ("aT", 0), 0)
                for g in range(3 if "mm" not in KSKIP else 0):
                    for k in range(16):
                        mm(bank(2 + g), aT[sl][:, k, :], wkv[:, k, g * 512:(g + 1) * 512], k == 0, k == 15,
                           [("aT", sl), ("wkv", g)], [("ps", 2 + g)])
                if i + 1 < NB_A:
                    norm_transpose(xs[1 - sl], ("xs", 1 - sl), gmix, "gmix", xb, "xb", 6 if sl == 0 else 0,
                                   aT[1 - sl], ("aT", 1 - sl), 0)
                    if i + 2 < NB_A:
                        dma("sync", xs[sl], xall[(i + 2) * 128:(i + 3) * 128, :], (), [("xs", sl)], f"xs{sl}")
                if "rope" not in KSKIP:
                    z = ps[:, 2:5, 0:256].rearrange("p t (h f x) -> p t h f x", h=2, f=2)
                    cosb = cs[sl][:, 0, :].unsqueeze(1).unsqueeze(1).to_broadcast([128, 3, 2, 64])
                    sinb = cs[sl][:, 1, :].unsqueeze(1).unsqueeze(1).to_broadcast([128, 3, 2, 64])
                    pk = [("ps", 2), ("ps", 3), ("ps", 4)]
                    vtt(rt1, z[:, :, :, 0, :], cosb, ALU.mult, pk + [("cs", sl)], ["rt1"])
                    vtt(rt2, z[:, :, :, 1, :], sinb, ALU.mult, pk + [("cs", sl)], ["rt2"])
                    vtt(krope[:, :, :, 0, :], rt1, rt2, ALU.subtract, ["rt1", "rt2"], ["krope0"])
                    vtt(rt1, z[:, :, :, 1, :], cosb, ALU.mult, pk + [("cs", sl)], ["rt1"])
                    vtt(rt2, z[:, :, :, 0, :], sinb, ALU.mult, pk + [("cs", sl)], ["rt2"])
                    vtt(krope[:, :, :, 1, :], rt1, rt2, ALU.add, ["rt1", "rt2"], ["krope1"])
                if "vcopy" not in KSKIP:
                    act(vcb, ps[:, 2, 256:512].rearrange("p (h x) -> p h x", h=2), AF.Copy, [("ps", 2)], ["vcb"])
                    for t_ in range(2):
                        act(vst[sl][:, t_, :, 0:128], ps[:, 3 + t_, 256:512].rearrange("p (h x) -> p h x", h=2), AF.Copy,
                            [("ps", 3 + t_)], [("vst", sl)])
                if "tr5" not in KSKIP:
                    pk5 = bank_bf(5).rearrange("p a (c t) -> p (a c) t", t=128)
                    for t in range(3):
                        for hk in range(2):
                            tr(pk5[:, t * 2 + hk, :], krope[:, t, hk, :, :].rearrange("p f x -> p (f x)"), identB[:],
                               ["krope0", "krope1", "identB"], [("ps", 5)])
                    for hk in range(2):
                        tr(pk5[:, 6 + hk, :], vcb[:, hk, :], identB[:], ["vcb", "identB"], [("ps", 5)])
                    vcopy(kts[sl], pk5, [("ps", 5)], [("kts", sl)])
                if "st" not in KSKIP:
                    dma("sync", KTs[:, :, :, i * 128:(i + 1) * 128].rearrange("t h d s -> d (t h) s"), kts[sl],
                        [("kts", sl)], [("KTs", i)], f"kst{sl}")
                    dma("sync", VSc[:, i * 128:(i + 1) * 128, :, :].rearrange("t s h c -> s t (h c)"),
                        vst[sl].rearrange("p t h c -> p t (h c)"), [("vst", sl), ("vst1", sl)], [("VSc", i)], f"vst{sl}")
            P.barrier()
            if KSTOP == "A":
                break

            AR.reset()
            w1b = [AR.alloc([128, 32, 128], BF16) for _ in range(2)]
            w2b = [AR.alloc([128, 128], BF16) for _ in range(2)]
            pe_f = [AR.alloc([32, 128], F32) for _ in range(2)]
            peT = [AR.alloc([128, 32], BF16) for _ in range(2)]
            c0 = [AR.alloc([128, 1], F32) for _ in range(2)]
            raw = [AR.alloc([128, S], BF16) for _ in range(2)]
            h1T = AR.alloc([128, 512], BF16)
            for ti, (w1d, w2d, ped) in enumerate([(w1_k, w2_k, pe_k), (w1_v, w2_v, pe_v)]):
                dma("gpsimd", w1b[ti], w1d.rearrange("(l d) e -> d l e", d=128), (), [("w1b", ti)], f"w1b{ti}")
                dma("gpsimd", w2b[ti], w2d, (), [("w2b", ti)], f"w2b{ti}")
                dma("sync", pe_f[ti], ped, (), [("pe_f", ti)], f"pef{ti}")
                tr(ps[:, 0, 0:32], pe_f[ti], identF[0:32, 0:32], [("pe_f", ti), "identF"], [("ps", 0)])
                vcopy(peT[ti], ps[:, 0, 0:32], [("ps", 0)], [("peT", ti)])
                for l in range(32):
                    mm(ps[:, 1, 0:1], w1b[ti][:, l, :], peT[ti][:, l:l + 1], l == 0, l == 31,
                       [("w1b", ti), ("peT", ti)], [("ps", 1)])
                vcopy(c0[ti], ps[:, 1, 0:1], [("ps", 1)], [("c0", ti)])
            vmemset(vcc[:, :, :, 128:130], 1.0, ["vcc1"])
            it = 0
            for ti in range(2):
                for hk in range(2):
                    sl = it % 2
                    it += 1
                    dma("sync", raw[sl], KTs[0 if ti == 0 else 3, hk, :, :], (), [("raw", sl)], f"raw{sl}")
                    r16 = raw[sl].rearrange("p (n s) -> p n s", s=16)
                    pb = 2 + sl
                    for l in range(32):
                        rhs = r16[:, 0:511, l] if l < 16 else r16[:, 1:512, l - 16]
                        mm(ps[:, pb, 0:511], w1b[ti][:, l, :], rhs, l == 0, l == 31, [("w1b", ti), ("raw", sl)], [("ps", pb)])
                    vmemset(h1T[:, 511:512], 0.0, ["h1T"])
                    act(h1T[:, 0:511], ps[:, pb, 0:511], AF.Silu, [("ps", pb), ("c0", ti)], ["h1T"], bias=c0[ti])
                    if ti == 0:
                        mm(bank(4), w2b[0], h1T, True, True, [("w2b", 0), "h1T"], [("ps", 4)])
                        vcopy(kcT[:, hk, :], bank(4), [("ps", 4)], ["kcT"])
                    else:
                        for nb in range(4):
                            mm(ps[:, 5, nb * 128:(nb + 1) * 128], h1T[:, nb * 128:(nb + 1) * 128], w2b[1], True, True,
                               [("w2b", 1), "h1T"], [("ps", 5)])
                        vcopy(vcc[:, :, hk, 0:128], ps[:, 5, :].rearrange("p (n x) -> p n x", n=4), [("ps", 5)], ["vcc"])
            if DEBUG:
                dma("sync", dbg_kc, kcT[:].rearrange("p a b -> p (a b)"), ["kcT"], ["dbg_kc"], "dbg1")
                dma("sync", dbg_vc, vcc[:].rearrange("p a b c -> p (a b c)"), ["vcc", "vcc1"], ["dbg_vc"], "dbg2")
            P.barrier()
            if KSTOP == "A2":
                break

            AR.reset()
            gmix = AR.alloc([128, 2048], F32)
            gconv = AR.alloc([128, 1024], F32)
            wcv = AR.alloc([128, 3, 1024], F32)
            shf = AR.alloc([128, 2, 128], F32)
            hfix = AR.alloc([32, 2, 128], F32)
            hmask = AR.alloc([32, 16], F32)
            uh = [AR.alloc([32, 256], F32) for _ in range(2)]
            xs = [AR.alloc([128, 2048], F32) for _ in range(2)]
            xb = AR.alloc([128, 2048], BF16)
            aTo = AR.alloc([128, 16, 1024], BF16)
            wch = [AR.alloc([128, 16, 256], BF16) for _ in range(4)]
            wgt = AR.alloc([128, 16, 24], BF16)
            ycb = AR.alloc([128, 8, 1024], BF16)
            cso = AR.alloc([128, 8, 2, 64], F32)
            ccs = [AR.alloc([128, 256], F32) for _ in range(2)]
            uu = [AR.alloc([128, 256], F32) for _ in range(2)]
            aa = [AR.alloc([128, 256], F32) for _ in range(2)]
            tb_ = [AR.alloc([128, 256], F32) for _ in range(2)]
            qr1 = AR.alloc([128, 2, 64], F32)
            qr2 = AR.alloc([128, 2, 64], F32)
            qrope = [AR.alloc([128, 2, 2, 64], BF16) for _ in range(2)]
            qts = [AR.alloc([128, 2, 128], BF16) for _ in range(2)]
            ycn = AR.alloc([128, 1024], BF16)
            ymT = [AR.alloc([128, 8, 128], BF16) for _ in range(2)]
            ssq = AR.alloc([128, 8, 4], F32)

            bc_load(gmix, g_mix, "gmix", "c_gmix")
            bc_load(gconv, g_conv, "gconv", "c_gconv")
            for kk in range(3):
                bc_load(wcv[:, kk, :], conv_w[kk, :], ("wcv", kk), f"c_wcv{kk}")
            dma("sync", shf, Sh_d.rearrange("p (a b) -> p a b", a=2), (), ["shf"], "c_sh")
            dma("sync", hfix, Hh_d[:, 0:256].rearrange("p (a b) -> p a b", a=2), (), ["hfix"], "c_hh")
            dma("sync", hmask, Hh_d[:, 256:272], (), ["hmask"], "c_hm")
            wcnt = [0]

            def next_w(src_cols, ncols=256):
                i = wcnt[0] % 4
                wcnt[0] += 1
                load_w(wch[i][:, :, 0:ncols], src_cols, ("wch", i), f"wch{i}")
                return wch[i], ("wch", i)

            for hf in range(2):
                if hf == 0:
                    dma("sync", xs[1][0:32, :], xhalo, (), [("xs", 1)], "xs1")
                    norm_transpose(xs[1][0:32, :], ("xs", 1), gmix, "gmix", xb, "xb", 0, aT_halo[:], "aT_halo", 0, np_=32)
                for mi in range(8):
                    m = hf * 8 + mi
                    sl = mi % 2
                    dma("sync", xs[sl], xown[m * 128:(m + 1) * 128, :], (), [("xs", sl)], f"xs{sl}")
                    norm_transpose(xs[sl], ("xs", sl), gmix, "gmix", xb, "xb", 0 if sl == 0 else 6,
                                   aTo[:, :, mi * 128:(mi + 1) * 128], ("aTo", mi), 0)
                dma("sync", cso, cs_own[hf * 1024:(hf + 1) * 1024, :].rearrange("(m p) (a b) -> p m a b", p=128, a=2),
                    (), ["cso"], "cso")
                for cg in range(4):
                    c0_, c1_ = cg * 256, (cg + 1) * 256
                    Wb, kWb = next_w(w_in[:, c0_:c1_])
                    Wc, kWc = next_w(w_in[:, 1024 + c0_:1024 + c1_])
                    Wh, kWh = next_w(w_in[:, 2048 + c0_:2048 + c1_])
                    if hf == 0:
                        for k in range(16):
                            mm(ps[0:32, 2, 0:256], aT_halo[:, k, :], Wc[:, k, :], k == 0, k == 15, ["aT_halo", kWc], [("ps", 2)])
                        for k in range(16):
                            mm(ps[0:32, 2, 256:512], aT_halo[:, k, :], Wh[:, k, :], k == 0, k == 15, ["aT_halo", kWh], [("ps", 2)])
                        act(ccs[0][0:32, :], ps[0:32, 2, 0:256], AF.Copy, [("ps", 2)], [("ccs", 0)])
                        vtt(u_halo[:, c0_:c1_], ps[0:32, 2, 256:512], ccs[0][0:32, :], ALU.mult, [("ps", 2), ("ccs", 0)], ["u_halo"])
                    for mi in range(8):
                        m = hf * 8 + mi
                        sl = mi % 2
                        bA, bB, bC = (2, 3, 4) if sl == 0 else (5, 6, 7)
                        at = aTo[:, :, mi * 128:(mi + 1) * 128]
                        for k in range(16):
                            mm(ps[:, bA, 0:256], at[:, k, :], Wc[:, k, :], k == 0, k == 15, [("aTo", mi), kWc], [("ps", bA)])
                        for k in range(16):
                            mm(ps[:, bA, 256:512], at[:, k, :], Wh[:, k, :], k == 0, k == 15, [("aTo", mi), kWh], [("ps", bA)])
                        for k in range(16):
                            mm(ps[:, bB, 0:256], at[:, k, :], Wb[:, k, :], k == 0, k == 15, [("aTo", mi), kWb], [("ps", bB)])
                        act(ccs[sl], ps[:, bA, 0:256], AF.Copy, [("ps", bA)], [("ccs", sl)])
                        vtt(uu[sl], ps[:, bA, 256:512], ccs[sl], ALU.mult, [("ps", bA), ("ccs", sl)], [("uu", sl)])
                        vts(uh[sl], u_halo[:, c0_:c1_], hmask[:, m:m + 1], None, ALU.mult, None, ["u_halo", "hmask"], [("uh", sl)])
                        mm(ps[:, bC, 0:256], shf[:, 0, :], uu[sl], True, False, ["shf", ("uu", sl)], [("ps", bC)])
                        mm(ps[:, bC, 0:256], hfix[:, 0, :], uh[sl], False, True, ["hfix", ("uh", sl)], [("ps", bC)])
                        mm(ps[:, bC, 256:512], shf[:, 1, :], uu[sl], True, False, ["shf", ("uu", sl)], [("ps", bC)])
                        mm(ps[:, bC, 256:512], hfix[:, 1, :], uh[sl], False, True, ["hfix", ("uh", sl)], [("ps", bC)])
                        vtt(aa[sl], uu[sl], wcv[:, 2, c0_:c1_], ALU.mult, [("uu", sl), ("wcv", 2)], [("aa", sl)])
                        vtt(tb_[sl], ps[:, bC, 0:256], wcv[:, 1, c0_:c1_], ALU.mult, [("ps", bC), ("wcv", 1)], [("tb", sl)])
                        vtt(aa[sl], aa[sl], tb_[sl], ALU.add, [("aa", sl), ("tb", sl)], [("aa", sl)])
                        vtt(tb_[sl], ps[:, bC, 256:512], wcv[:, 0, c0_:c1_], ALU.mult, [("ps", bC), ("wcv", 0)], [("tb", sl)])
                        vtt(aa[sl], aa[sl], tb_[sl], ALU.add, [("aa", sl), ("tb", sl)], [("aa", sl)])
                        vtt(ycb[:, mi, c0_:c1_], ps[:, bB, 0:256], aa[sl], ALU.mult, [("ps", bB), ("aa", sl)], [("ycb", mi, cg)])
                        act(junk[:, 0:256], ycb[:, mi, c0_:c1_], AF.Square, [("ycb", mi, cg)], ["junk", ("ssq", mi, cg)],
                            accum_out=ssq[:, mi, cg:cg + 1])
                for qc in range(4):
                    Wq, kWq = next_w(w_in[:, 3072 + qc * 256:3072 + (qc + 1) * 256])
                    for mi in range(8):
                        m = hf * 8 + mi
                        sl = mi % 2
                        bq = 2 if sl == 0 else 5
                        at = aTo[:, :, mi * 128:(mi + 1) * 128]
                        for k in range(16):
                            mm(ps[:, bq, 0:256], at[:, k, :], Wq[:, k, :], k == 0, k == 15, [("aTo", mi), kWq], [("ps", bq)])
                        z = ps[:, bq, 0:256].rearrange("p (h f x) -> p h f x", h=2, f=2)
                        cosb = cso[:, mi, 0, :].unsqueeze(1).to_broadcast([128, 2, 64])
                        sinb = cso[:, mi, 1, :].unsqueeze(1).to_broadcast([128, 2, 64])
                        kz = [("ps", bq), "cso"]
                        vtt(qr1, z[:, :, 0, :], cosb, ALU.mult, kz, ["qr1"])
                        vtt(qr2, z[:, :, 1, :], sinb, ALU.mult, kz, ["qr2"])
                        vtt(qrope[sl][:, :, 0, :], qr1, qr2, ALU.subtract, ["qr1", "qr2"], [("qrope0", sl)])
                        vtt(qr1, z[:, :, 1, :], cosb, ALU.mult, kz, ["qr1"])
                        vtt(qr2, z[:, :, 0, :], sinb, ALU.mult, kz, ["qr2"])
                        vtt(qrope[sl][:, :, 1, :], qr1, qr2, ALU.add, ["qr1", "qr2"], [("qrope1", sl)])
                        bt = 3 if sl == 0 else 6
                        ptq = bank_bf(bt)[:, 0, 0:256].rearrange("p (h t) -> p h t", h=2)
                        for hh in range(2):
                            tr(ptq[:, hh, :], qrope[sl][:, hh, :, :].rearrange("p f x -> p (f x)"), identB[:],
                               [("qrope0", sl), ("qrope1", sl), "identB"], [("ps", bt)])
                        act(qts[sl], ptq, AF.Copy, [("ps", bt)], [("qts", sl)])
                        dma("sync", qTs[m, :, qc * 2:qc * 2 + 2, :], qts[sl], [("qts", sl)], [("qTs", m, qc)], f"qts{sl}")
                load_w(wgt, w_in[:, 5632:5656], "wgt", "wgt")
                for mi in range(8):
                    m = hf * 8 + mi
                    bq = 4 if mi % 2 == 0 else 7
                    at = aTo[:, :, mi * 128:(mi + 1) * 128]
                    for k in range(16):
                        mm(ps[:, bq, 0:24], at[:, k, :], wgt[:, k, :], k == 0, k == 15, [("aTo", mi), "wgt"], [("ps", bq)])
                    act(gates[:, m, :], ps[:, bq, 0:24], AF.Sigmoid, [("ps", bq)], [("gates", m)])
                for mi in range(8):
                    m = hf * 8 + mi
                    sl = mi % 2
                    P.op("vector", lambda e, mi=mi: e.tensor_reduce(out=stat[:, 8:9], in_=ssq[:, mi, :], axis=AX.X, op=ALU.add),
                         [("ssq", mi, c) for c in range(4)], [("stat", 8)])
                    act(stat[:, 9:10], stat[:, 8:9], AF.Sqrt, [("stat", 8)], [("stat", 9)], scale=1.0 / 1024, bias=eps_t[:])
                    vrecip(stat[:, 10:11], stat[:, 9:10], [("stat", 9)], [("stat", 10)])
                    vstt(ycn, ycb[:, mi, :], stat[:, 10:11], gconv, ALU.mult, ALU.mult,
                         [("ycb", mi, c) for c in range(4)] + [("stat", 10), "gconv"], ["ycn"])
                    bt = 3 if sl == 0 else 6
                    pty = bank_bf(bt).rearrange("p a (c t) -> p (a c) t", t=128)
                    for c in range(8):
                        tr(pty[:, c, :], ycn[:, c * 128:(c + 1) * 128], identB[:], ["ycn", "identB"], [("ps", bt)])
                    act(ymT[sl], pty, AF.Copy, [("ps", bt)], [("ymT", sl)])
                    dma("sync", ymixT[m, :, 0:8, :], ymT[sl], [("ymT", sl)], [("ymixT", m, 0)], f"ymT{sl}")
            if DEBUG:
                dma("sync", dbg_gates, gates[:].rearrange("p a b -> p (a b)"), [("gates", m) for m in range(16)], ["dbg_g"], "dbg3")
            P.barrier()
            if KSTOP == "0":
                break

            AR.reset()
            ksT = AR.alloc([128, 2, S], BF16)
            vsA = AR.alloc([128, 64, 2, 130], BF16)
            Eall = AR.alloc([128, 64, 128], BF16)
            cbs = AR.alloc([128, 4, 4, 128], BF16)
            cbw = AR.alloc([128, 8, 4, 128], BF16)
            cbs_f = AR.alloc([128, 8, 128], F32)
            gattn = AR.alloc([128, 1024], F32)
            sfz = AR.alloc([128, 9], F32)
            suz = AR.alloc([128, 9], F32)
            qT = [AR.alloc([128, 8, 128], BF16) for _ in range(2)]
            kwT = [AR.alloc([128, 2, 1024], BF16) for _ in range(2)]
            vwA = [AR.alloc([128, 8, 2, 130], BF16) for _ in range(2)]
            cbc_f = [AR.alloc([128, 2, 128], F32) for _ in range(2)]
            cbc = [AR.alloc([128, 2, 4, 128], BF16) for _ in range(2)]
            cbT = [AR.alloc([128, 64], F32) for _ in range(2)]
            ee = [AR.alloc([128, 512], F32) for _ in range(2)]
            psum_ = AR.alloc([128, 512], F32)
            imp = AR.alloc([128, 128], F32)
            imp2 = AR.alloc([128, 128], F32)
            m8 = AR.alloc([128, 16], F32)
            selb = AR.alloc([128, 128], F32)
            sbT = [AR.alloc([128, 4, 128], BF16) for _ in range(2)]
            pT = [AR.alloc([128, 512], BF16) for _ in range(3)]
            yat = AR.alloc([128, 8, 128], F32)
            ytmp = AR.alloc([128, 8, 128], F32)
            coef = AR.alloc([128, 8], F32)
            dden = AR.alloc([128, 8], F32)
            yan = AR.alloc([128, 1024], BF16)
            ymT2 = [AR.alloc([128, 8, 128], BF16) for _ in range(2)]
            Dh = AR.alloc([128, 8], F32)

            for hk in range(2):
                dma("sync", ksT[:, hk, :], KTs[1, hk, :, :], (), [("ksT", hk)], f"ksT{hk}")
            for qd in range(4):
                dma("sync", vsA[:, qd * 16:(qd + 1) * 16, :, :].rearrange("p b h c -> p b (h c)"),
                    VSc[0, qd * 2048:(qd + 1) * 2048, :, :].rearrange("(b p) h c -> p b (h c)", p=128), (), [("vsA", qd)], f"vsA{qd}")
            for qd in range(8):
                dma("gpsimd", Eall[:, qd * 8:(qd + 1) * 8, :], E_all_d[:, qd * 1024:(qd + 1) * 1024].rearrange("p (a b) -> p a b", a=8),
                    (), [("Eall", qd // 2)], f"Eall{qd}")
            dma("sync", cbs_f[:, 0:4, :], cb_sel_d.rearrange("p (a b) -> p a b", a=4), (), ["cbs_f"], "c_cbs")
            vcopy(cbs, cbs_f[:, 0:4, :].unsqueeze(2).to_broadcast([128, 4, 4, 128]), ["cbs_f"], ["cbs"])
            dma("sync", cbs_f, cb_win_d.rearrange("p (a b) -> p a b", a=8), ["cbs_f"], ["cbs_f"], "c_cbs")
            vcopy(cbw, cbs_f.unsqueeze(2).to_broadcast([128, 8, 4, 128]), ["cbs_f"], ["cbw"])
            bc_load(gattn, g_attn, "gattn", "c_gattn")
            dma("sync", sfz, self_force_d, (), ["sfz"], "c_sfz")
            dma("sync", suz, self_future_d, (), ["suz"], "c_suz")
            vmemset(imp, -1.0, ["imp"])

            sctr = [0]
            pctr = [0]
            S_BANKS = [0, 1]
            O_BANK0 = 2

            def o_view(h):
                return ps[:, O_BANK0 + h // 2, (h % 2) * 256:(h % 2) * 256 + 129]

            def attend(m, hk, visits, o_started):
                qsl = m % 2
                qrhs = qT[qsl][:, 4 * hk:4 * hk + 4, :].rearrange("p g q -> p (g q)")
                def scores(vi):
                    kT_ap, kkeys, v_ap, vkeys, biases = visits[vi]
                    sb_ = S_BANKS[sctr[0] % 2]
                    sctr[0] += 1
                    nb = len(biases)
                    mm(bank(sb_), kT_ap, qrhs, True, nb == 0, kkeys + [("qT", qsl)], [("ps", sb_)])
                    for bi, (bl, br, bk) in enumerate(biases):
                        mm(bank(sb_), bl, br, False, bi == nb - 1, bk, [("ps", sb_)])
                    return sb_

                cur = scores(0)
                for vi in range(len(visits)):
                    kT_ap, kkeys, v_ap, vkeys, biases = visits[vi]
                    last = vi == len(visits) - 1
                    nxt = scores(vi + 1) if not last else None
                    sb_ = cur
                    pi = pctr[0] % 3
                    pctr[0] += 1
                    act(pT[pi], bank(sb_), AF.Exp, [("ps", sb_)], [("pT", pi)], scale=SCALE)
                    for g_ in range(4):
                        h = 4 * hk + g_
                        bnk = O_BANK0 + h // 2
                        st = bnk not in o_started
                        o_started.add(bnk)
                        mm(o_view(h), pT[pi][:, g_ * 128:(g_ + 1) * 128], v_ap, st, last, [("pT", pi)] + vkeys,
                           [("ps", bnk)], skip=True)
                    cur = nxt

            def finalize(m, br, first):
                ov = ps[:, O_BANK0:O_BANK0 + 4, :].rearrange("p b (h x) -> p (b h) x", h=2)
                okeys = [("ps", O_BANK0 + b_) for b_ in range(4)]
                vts(dden, ov[:, :, 128], 1e-30, None, ALU.max, None, okeys, ["dden"])
                vrecip(dden, dden, ["dden"], ["dden"])
                gv = gates[:, m, :].rearrange("p (h b) -> p h b", b=3)[:, :, br]
                vtt(coef, dden, gv, ALU.mult, ["dden", ("gates", m)], ["coef"])
                cb_ = coef[:, :].unsqueeze(2).to_broadcast([128, 8, 128])
                if first:
                    vtt(yat, ov[:, :, 0:128], cb_, ALU.mult, okeys + ["coef"], ["yat"])
                else:
                    vtt(ytmp, ov[:, :, 0:128], cb_, ALU.mult, okeys + ["coef"], ["ytmp"])
                    vtt(yat, yat, ytmp, ALU.add, ["yat", "ytmp"], ["yat"])

            for m in range(16):
                qsl = m % 2
                dma("sync", qT[qsl], qTs[m], (), [("qT", qsl)], f"qT{qsl}")
                nwin0 = 4 if m == 0 else 0
                t0 = 512 * (m - 1)
                for hk in range(2):
                    if m == 0:
                        dma("sync", kwT[qsl][:, hk, 512:1024], KTs[2, hk, :, 0:512], (), [("kwT", qsl)], f"kwT{qsl}")
                    else:
                        dma("sync", kwT[qsl][:, hk, :], KTs[2, hk, :, t0:t0 + 1024], (), [("kwT", qsl)], f"kwT{qsl}")
                if m == 0:
                    dma("sync", vwA[qsl][:, 4:8, :, :].rearrange("p b h c -> p b (h c)"),
                        VSc[1, 0:512, :, :].rearrange("(b p) h c -> p b (h c)", p=128), (), [("vwA", qsl)], f"vwA{qsl}")
                else:
                    dma("sync", vwA[qsl].rearrange("p b h c -> p b (h c)"),
                        VSc[1, t0:t0 + 1024, :, :].rearrange("(b p) h c -> p b (h c)", p=128), (), [("vwA", qsl)], f"vwA{qsl}")
                dma("sync", cbc_f[qsl], cb_cmp_d[m].rearrange("p (a b) -> p a b", a=2), (), [("cbc_f", qsl)], f"cbcf{qsl}")
                vcopy(cbc[qsl], cbc_f[qsl].unsqueeze(2).to_broadcast([128, 2, 4, 128]), [("cbc_f", qsl)], [("cbc", qsl)])
                dma("sync", cbT[qsl], cbT_cmp_d[m], (), [("cbT", qsl)], f"cbT{qsl}")
                Nm = 32 * (m + 1)
                ns = 8 * (m + 1)
                NBc = m // 4 + 1
                zlo = max(0, 32 * m - 32)
                zc0 = zlo - (32 * m - 32)
                for hk in range(2):
                    for g_ in range(4):
                        h = 4 * hk + g_
                        tbk = 6 + (h % 2)
                        esl = h % 2
                        mm(ps[:, tbk, 0:Nm], qT[qsl][:, h, :], kcT[:, hk, 0:Nm], True, True, [("qT", qsl), "kcT"], [("ps", tbk)])
                        vtt(ps[:, tbk, zlo:Nm], ps[:, tbk, zlo:Nm], cbT[qsl][:, zc0:64], ALU.add,
                            [("ps", tbk), ("cbT", qsl)], [("ps", tbk)])
                        act(ee[esl][:, 0:Nm], ps[:, tbk, 0:Nm], AF.Exp, [("ps", tbk)], [("ee", esl), ("Dh", h)],
                            scale=SCALE, accum_out=Dh[:, h:h + 1])
                        vts(Dh[:, h:h + 1], Dh[:, h:h + 1], 1e-30, None, ALU.max, None, [("Dh", h)], [("Dh", h)])
                        vrecip(Dh[:, h:h + 1], Dh[:, h:h + 1], [("Dh", h)], [("Dh", h)])
                        if g_ == 0:
                            vts(psum_[:, 0:Nm], ee[esl][:, 0:Nm], Dh[:, h:h + 1], None, ALU.mult, None,
                                [("ee", esl), ("Dh", h)], ["psum"])
                        else:
                            vstt(psum_[:, 0:Nm], ee[esl][:, 0:Nm], Dh[:, h:h + 1], psum_[:, 0:Nm], ALU.mult, ALU.add,
                                 [("ee", esl), ("Dh", h), "psum"], ["psum"])
                    P.op("vector", lambda e, Nm=Nm, ns=ns: e.tensor_reduce(
                        out=imp[:, 0:ns], in_=psum_[:, 0:Nm].rearrange("p (s f) -> p s f", f=4), axis=AX.X, op=ALU.add),
                        ["psum"], ["imp"])
                    vtt(imp[:, 1:ns], imp[:, 1:ns], psum_[:, 0:Nm].rearrange("p (s f) -> p s f", f=4)[:, 0:ns - 1, 3], ALU.add,
                        ["imp", "psum"], ["imp"])
                    vmemset(imp[:, 0:1], 10.0, ["imp"])
                    if m == 0:
                        zs, zt = imp[:, 0:8], slice(1, 9)
                    else:
                        zs, zt = imp[:, 8 * m - 1:8 * m + 8], slice(0, 9)
                    vtt(zs, zs, sfz[:, zt], ALU.max, ["imp", "sfz"], ["imp"])
                    vtt(zs, zs, suz[:, zt], ALU.min, ["imp", "suz"], ["imp"])
                    P.op("vector", lambda e: e.max(out=m8[:, 0:8], in_=imp[:, :]), ["imp"], ["m8a"])
                    P.op("vector", lambda e: e.match_replace(out=imp2[:, :], in_to_replace=m8[:, 0:8], in_values=imp[:, :],
                                                             imm_value=-2.0), ["imp", "m8a"], ["imp2"])
                    P.op("vector", lambda e: e.max(out=m8[:, 8:16], in_=imp2[:, :]), ["imp2"], ["m8b"])
                    vts(selb, imp, m8[:, 15:16], 1.0, ALU.is_ge, ALU.subtract, ["imp", "m8b"], ["selb"])
                    tr(ps[:, 6, 0:128], selb, identF[:], ["selb", "identF"], [("ps", 6)])
                    act(sbT[hk], ps[:, 6, 0:128].unsqueeze(1).to_broadcast([128, 4, 128]), AF.Copy, [("ps", 6)], [("sbT", hk)],
                        scale=-NEG)
                idB = identB[:]
                o_started = set()
                for hk in range(2):
                    visits = []
                    for nb in range(NBc):
                        biases = []
                        w_ = nb - (NBc - 2)
                        if w_ >= 0:
                            biases.append((idB, cbc[qsl][:, w_, :, :].rearrange("p g q -> p (g q)"), ["identB", ("cbc", qsl)]))
                        visits.append((kcT[:, hk, nb * 128:(nb + 1) * 128], ["kcT"], vcc[:, nb, hk, 0:129], ["vcc", "vcc1"], biases))
                    attend(m, hk, visits, o_started)
                finalize(m, 0, True)
                o_started = set()
                for hk in range(2):
                    visits = []
                    for jj in range(nwin0, 8):
                        biases = [(idB, cbw[:, jj, :, :].rearrange("p g q -> p (g q)"), ["identB", "cbw"])]
                        visits.append((kwT[qsl][:, hk, jj * 128:(jj + 1) * 128], [("kwT", qsl)], vwA[qsl][:, jj, hk, 0:129],
                                       [("vwA", qsl)], biases))
                    attend(m, hk, visits, o_started)
                finalize(m, 2, False)
                o_started = set()
                for hk in range(2):
                    visits = []
                    for kb in range(4 * m + 4):
                        biases = [(Eall[:, kb, :], sbT[hk].rearrange("p g q -> p (g q)"), [("Eall", kb // 16), ("sbT", hk)])]
                        if kb >= 4 * m:
                            biases.append((idB, cbs[:, kb - 4 * m, :, :].rearrange("p g q -> p (g q)"), ["identB", "cbs"]))
                        visits.append((ksT[:, hk, kb * 128:(kb + 1) * 128], [("ksT", hk)], vsA[:, kb, hk, 0:129],
                                       [("vsA", kb // 16)], biases))
                    attend(m, hk, visits, o_started)
                finalize(m, 1, False)
                yflat = yat.rearrange("p h x -> p (h x)")
                rs, krs = rms_rstd(yflat, 1024, 16, "yat")
                vstt(yan, yflat, rs, gattn, ALU.mult, ALU.mult, ["yat", krs, "gattn"], ["yan"])
                pty = bank_bf(7).rearrange("p a (c t) -> p (a c) t", t=128)
                for c in range(8):
                    tr(pty[:, c, :], yan[:, c * 128:(c + 1) * 128], identB[:], ["yan", "identB"], [("ps", 7)])
                act(ymT2[qsl], pty, AF.Copy, [("ps", 7)], [("ymT2", qsl)])
                dma("sync", ymixT[m, :, 8:16, :], ymT2[qsl], [("ymT2", qsl)], [("ymixT", m, 1)], f"ymT2{qsl}")
            P.barrier()
            if KSTOP == "B":
                break

            AR.reset()
            gbuf = AR.alloc([128, 2048], F32)
            ymx = [AR.alloc([128, 16, 128], BF16) for _ in range(2)]
            hh_ = AR.alloc([128, 4, 2048], F32)
            xb = AR.alloc([128, 2048], BF16)
            fT = AR.alloc([128, 16, 512], BF16)
            actT = AR.alloc([128, 44, 512], BF16)
            wst = [AR.alloc([128, 16, 256], BF16) for _ in range(3)]
            stg = [AR.alloc([128, 2816], F32) for _ in range(2)]
            wdn = [AR.alloc([128, 11, 256], BF16) for _ in range(2)]
            sg = [AR.alloc([128, 512], F32) for _ in range(2)]
            wc2 = [0]

            cst = [0]

            def load_cast(dst, src, kdst, a, n):
                i = cst[0] % 2
                cst[0] += 1
                st = stg[i][:, 0:a * n].rearrange("p (a n) -> p a n", a=a)
                dma("sync", st, src, (), [("stg", i)], f"stg{i}")
                if i == 0:
                    vcopy(dst, st, [("stg", i)], [kdst])
                else:
                    act(dst, st, AF.Copy, [("stg", i)], [kdst])

            def next_w2(src_cols):
                i = wc2[0] % 3
                wc2[0] += 1
                sv = src_cols.rearrange("(kc p) n -> p kc n", p=128)
                for hf_ in range(2):
                    load_cast(wst[i][:, hf_ * 8:(hf_ + 1) * 8, :], sv[:, hf_ * 8:(hf_ + 1) * 8, :], ("wst", i), 8, 256)
                return wst[i], ("wst", i)

            dctr = [0]
            octr = [0]
            for tt in range(4):
                for tb in range(4):
                    m = tt * 4 + tb
                    dma("sync", hh_[:, tb, :], xown[m * 128:(m + 1) * 128, :], (), [("hh", tb)], f"hh{tb}")
                for oc in range(8):
                    Wo, kWo = next_w2(w_out[:, oc * 256:(oc + 1) * 256])
                    for tb in range(4):
                        m = tt * 4 + tb
                        sl = tb % 2
                        dma("sync", ymx[sl], ymixT[m], (), [("ymx", sl)], f"ymx{sl}")
                        ob = octr[0] % 2
                        octr[0] += 1
                        for k in range(16):
                            mm(ps[:, ob, 0:256], ymx[sl][:, k, :], Wo[:, k, :], k == 0, k == 15, [("ymx", sl), kWo], [("ps", ob)])
                        vtt(hh_[:, tb, oc * 256:(oc + 1) * 256], ps[:, ob, 0:256], hh_[:, tb, oc * 256:(oc + 1) * 256], ALU.add,
                            [("ps", ob), ("hh", tb)], [("hh", tb)])
                bc_load(gbuf, g_ffn, "gbuf", "c_gbuf")
                for tb in range(4):
                    norm_transpose(hh_[:, tb, :], ("hh", tb), gbuf, "gbuf", xb, "xb", 2 if tb % 2 == 0 else 4,
                                   fT[:, :, tb * 128:(tb + 1) * 128], ("fT", tb), 20)
                fkeys = [("fT", tb) for tb in range(4)]
                for hp in range(22):
                    Wg, kWg = next_w2(w_gate[:, hp * 256:(hp + 1) * 256])
                    Wu, kWu = next_w2(w_up[:, hp * 256:(hp + 1) * 256])
                    for c2 in range(2):
                        hc = hp * 2 + c2
                        gb = 0 + 2 * (hc % 2)
                        ub = gb + 1
                        for k in range(16):
                            mm(bank(gb), Wg[:, k, c2 * 128:(c2 + 1) * 128], fT[:, k, :], k == 0, k == 15, [kWg] + fkeys, [("ps", gb)])
                        for k in range(16):
                            mm(bank(ub), Wu[:, k, c2 * 128:(c2 + 1) * 128], fT[:, k, :], k == 0, k == 15, [kWu] + fkeys, [("ps", ub)])
                        act(sg[hc % 2], bank(gb), AF.Silu, [("ps", gb)], [("sg", hc % 2)])
                        vtt(actT[:, hc, :], bank(ub), sg[hc % 2], ALU.mult, [("ps", ub), ("sg", hc % 2)], [("actT", hc)])
                wdv = w_down.rearrange("(hc p) n -> p hc n", p=128)
                for cg in range(8):
                    for hq in range(4):
                        di = dctr[0] % 2
                        dctr[0] += 1
                        load_cast(wdn[di], wdv[:, hq * 11:(hq + 1) * 11, cg * 256:(cg + 1) * 256], ("wdn", di), 11, 256)
                        for tb in range(4):
                            for c in range(11):
                                hc = hq * 11 + c
                                mm(ps[:, 4 + tb, 0:256], actT[:, hc, tb * 128:(tb + 1) * 128], wdn[di][:, c, :], hc == 0, hc == 43,
                                   [("actT", hc), ("wdn", di)], [("ps", 4 + tb)])
                    for tb in range(4):
                        vtt(hh_[:, tb, cg * 256:(cg + 1) * 256], ps[:, 4 + tb, 0:256], hh_[:, tb, cg * 256:(cg + 1) * 256], ALU.add,
                            [("ps", 4 + tb), ("hh", tb)], [("hh", tb)])
                bc_load(gbuf, g_fin, "gbuf", "c_gbuf")
                for tb in range(4):
                    m = tt * 4 + tb
                    rs, krs = rms_rstd(hh_[:, tb, :], 2048, 24, ("hh", tb))
                    vstt(hh_[:, tb, :], hh_[:, tb, :], rs, gbuf, ALU.mult, ALU.mult, [("hh", tb), krs, "gbuf"], [("hh", tb)])
                    dma("sync", y[m * 128:(m + 1) * 128, :], hh_[:, tb, :], [("hh", tb)], [("y", m)], f"yout{tb}")
            o_ = P.op("sync", lambda e: e.nop(), [("y", m) for m in range(16)], ())
            o_.is_nop = True
            P.barrier()
        P.emit(nc, ctx)
    return nc


def _tables(j):
    f32 = np.float32
    k = np.arange(128)[:, None]
    q = np.arange(128)[None, :]
    cb_sel = np.zeros((128, 4, 128), f32)
    for jj in range(4):
        if jj < j:
            v = np.ones((128, 128), bool)
        elif jj == j:
            v = k <= q
        else:
            v = np.zeros((128, 128), bool)
        cb_sel[:, jj, :] = np.where(v, 0.0, NEG)
    cb_win = np.zeros((128, 8, 128), f32)
    for jj in range(8):
        delta = 128 * (j + 4 - jj) + q - k
        cb_win[:, jj, :] = np.where((delta >= 0) & (delta < 512), 0.0, NEG)
    cb_cmp = np.zeros((16, 128, 2, 128), f32)
    cbT_cmp = np.zeros((16, 128, 64), f32)
    for m in range(16):
        t = 128 * (4 * m + j) + np.arange(128)
        NBc = m // 4 + 1
        for w in range(2):
            nb = NBc - 2 + w
            n = 128 * nb + np.arange(128)
            valid = (16 * n[:, None] + 31 <= t[None, :]) & (n[:, None] >= 0)
            cb_cmp[m, :, w, :] = np.where(valid, 0.0, NEG)
        n = 32 * m - 32 + np.arange(64)
        valid = 16 * n[None, :] + 31 <= t[:, None]
        cbT_cmp[m] = np.where(valid, 0.0, NEG)
    qq = np.arange(128)[:, None]
    c = np.arange(9)[None, :]
    rel = c - 1 - 2 * j - (qq >= 64)
    sel_force = np.where((rel == 0) | (rel == -1), 10.0, -1e9).astype(f32)
    sel_future = np.where(rel > 0, -1.0, 1e9).astype(f32)
    return dict(cb_sel=cb_sel.reshape(128, 512), cb_win=cb_win.reshape(128, 1024),
                cb_cmp=cb_cmp.reshape(16, 128, 256), cbT_cmp=cbT_cmp, sel_force=sel_force, sel_future=sel_future)


def _shared_tables():
    f32 = np.float32
    half = 64
    inv = 1.0 / (10000.0 ** (np.arange(half, dtype=np.float32) / half))
    ang = np.arange(S, dtype=np.float32)[:, None] * inv[None, :]
    cs_all = np.concatenate([np.cos(ang), np.sin(ang)], axis=1).astype(f32)
    s = np.arange(128)[:, None, None]
    kb = np.arange(64)[None, :, None]
    kk = np.arange(128)[None, None, :]
    E_all = (s == 2 * kb + kk // 64).astype(f32).reshape(128, 64 * 128)
    kr = np.arange(128)[:, None]
    mc = np.arange(128)[None, :]
    Sh = np.stack([(kr == mc - 1), (kr == mc - 2)], axis=1).astype(f32).reshape(128, 256)
    Hh = np.zeros((32, 272), f32)
    r = np.arange(32)
    Hh[r % 2 == 1, 0] = 1.0
    Hh[r % 2 == 0, 128 + 0] = 1.0
    Hh[r % 2 == 1, 128 + 1] = 1.0
    for m in range(16):
        Hh[2 * m:2 * m + 2, 256 + m] = 1.0
    return dict(cs_all=cs_all, E_all=E_all, Sh=Sh, Hh=Hh, ident=np.eye(128, dtype=f32))


_NC_CACHE = {}


def kernel(**inputs):
    x = np.asarray(inputs["x"], dtype=np.float32)
    sh = _shared_tables()
    base = {
        "w_in": np.ascontiguousarray(inputs["w_in"][0]),
        "w_out": np.ascontiguousarray(inputs["w_out"][0]),
        "w_gate": np.ascontiguousarray(inputs["w_gate"][0]),
        "w_up": np.ascontiguousarray(inputs["w_up"][0]),
        "w_down": np.ascontiguousarray(inputs["w_down"][0]),
        "conv_w": np.ascontiguousarray(inputs["conv_w"][0]),
        "cmp_pe_k": np.ascontiguousarray(inputs["cmp_pe_k"][0]),
        "cmp_w1_k": np.ascontiguousarray(inputs["cmp_w1_k"][0]),
        "cmp_w2_k": np.ascontiguousarray(inputs["cmp_w2_k"][0]),
        "cmp_pe_v": np.ascontiguousarray(inputs["cmp_pe_v"][0]),
        "cmp_w1_v": np.ascontiguousarray(inputs["cmp_w1_v"][0]),
        "cmp_w2_v": np.ascontiguousarray(inputs["cmp_w2_v"][0]),
        "norm_mix": np.ascontiguousarray(inputs["norm_mix"][0]),
        "norm_conv_out": np.ascontiguousarray(inputs["norm_conv_out"][0]),
        "norm_attn_out": np.ascontiguousarray(inputs["norm_attn_out"][0]),
        "norm_ffn": np.ascontiguousarray(inputs["norm_ffn"][0]),
        "norm_final": np.ascontiguousarray(inputs["norm_final"]),
    }
    base = {k: np.asarray(v, dtype=np.float32) for k, v in base.items()}
    base.update(sh)
    in_maps = []
    for c in range(8):
        b, j = c // 4, c % 4
        blocks = [4 * m + j for m in range(16)]
        xb_ = x[b]
        xown = np.concatenate([xb_[128 * qb:128 * qb + 128] for qb in blocks], axis=0)
        xhalo = np.zeros((32, D), np.float32)
        for m, qb in enumerate(blocks):
            if qb > 0:
                xhalo[2 * m:2 * m + 2] = xb_[128 * qb - 2:128 * qb]
        cs_own = np.concatenate([sh["cs_all"][128 * qb:128 * qb + 128] for qb in blocks], axis=0)
        im = dict(base)
        im.update(_tables(j))
        im.update(xall=np.ascontiguousarray(xb_[:KS]), xown=np.ascontiguousarray(xown), xhalo=xhalo,
                  cs_own=np.ascontiguousarray(cs_own))
        im["cs_all"] = np.ascontiguousarray(im["cs_all"][:KS])
        if SHRINK_KEEP is not None:
            im = {k: (v if k in SHRINK_KEEP else np.zeros((1, 1), np.float32)) for k, v in im.items()}
        in_maps.append(im)
    if "nc" not in _NC_CACHE:
        _NC_CACHE["nc"] = build_nc()
    nc = _NC_CACHE["nc"]
    res = run_bass_kernel_spmd(nc, in_maps[:KCORES], core_ids=list(range(KCORES)))
    out = np.zeros((2, S, D), np.float32)
    for c in range(KCORES):
        b, j = c // 4, c % 4
        yv = res.results[c]["y"]
        for m in range(16):
            qb = 4 * m + j
            out[b, 128 * qb:128 * qb + 128] = yv[128 * m:128 * m + 128]
    if DEBUG:
        kernel.last_results = res.results
    return out
```

```python
import os
import numpy as np
import concourse.bass as bass
import concourse.mybir as mybir
from concourse.bass_utils import run_bass_kernel_spmd
from contextlib import ExitStack

F32 = mybir.dt.float32
BF16 = mybir.dt.bfloat16
U8 = mybir.dt.uint8
AF = mybir.ActivationFunctionType
ALU = mybir.AluOpType
AX = mybir.AxisListType

D = 2048
S = 8192
DP = 5656
FF = 5632
NEG = -30000.0
SCALE = 128 ** -0.5
EPS = 1e-6
ENGS = ["tensor", "vector", "scalar", "gpsimd", "sync"]
DEBUG = bool(int(os.environ.get("KDEBUG", "0")))
KSTOP = os.environ.get("KSTOP", "")
KCORES = int(os.environ.get("KCORES", "8"))
KS = int(os.environ.get("KS", "8192"))
KSKIP = set(os.environ.get("KSKIP", "").split(","))
_UA = {"xall", "w_in", "norm_mix", "cs_all", "ident"}
_UA2 = _UA | {"cmp_pe_k", "cmp_w1_k", "cmp_w2_k", "cmp_pe_v", "cmp_w1_v", "cmp_w2_v"}
_U0 = _UA2 | {"xown", "xhalo", "conv_w", "norm_conv_out", "cs_own", "Sh", "Hh"}
_UB = _U0 | {"cb_sel", "cb_win", "cb_cmp", "cbT_cmp", "sel_force", "sel_future", "E_all", "norm_attn_out"}
_USED = {"A": _UA, "A2": _UA2, "0": _U0, "B": _UB}
SHRINK_KEEP = _USED.get(KSTOP)


class Op:
    __slots__ = ("eng", "fn", "deps", "needs_inc", "count", "grp", "is_nop")


class DmaGroup:
    __slots__ = ("stream", "n", "final", "last")


class Prog:
    def __init__(self):
        self.q = {e: [] for e in ENGS}
        self.keys = {}
        self.streams = {}

    def dma_group(self, stream):
        g = DmaGroup()
        g.stream = stream
        g.n = 0
        g.final = None
        g.last = None
        self.streams.setdefault(stream, []).append(g)
        return g

    def op(self, eng, fn, reads=(), writes=(), grp=None, extra=()):
        o = Op()
        o.eng = eng
        o.fn = fn
        o.needs_inc = False
        o.count = None
        o.grp = grp
        o.is_nop = False
        if grp is not None:
            grp.n += 1
            grp.last = o
        ident = eng if grp is None else ("dma", id(grp))
        deps = set(extra)
        writes = list(writes) + [k for k in reads if isinstance(k, tuple) and k[0] == "ps" and k not in writes]
        reads = [k for k in reads if not (isinstance(k, tuple) and k[0] == "ps")]
        for k in reads:
            st = self.keys.setdefault(k, ({}, {}))
            deps.update(st[0].values())
            st[1][ident] = o
        for k in writes:
            st = self.keys.setdefault(k, ({}, {}))
            deps.update(st[0].values())
            deps.update(st[1].values())
            if st[1]:
                st[0].clear()
                st[1].clear()
            st[0][ident] = o
        deps.discard(o)
        o.deps = deps
        self.q[eng].append(o)
        return o

    def barrier(self):
        arr = []
        for e in ENGS:
            for o in reversed(self.q[e]):
                if o.grp is None and not getattr(o, "is_nop", False):
                    arr.append(o)
                    break
        lasts = [g[-1].last for g in self.streams.values() if g and g[-1].last is not None]
        for e in ENGS:
            o = self.op(e, lambda eng: eng.nop(), extra=arr + lasts)
            o.is_nop = True
        self.keys = {}

    def emit(self, nc, ctx):
        engsem = {e: ctx.enter_context(nc.semaphore("es_" + e)) for e in ENGS}
        ssem = {}
        for s, groups in self.streams.items():
            ssem[s] = ctx.enter_context(nc.semaphore("ds_" + str(s)))
            cum = 0
            for g in groups:
                cum += 16 * g.n
                g.final = cum
        for e in ENGS:
            for o in self.q[e]:
                for d in o.deps:
                    if d.grp is None and not (d.eng == e and e == "tensor"):
                        d.needs_inc = True
        for e in ENGS:
            c = 0
            for o in self.q[e]:
                if o.grp is None and o.needs_inc:
                    c += 1
                    o.count = c
        block = ctx.enter_context(nc.Block())
        prog = self

        def run(e):
            def body(eng):
                waited = {}
                for o in prog.q[e]:
                    needs = {}
                    for d in o.deps:
                        if d.grp is not None:
                            s, v, key = ssem[d.grp.stream], d.grp.final, ("s", d.grp.stream)
                        else:
                            if d.eng == e and e == "tensor":
                                continue
                            s, v, key = engsem[d.eng], d.count, ("e", d.eng)
                        if key not in needs or needs[key][1] < v:
                            needs[key] = (s, v)
                    for key, (s, v) in needs.items():
                        if waited.get(key, 0) < v:
                            eng.wait_ge(s, v)
                            waited[key] = v
                    ins = o.fn(eng)
                    if o.grp is not None:
                        ins.then_inc(ssem[o.grp.stream], 16)
                    elif o.needs_inc:
                        ins.then_inc(engsem[e], 1)
            return body

        block.tensor(run("tensor"))
        block.vector(run("vector"))
        block.scalar(run("scalar"))
        block.gpsimd(run("gpsimd"))
        block.sync(run("sync"))


class Arena:
    def __init__(self, t, size):
        self.t = t
        self.size = size
        self.off = 0

    def reset(self):
        self.off = 0

    def alloc(self, shape, dt):
        esz = 4 if dt == F32 else (2 if dt == BF16 else 1)
        n = 1
        for s in shape[1:]:
            n *= s
        nb = (n * esz + 63) // 64 * 64
        assert self.off + nb <= self.size, ("arena overflow", self.off, nb, self.size)
        ap = self.t[0:shape[0], self.off:self.off + n * esz].bitcast(dt)
        self.off += nb
        if len(shape) == 3:
            ap = ap.rearrange("p (a b) -> p a b", a=shape[1])
        elif len(shape) == 4:
            ap = ap.rearrange("p (a b c) -> p a b c", a=shape[1], b=shape[2])
        elif len(shape) == 5:
            ap = ap.rearrange("p (a b c d) -> p a b c d", a=shape[1], b=shape[2], c=shape[3])
        return ap


def build_nc():
    nc = bass.Bass("TRN2", target_bir_lowering=False)

    def din(name, shape):
        if SHRINK_KEEP is not None and name not in SHRINK_KEEP:
            shape = [1, 1]
        return nc.dram_tensor(name, list(shape), F32, kind="ExternalInput").ap()

    xall = din("xall", [KS, D])
    xown = din("xown", [2048, D])
    xhalo = din("xhalo", [32, D])
    w_in = din("w_in", [D, DP])
    w_out = din("w_out", [D, D])
    w_gate = din("w_gate", [D, FF])
    w_up = din("w_up", [D, FF])
    w_down = din("w_down", [FF, D])
    conv_w = din("conv_w", [3, 1024])
    pe_k = din("cmp_pe_k", [32, 128])
    w1_k = din("cmp_w1_k", [4096, 128])
    w2_k = din("cmp_w2_k", [128, 128])
    pe_v = din("cmp_pe_v", [32, 128])
    w1_v = din("cmp_w1_v", [4096, 128])
    w2_v = din("cmp_w2_v", [128, 128])
    g_mix = din("norm_mix", [D])
    g_conv = din("norm_conv_out", [1024])
    g_attn = din("norm_attn_out", [1024])
    g_ffn = din("norm_ffn", [D])
    g_fin = din("norm_final", [D])
    cs_all = din("cs_all", [KS, 128])
    cs_own = din("cs_own", [2048, 128])
    cb_sel_d = din("cb_sel", [128, 4 * 128])
    cb_win_d = din("cb_win", [128, 8 * 128])
    cb_cmp_d = din("cb_cmp", [16, 128, 2 * 128])
    cbT_cmp_d = din("cbT_cmp", [16, 128, 64])
    self_force_d = din("sel_force", [128, 9])
    self_future_d = din("sel_future", [128, 9])
    E_all_d = din("E_all", [128, 64 * 128])
    Sh_d = din("Sh", [128, 2 * 128])
    Hh_d = din("Hh", [32, 272])
    ident_d = din("ident", [128, 128])
    y = nc.dram_tensor("y", [2048, D], F32, kind="ExternalOutput").ap()

    skind = dict(kind="ExternalOutput") if DEBUG else {}
    KTs = nc.dram_tensor("KTs", [4, 2, 128, KS], BF16, **skind).ap()
    VSc = nc.dram_tensor("VSc", [2, KS, 2, 130], BF16, **skind).ap()
    qTs = nc.dram_tensor("qTs", [16, 128, 8, 128], BF16, **skind).ap()
    ymixT = nc.dram_tensor("ymixT", [16, 128, 16, 128], BF16, **skind).ap()
    if DEBUG:
        dbg_kc = nc.dram_tensor("dbg_kc", [128, 2 * 512], BF16, kind="ExternalOutput").ap()
        dbg_vc = nc.dram_tensor("dbg_vc", [128, 4 * 2 * 130], BF16, kind="ExternalOutput").ap()
        dbg_gates = nc.dram_tensor("dbg_gates", [128, 16 * 24], F32, kind="ExternalOutput").ap()

    ctx = ExitStack()
    with ctx:
        P = Prog()
        AR_SIZE = 176 * 1024
        arena_t = ctx.enter_context(nc.sbuf_tensor("arena", [128, AR_SIZE], U8))
        AR = Arena(arena_t, AR_SIZE)

        def sbt(name, shape, dt):
            return ctx.enter_context(nc.sbuf_tensor(name, shape, dt))

        ps = ctx.enter_context(nc.psum_tensor("ps", [128, 8, 512], F32))

        def bank(i):
            return ps[:, i, :]

        def bank_bf(i, n=1):
            return ps[:, i:i + n, :].bitcast(BF16)

        identF = sbt("identF", [128, 128], F32)
        identB = sbt("identB", [128, 128], BF16)
        eps_t = sbt("eps_t", [128, 1], F32)
        kcT = sbt("kcT", [128, 2, 512], BF16)
        vcc = sbt("vcc", [128, 4, 2, 130], BF16)
        gates = sbt("gates", [128, 16, 24], F32)
        u_halo = sbt("u_halo", [32, 1024], F32)
        aT_halo = sbt("aT_halo", [128, 16, 32], BF16)
        junk = sbt("junk", [128, 2048], BF16)
        stat = sbt("stat", [128, 64], F32)

        def dma(queue, out, in_, reads, writes, stream, grp=None):
            g = grp if grp is not None else P.dma_group(stream)
            P.op(queue, lambda e: e.dma_start(out=out, in_=in_), reads, writes, grp=g)
            return g

        def mm(out, lhsT, rhs, start, stop, reads, writes, skip=False):
            P.op("tensor", lambda e: e.matmul(out, lhsT=lhsT, rhs=rhs, start=start, stop=stop,
                                              skip_group_check=skip), reads, writes)

        def tr(out, in_, ident, reads, writes):
            P.op("tensor", lambda e: e.transpose(out=out, in_=in_, identity=ident), reads, writes)

        def act(out, in_, func, reads, writes, **kw):
            P.op("scalar", lambda e: e.activation(out=out, in_=in_, func=func, **kw), reads, writes)

        def vtt(out, in0, in1, op, reads, writes):
            P.op("vector", lambda e: e.tensor_tensor(out=out, in0=in0, in1=in1, op=op), reads, writes)

        def vts(out, in0, s1, s2, op0, op1, reads, writes):
            if op1 is None:
                P.op("vector", lambda e: e.tensor_scalar(out=out, in0=in0, scalar1=s1, scalar2=None, op0=op0),
                     reads, writes)
            else:
                P.op("vector", lambda e: e.tensor_scalar(out=out, in0=in0, scalar1=s1, scalar2=s2, op0=op0, op1=op1),
                     reads, writes)

        def vstt(out, in0, scalar, in1, op0, op1, reads, writes):
            P.op("vector", lambda e: e.scalar_tensor_tensor(out=out, in0=in0, scalar=scalar, in1=in1,
                                                            op0=op0, op1=op1), reads, writes)

        def vcopy(out, in_, reads, writes):
            P.op("vector", lambda e: e.tensor_copy(out=out, in_=in_), reads, writes)

        def vrecip(out, in_, reads, writes):
            P.op("vector", lambda e: e.reciprocal(out=out, in_=in_), reads, writes)

        def vmemset(ap, val, writes):
            P.op("vector", lambda e: e.memset(ap, val), (), writes)

        def bc_load(dst, src_row, key, stream):
            dma("sync", dst, src_row.partition_broadcast(128), (), [key], stream)

        def rms_rstd(src, n, col, kin, np_=128):
            ss = stat[0:np_, col:col + 1]
            rt = stat[0:np_, col + 1:col + 2]
            rs = stat[0:np_, col + 2:col + 3]
            act(junk[0:np_, 0:n], src, AF.Square, [kin], ["junk", ("stat", col)], accum_out=ss)
            act(rt, ss, AF.Sqrt, [("stat", col)], [("stat", col + 1)], scale=1.0 / n, bias=eps_t[0:np_, :])
            vrecip(rs, rt, [("stat", col + 1)], [("stat", col + 2)])
            return rs, ("stat", col + 2)

        def norm_transpose(src, ksrc, gain_bc, kgain, xb, kxb, pbank, dst, kdst, col, np_=128):
            rs, krs = rms_rstd(src, 2048, col, ksrc, np_)
            vstt(xb[0:np_, :], src, rs, gain_bc[0:np_, :], ALU.mult, ALU.mult, [ksrc, krs, kgain], [kxb])
            pt = bank_bf(pbank, 2).rearrange("p a (c t) -> p (a c) t", t=128)
            idn = identB[0:np_, 0:np_]
            for k in range(16):
                tr(pt[:, k, 0:np_], xb[0:np_, k * 128:(k + 1) * 128], idn, [kxb, "identB"],
                   [("ps", pbank), ("ps", pbank + 1)])
            act(dst, pt[:, :, 0:np_], AF.Copy, [("ps", pbank), ("ps", pbank + 1)], [kdst])

        def load_w(dst, src_cols, key, stream):
            dma("gpsimd", dst, src_cols.rearrange("(kc p) n -> p kc n", p=128), (), [key], stream)

        dma("sync", identF[:], ident_d, (), ["identF"], "c_id")
        vcopy(identB[:], identF[:], ["identF"], ["identB"])
        vmemset(eps_t[:], EPS, ["eps"])
        P.barrier()
        for _once in (0,):

            AR.reset()
            wkv = AR.alloc([128, 16, 1536], BF16)
            gmix = AR.alloc([128, 2048], F32)
            xs = [AR.alloc([128, 2048], F32) for _ in range(2)]
            xb = AR.alloc([128, 2048], BF16)
            aT = [AR.alloc([128, 16, 128], BF16) for _ in range(2)]
            cs = [AR.alloc([128, 2, 64], F32) for _ in range(2)]
            rt1 = AR.alloc([128, 3, 2, 64], F32)
            rt2 = AR.alloc([128, 3, 2, 64], F32)
            krope = AR.alloc([128, 3, 2, 2, 64], BF16)
            vcb = AR.alloc([128, 2, 128], BF16)
            vst = [AR.alloc([128, 2, 2, 130], BF16) for _ in range(2)]
            kts = [AR.alloc([128, 8, 128], BF16) for _ in range(2)]

            for g in range(3):
                load_w(wkv[:, :, g * 512:(g + 1) * 512], w_in[:, 4096 + g * 512:4096 + (g + 1) * 512], ("wkv", g), f"wkv{g}")
            bc_load(gmix, g_mix, "gmix", "c_gmix")
            for s_ in range(2):
                if "ms" not in KSKIP:
                    vmemset(vst[s_][:, :, :, 128:130], 1.0, [("vst1", s_)])
            NB_A = int(os.environ.get("KNBA", "64"))
            dma("sync", xs[0], xall[0:128, :], (), [("xs", 0)], "xs0")
            if NB_A > 1:
                dma("sync", xs[1], xall[128:256, :], (), [("xs", 1)], "xs1")
            for i in range(NB_A):
                sl = i % 2
                dma("sync", cs[sl], cs_all[i * 128:(i + 1) * 128, :].rearrange("p (a b) -> p a b", a=2), (), [("cs", sl)], f"cs{sl}")
                if i == 0:
                    norm_transpose(xs[0], ("xs", 0), gmix, "gmix", xb, "xb", 0, aT[0], ("aT", 0), 0)
                for g in range(3 if "mm" not in KSKIP else 0):
                    for k in range(16):
                        mm(bank(2 + g), aT[sl][:, k, :], wkv[:, k, g * 512:(g + 1) * 512], k == 0, k == 15,
                           [("aT", sl), ("wkv", g)], [("ps", 2 + g)])
                if i + 1 < NB_A:
                    norm_transpose(xs[1 - sl], ("xs", 1 - sl), gmix, "gmix", xb, "xb", 6 if sl == 0 else 0,
                                   aT[1 - sl], ("aT", 1 - sl), 0)
                    if i + 2 < NB_A:
                        dma("sync", xs[sl], xall[(i + 2) * 128:(i + 3) * 128, :], (), [("xs", sl)], f"xs{sl}")
                if "rope" not in KSKIP:
                    z = ps[:, 2:5, 0:256].rearrange("p t (h f x) -> p t h f x", h=2, f=2)
                    cosb = cs[sl][:, 0, :].unsqueeze(1).unsqueeze(1).to_broadcast([128, 3, 2, 64])
                    sinb = cs[sl][:, 1, :].unsqueeze(1).unsqueeze(1).to_broadcast([128, 3, 2, 64])
                    pk = [("ps", 2), ("ps", 3), ("ps", 4)]
                    vtt(rt1, z[:, :, :, 0, :], cosb, ALU.mult, pk + [("cs", sl)], ["rt1"])
                    vtt(rt2, z[:, :, :, 1, :], sinb, ALU.mult, pk + [("cs", sl)], ["rt2"])
                    vtt(krope[:, :, :, 0, :], rt1, rt2, ALU.subtract, ["rt1", "rt2"], ["krope0"])
                    vtt(rt1, z[:, :, :, 1, :], cosb, ALU.mult, pk + [("cs", sl)], ["rt1"])
                    vtt(rt2, z[:, :, :, 0, :], sinb, ALU.mult, pk + [("cs", sl)], ["rt2"])
                    vtt(krope[:, :, :, 1, :], rt1, rt2, ALU.add, ["rt1", "rt2"], ["krope1"])
                if "vcopy" not in KSKIP:
                    act(vcb, ps[:, 2, 256:512].rearrange("p (h x) -> p h x", h=2), AF.Copy, [("ps", 2)], ["vcb"])
                    for t_ in range(2):
                        act(vst[sl][:, t_, :, 0:128], ps[:, 3 + t_, 256:512].rearrange("p (h x) -> p h x", h=2), AF.Copy,
                            [("ps", 3 + t_)], [("vst", sl)])
                if "tr5" not in KSKIP:
                    pk5 = bank_bf(5).rearrange("p a (c t) -> p (a c) t", t=128)
                    for t in range(3):
                        for hk in range(2):
                            tr(pk5[:, t * 2 + hk, :], krope[:, t, hk, :, :].rearrange("p f x -> p (f x)"), identB[:],
                               ["krope0", "krope1", "identB"], [("ps", 5)])
                    for hk in range(2):
                        tr(pk5[:, 6 + hk, :], vcb[:, hk, :], identB[:], ["vcb", "identB"], [("ps", 5)])
                    vcopy(kts[sl], pk5, [("ps", 5)], [("kts", sl)])
                if "st" not in KSKIP:
                    dma("sync", KTs[:, :, :, i * 128:(i + 1) * 128].rearrange("t h d s -> d (t h) s"), kts[sl],
                        [("kts", sl)], [("KTs", i)], f"kst{sl}")
                    dma("sync", VSc[:, i * 128:(i + 1) * 128, :, :].rearrange("t s h c -> s t (h c)"),
                        vst[sl].rearrange("p t h c -> p t (h c)"), [("vst", sl), ("vst1", sl)], [("VSc", i)], f"vst{sl}")
            P.barrier()
            if KSTOP == "A":
                break

            AR.reset()
            w1b = [AR.alloc([128, 32, 128], BF16) for _ in range(2)]
            w2b = [AR.alloc([128, 128], BF16) for _ in range(2)]
            pe_f = [AR.alloc([32, 128], F32) for _ in range(2)]
            peT = [AR.alloc([128, 32], BF16) for _ in range(2)]
            c0 = [AR.alloc([128, 1], F32) for _ in range(2)]
            raw = [AR.alloc([128, S], BF16) for _ in range(2)]
            h1T = AR.alloc([128, 512], BF16)
            for ti, (w1d, w2d, ped) in enumerate([(w1_k, w2_k, pe_k), (w1_v, w2_v, pe_v)]):
                dma("gpsimd", w1b[ti], w1d.rearrange("(l d) e -> d l e", d=128), (), [("w1b", ti)], f"w1b{ti}")
                dma("gpsimd", w2b[ti], w2d, (), [("w2b", ti)], f"w2b{ti}")
                dma("sync", pe_f[ti], ped, (), [("pe_f", ti)], f"pef{ti}")
                tr(ps[:, 0, 0:32], pe_f[ti], identF[0:32, 0:32], [("pe_f", ti), "identF"], [("ps", 0)])
                vcopy(peT[ti], ps[:, 0, 0:32], [("ps", 0)], [("peT", ti)])
                for l in range(32):
                    mm(ps[:, 1, 0:1], w1b[ti][:, l, :], peT[ti][:, l:l + 1], l == 0, l == 31,
                       [("w1b", ti), ("peT", ti)], [("ps", 1)])
                vcopy(c0[ti], ps[:, 1, 0:1], [("ps", 1)], [("c0", ti)])
            vmemset(vcc[:, :, :, 128:130], 1.0, ["vcc1"])
            it = 0
            for ti in range(2):
                for hk in range(2):
                    sl = it % 2
                    it += 1
                    dma("sync", raw[sl], KTs[0 if ti == 0 else 3, hk, :, :], (), [("raw", sl)], f"raw{sl}")
                    r16 = raw[sl].rearrange("p (n s) -> p n s", s=16)
                    pb = 2 + sl
                    for l in range(32):
                        rhs = r16[:, 0:511, l] if l < 16 else r16[:, 1:512, l - 16]
                        mm(ps[:, pb, 0:511], w1b[ti][:, l, :], rhs, l == 0, l == 31, [("w1b", ti), ("raw", sl)], [("ps", pb)])
                    vmemset(h1T[:, 511:512], 0.0, ["h1T"])
                    act(h1T[:, 0:511], ps[:, pb, 0:511], AF.Silu, [("ps", pb), ("c0", ti)], ["h1T"], bias=c0[ti])
                    if ti == 0:
                        mm(bank(4), w2b[0], h1T, True, True, [("w2b", 0), "h1T"], [("ps", 4)])
                        vcopy(kcT[:, hk, :], bank(4), [("ps", 4)], ["kcT"])
                    else:
                        for nb in range(4):
                            mm(ps[:, 5, nb * 128:(nb + 1) * 128], h1T[:, nb * 128:(nb + 1) * 128], w2b[1], True, True,
                               [("w2b", 1), "h1T"], [("ps", 5)])
                        vcopy(vcc[:, :, hk, 0:128], ps[:, 5, :].rearrange("p (n x) -> p n x", n=4), [("ps", 5)], ["vcc"])
            if DEBUG:
                dma("sync", dbg_kc, kcT[:].rearrange("p a b -> p (a b)"), ["kcT"], ["dbg_kc"], "dbg1")
                dma("sync", dbg_vc, vcc[:].rearrange("p a b c -> p (a b c)"), ["vcc", "vcc1"], ["dbg_vc"], "dbg2")
            P.barrier()
            if KSTOP == "A2":
                break

            AR.reset()
            gmix = AR.alloc([128, 2048], F32)
            gconv = AR.alloc([128, 1024], F32)
            wcv = AR.alloc([128, 3, 1024], F32)
            shf = AR.alloc([128, 2, 128], F32)
            hfix = AR.alloc([32, 2, 128], F32)
            hmask = AR.alloc([32, 16], F32)
            uh = [AR.alloc([32, 256], F32) for _ in range(2)]
            xs = [AR.alloc([128, 2048], F32) for _ in range(2)]
            xb = AR.alloc([128, 2048], BF16)
            aTo = AR.alloc([128, 16, 1024], BF16)
            wch = [AR.alloc([128, 16, 256], BF16) for _ in range(6)]
            wgt = AR.alloc([128, 16, 24], BF16)
            ycb = AR.alloc([128, 8, 1024], BF16)
            cso = AR.alloc([128, 8, 2, 64], F32)
            ccs = [AR.alloc([128, 256], F32) for _ in range(2)]
            uu = [AR.alloc([128, 256], F32) for _ in range(2)]
            aa = [AR.alloc([128, 256], F32) for _ in range(2)]
            tb_ = [AR.alloc([128, 256], F32) for _ in range(2)]
            qr1 = AR.alloc([128, 2, 64], F32)
            qr2 = AR.alloc([128, 2, 64], F32)
            qrope = [AR.alloc([128, 2, 2, 64], BF16) for _ in range(2)]
            qts = [AR.alloc([128, 2, 128], BF16) for _ in range(2)]
            ycn = AR.alloc([128, 1024], BF16)
            ymT = [AR.alloc([128, 8, 128], BF16) for _ in range(2)]
            ssq = AR.alloc([128, 8, 4], F32)

            bc_load(gmix, g_mix, "gmix", "c_gmix")
            bc_load(gconv, g_conv, "gconv", "c_gconv")
            for kk in range(3):
                bc_load(wcv[:, kk, :], conv_w[kk, :], ("wcv", kk), f"c_wcv{kk}")
            dma("sync", shf, Sh_d.rearrange("p (a b) -> p a b", a=2), (), ["shf"], "c_sh")
            dma("sync", hfix, Hh_d[:, 0:256].rearrange("p (a b) -> p a b", a=2), (), ["hfix"], "c_hh")
            dma("sync", hmask, Hh_d[:, 256:272], (), ["hmask"], "c_hm")
            wcnt = [0]

            def next_w(src_cols, ncols=256):
                i = wcnt[0] % 6
                wcnt[0] += 1
                load_w(wch[i][:, :, 0:ncols], src_cols, ("wch", i), f"wch{i}")
                return wch[i], ("wch", i)

            for hf in range(2):
                if hf == 0:
                    dma("sync", xs[1][0:32, :], xhalo, (), [("xs", 1)], "xs1")
                    norm_transpose(xs[1][0:32, :], ("xs", 1), gmix, "gmix", xb, "xb", 0, aT_halo[:], "aT_halo", 0, np_=32)
                for mi in range(8):
                    m = hf * 8 + mi
                    sl = mi % 2
                    dma("sync", xs[sl], xown[m * 128:(m + 1) * 128, :], (), [("xs", sl)], f"xs{sl}")
                    norm_transpose(xs[sl], ("xs", sl), gmix, "gmix", xb, "xb", 0 if sl == 0 else 6,
                                   aTo[:, :, mi * 128:(mi + 1) * 128], ("aTo", mi), 0)
                dma("sync", cso, cs_own[hf * 1024:(hf + 1) * 1024, :].rearrange("(m p) (a b) -> p m a b", p=128, a=2),
                    (), ["cso"], "cso")
                for cg in range(4):
                    c0_, c1_ = cg * 256, (cg + 1) * 256
                    Wb, kWb = next_w(w_in[:, c0_:c1_])
                    Wc, kWc = next_w(w_in[:, 1024 + c0_:1024 + c1_])
                    Wh, kWh = next_w(w_in[:, 2048 + c0_:2048 + c1_])
                    if hf == 0:
                        for k in range(16):
                            mm(ps[0:32, 2, 0:256], aT_halo[:, k, :], Wc[:, k, :], k == 0, k == 15, ["aT_halo", kWc], [("ps", 2)])
                        for k in range(16):
                            mm(ps[0:32, 2, 256:512], aT_halo[:, k, :], Wh[:, k, :], k == 0, k == 15, ["aT_halo", kWh], [("ps", 2)])
                        act(ccs[0][0:32, :], ps[0:32, 2, 0:256], AF.Copy, [("ps", 2)], [("ccs", 0)])
                        vtt(u_halo[:, c0_:c1_], ps[0:32, 2, 256:512], ccs[0][0:32, :], ALU.mult, [("ps", 2), ("ccs", 0)], ["u_halo"])
                    for mi in range(8):
                        m = hf * 8 + mi
                        sl = mi % 2
                        bA, bB, bC = (2, 3, 4) if sl == 0 else (5, 6, 7)
                        at = aTo[:, :, mi * 128:(mi + 1) * 128]
                        for k in range(16):
                            mm(ps[:, bA, 0:256], at[:, k, :], Wc[:, k, :], k == 0, k == 15, [("aTo", mi), kWc], [("ps", bA)])
                        for k in range(16):
                            mm(ps[:, bA, 256:512], at[:, k, :], Wh[:, k, :], k == 0, k == 15, [("aTo", mi), kWh], [("ps", bA)])
                        for k in range(16):
                            mm(ps[:, bB, 0:256], at[:, k, :], Wb[:, k, :], k == 0, k == 15, [("aTo", mi), kWb], [("ps", bB)])
                        act(ccs[sl], ps[:, bA, 0:256], AF.Copy, [("ps", bA)], [("ccs", sl)])
                        vtt(uu[sl], ps[:, bA, 256:512], ccs[sl], ALU.mult, [("ps", bA), ("ccs", sl)], [("uu", sl)])
                        vts(uh[sl], u_halo[:, c0_:c1_], hmask[:, m:m + 1], None, ALU.mult, None, ["u_halo", "hmask"], [("uh", sl)])
                        mm(ps[:, bC, 0:256], shf[:, 0, :], uu[sl], True, False, ["shf", ("uu", sl)], [("ps", bC)])
                        mm(ps[:, bC, 0:256], hfix[:, 0, :], uh[sl], False, True, ["hfix", ("uh", sl)], [("ps", bC)])
                        mm(ps[:, bC, 256:512], shf[:, 1, :], uu[sl], True, False, ["shf", ("uu", sl)], [("ps", bC)])
                        mm(ps[:, bC, 256:512], hfix[:, 1, :], uh[sl], False, True, ["hfix", ("uh", sl)], [("ps", bC)])
                        vtt(aa[sl], uu[sl], wcv[:, 2, c0_:c1_], ALU.mult, [("uu", sl), ("wcv", 2)], [("aa", sl)])
                        vtt(tb_[sl], ps[:, bC, 0:256], wcv[:, 1, c0_:c1_], ALU.mult, [("ps", bC), ("wcv", 1)], [("tb", sl)])
                        vtt(aa[sl], aa[sl], tb_[sl], ALU.add, [("aa", sl), ("tb", sl)], [("aa", sl)])
                        vtt(tb_[sl], ps[:, bC, 256:512], wcv[:, 0, c0_:c1_], ALU.mult, [("ps", bC), ("wcv", 0)], [("tb", sl)])
                        vtt(aa[sl], aa[sl], tb_[sl], ALU.add, [("aa", sl), ("tb", sl)], [("aa", sl)])
                        vtt(ycb[:, mi, c0_:c1_], ps[:, bB, 0:256], aa[sl], ALU.mult, [("ps", bB), ("aa", sl)], [("ycb", mi, cg)])
                        act(junk[:, 0:256], ycb[:, mi, c0_:c1_], AF.Square, [("ycb", mi, cg)], ["junk", ("ssq", mi, cg)],
                            accum_out=ssq[:, mi, cg:cg + 1])
                for qc in range(4):
                    Wq, kWq = next_w(w_in[:, 3072 + qc * 256:3072 + (qc + 1) * 256])
                    for mi in range(8):
                        m = hf * 8 + mi
                        sl = mi % 2
                        bq = 2 if sl == 0 else 5
                        at = aTo[:, :, mi * 128:(mi + 1) * 128]
                        for k in range(16):
                            mm(ps[:, bq, 0:256], at[:, k, :], Wq[:, k, :], k == 0, k == 15, [("aTo", mi), kWq], [("ps", bq)])
                        z = ps[:, bq, 0:256].rearrange("p (h f x) -> p h f x", h=2, f=2)
                        cosb = cso[:, mi, 0, :].unsqueeze(1).to_broadcast([128, 2, 64])
                        sinb = cso[:, mi, 1, :].unsqueeze(1).to_broadcast([128, 2, 64])
                        kz = [("ps", bq), "cso"]
                        vtt(qr1, z[:, :, 0, :], cosb, ALU.mult, kz, ["qr1"])
                        vtt(qr2, z[:, :, 1, :], sinb, ALU.mult, kz, ["qr2"])
                        vtt(qrope[sl][:, :, 0, :], qr1, qr2, ALU.subtract, ["qr1", "qr2"], [("qrope0", sl)])
                        vtt(qr1, z[:, :, 1, :], cosb, ALU.mult, kz, ["qr1"])
                        vtt(qr2, z[:, :, 0, :], sinb, ALU.mult, kz, ["qr2"])
                        vtt(qrope[sl][:, :, 1, :], qr1, qr2, ALU.add, ["qr1", "qr2"], [("qrope1", sl)])
                        bt = 3 if sl == 0 else 6
                        ptq = bank_bf(bt)[:, 0, 0:256].rearrange("p (h t) -> p h t", h=2)
                        for hh in range(2):
                            tr(ptq[:, hh, :], qrope[sl][:, hh, :, :].rearrange("p f x -> p (f x)"), identB[:],
                               [("qrope0", sl), ("qrope1", sl), "identB"], [("ps", bt)])
                        act(qts[sl], ptq, AF.Copy, [("ps", bt)], [("qts", sl)])
                        dma("sync", qTs[m, :, qc * 2:qc * 2 + 2, :], qts[sl], [("qts", sl)], [("qTs", m, qc)], f"qts{sl}")
                load_w(wgt, w_in[:, 5632:5656], "wgt", "wgt")
                for mi in range(8):
                    m = hf * 8 + mi
                    bq = 4 if mi % 2 == 0 else 7
                    at = aTo[:, :, mi * 128:(mi + 1) * 128]
                    for k in range(16):
                        mm(ps[:, bq, 0:24], at[:, k, :], wgt[:, k, :], k == 0, k == 15, [("aTo", mi), "wgt"], [("ps", bq)])
                    act(gates[:, m, :], ps[:, bq, 0:24], AF.Sigmoid, [("ps", bq)], [("gates", m)])
                for mi in range(8):
                    m = hf * 8 + mi
                    sl = mi % 2
                    P.op("vector", lambda e, mi=mi: e.tensor_reduce(out=stat[:, 8:9], in_=ssq[:, mi, :], axis=AX.X, op=ALU.add),
                         [("ssq", mi, c) for c in range(4)], [("stat", 8)])
                    act(stat[:, 9:10], stat[:, 8:9], AF.Sqrt, [("stat", 8)], [("stat", 9)], scale=1.0 / 1024, bias=eps_t[:])
                    vrecip(stat[:, 10:11], stat[:, 9:10], [("stat", 9)], [("stat", 10)])
                    vstt(ycn, ycb[:, mi, :], stat[:, 10:11], gconv, ALU.mult, ALU.mult,
                         [("ycb", mi, c) for c in range(4)] + [("stat", 10), "gconv"], ["ycn"])
                    bt = 3 if sl == 0 else 6
                    pty = bank_bf(bt).rearrange("p a (c t) -> p (a c) t", t=128)
                    for c in range(8):
                        tr(pty[:, c, :], ycn[:, c * 128:(c + 1) * 128], identB[:], ["ycn", "identB"], [("ps", bt)])
                    act(ymT[sl], pty, AF.Copy, [("ps", bt)], [("ymT", sl)])
                    dma("sync", ymixT[m, :, 0:8, :], ymT[sl], [("ymT", sl)], [("ymixT", m, 0)], f"ymT{sl}")
            if DEBUG:
                dma("sync", dbg_gates, gates[:].rearrange("p a b -> p (a b)"), [("gates", m) for m in range(16)], ["dbg_g"], "dbg3")
            P.barrier()
            if KSTOP == "0":
                break

            AR.reset()
            ksT = AR.alloc([128, 2, S], BF16)
            vsA = AR.alloc([128, 64, 2, 130], BF16)
            Eall = AR.alloc([128, 64, 128], BF16)
            cbs = AR.alloc([128, 4, 4, 128], BF16)
            cbw = AR.alloc([128, 8, 4, 128], BF16)
            cbs_f = AR.alloc([128, 8, 128], F32)
            gattn = AR.alloc([128, 1024], F32)
            sfz = AR.alloc([128, 9], F32)
            suz = AR.alloc([128, 9], F32)
            qT = [AR.alloc([128, 8, 128], BF16) for _ in range(2)]
            kwT = [AR.alloc([128, 2, 1024], BF16) for _ in range(2)]
            vwA = [AR.alloc([128, 8, 2, 130], BF16) for _ in range(2)]
            cbc_f = [AR.alloc([128, 2, 128], F32) for _ in range(2)]
            cbc = [AR.alloc([128, 2, 4, 128], BF16) for _ in range(2)]
            cbT = [AR.alloc([128, 64], F32) for _ in range(2)]
            ee = [AR.alloc([128, 512], F32) for _ in range(2)]
            psum_ = AR.alloc([128, 512], F32)
            imp = AR.alloc([128, 128], F32)
            imp2 = AR.alloc([128, 128], F32)
            m8 = AR.alloc([128, 16], F32)
            selb = AR.alloc([128, 128], F32)
            sbT = [AR.alloc([128, 4, 128], BF16) for _ in range(2)]
            pT = [AR.alloc([128, 512], BF16) for _ in range(3)]
            yat = AR.alloc([128, 8, 128], F32)
            ytmp = AR.alloc([128, 8, 128], F32)
            coef = AR.alloc([128, 8], F32)
            dden = AR.alloc([128, 8], F32)
            yan = AR.alloc([128, 1024], BF16)
            ymT2 = [AR.alloc([128, 8, 128], BF16) for _ in range(2)]
            Dh = AR.alloc([128, 8], F32)

            for hk in range(2):
                dma("sync", ksT[:, hk, :], KTs[1, hk, :, :], (), [("ksT", hk)], f"ksT{hk}")
            for qd in range(4):
                dma("sync", vsA[:, qd * 16:(qd + 1) * 16, :, :].rearrange("p b h c -> p b (h c)"),
                    VSc[0, qd * 2048:(qd + 1) * 2048, :, :].rearrange("(b p) h c -> p b (h c)", p=128), (), [("vsA", qd)], f"vsA{qd}")
            for qd in range(8):
                dma("gpsimd", Eall[:, qd * 8:(qd + 1) * 8, :], E_all_d[:, qd * 1024:(qd + 1) * 1024].rearrange("p (a b) -> p a b", a=8),
                    (), [("Eall", qd // 2)], f"Eall{qd}")
            dma("sync", cbs_f[:, 0:4, :], cb_sel_d.rearrange("p (a b) -> p a b", a=4), (), ["cbs_f"], "c_cbs")
            vcopy(cbs, cbs_f[:, 0:4, :].unsqueeze(2).to_broadcast([128, 4, 4, 128]), ["cbs_f"], ["cbs"])
            dma("sync", cbs_f, cb_win_d.rearrange("p (a b) -> p a b", a=8), ["cbs_f"], ["cbs_f"], "c_cbs")
            vcopy(cbw, cbs_f.unsqueeze(2).to_broadcast([128, 8, 4, 128]), ["cbs_f"], ["cbw"])
            bc_load(gattn, g_attn, "gattn", "c_gattn")
            dma("sync", sfz, self_force_d, (), ["sfz"], "c_sfz")
            dma("sync", suz, self_future_d, (), ["suz"], "c_suz")
            vmemset(imp, -1.0, ["imp"])

            sctr = [0]
            pctr = [0]
            S_BANKS = [0, 1]
            O_BANK0 = 2

            def o_view(h):
                return ps[:, O_BANK0 + h // 2, (h % 2) * 256:(h % 2) * 256 + 129]

            def attend(m, hk, visits, o_started):
                qsl = m % 2
                qrhs = qT[qsl][:, 4 * hk:4 * hk + 4, :].rearrange("p g q -> p (g q)")
                def scores(vi):
                    kT_ap, kkeys, v_ap, vkeys, biases = visits[vi]
                    sb_ = S_BANKS[sctr[0] % 2]
                    sctr[0] += 1
                    nb = len(biases)
                    mm(bank(sb_), kT_ap, qrhs, True, nb == 0, kkeys + [("qT", qsl)], [("ps", sb_)])
                    for bi, (bl, br, bk) in enumerate(biases):
                        mm(bank(sb_), bl, br, False, bi == nb - 1, bk, [("ps", sb_)])
                    return sb_

                cur = scores(0)
                for vi in range(len(visits)):
                    kT_ap, kkeys, v_ap, vkeys, biases = visits[vi]
                    last = vi == len(visits) - 1
                    nxt = scores(vi + 1) if not last else None
                    sb_ = cur
                    pi = pctr[0] % 3
                    pctr[0] += 1
                    act(pT[pi], bank(sb_), AF.Exp, [("ps", sb_)], [("pT", pi)], scale=SCALE)
                    for g_ in range(4):
                        h = 4 * hk + g_
                        bnk = O_BANK0 + h // 2
                        st = bnk not in o_started
                        o_started.add(bnk)
                        mm(o_view(h), pT[pi][:, g_ * 128:(g_ + 1) * 128], v_ap, st, last, [("pT", pi)] + vkeys,
                           [("ps", bnk)], skip=True)
                    cur = nxt

            def finalize(m, br, first):
                ov = ps[:, O_BANK0:O_BANK0 + 4, :].rearrange("p b (h x) -> p (b h) x", h=2)
                okeys = [("ps", O_BANK0 + b_) for b_ in range(4)]
                vts(dden, ov[:, :, 128], 1e-30, None, ALU.max, None, okeys, ["dden"])
                vrecip(dden, dden, ["dden"], ["dden"])
                gv = gates[:, m, :].rearrange("p (h b) -> p h b", b=3)[:, :, br]
                vtt(coef, dden, gv, ALU.mult, ["dden", ("gates", m)], ["coef"])
                cb_ = coef[:, :].unsqueeze(2).to_broadcast([128, 8, 128])
                if first:
                    vtt(yat, ov[:, :, 0:128], cb_, ALU.mult, okeys + ["coef"], ["yat"])
                else:
                    vtt(ytmp, ov[:, :, 0:128], cb_, ALU.mult, okeys + ["coef"], ["ytmp"])
                    vtt(yat, yat, ytmp, ALU.add, ["yat", "ytmp"], ["yat"])

            def issue_loads(m):
                qsl = m % 2
                dma("sync", qT[qsl], qTs[m], (), [("qT", qsl)], f"qT{qsl}")
                t0 = 512 * (m - 1)
                for hk in range(2):
                    if m == 0:
                        dma("sync", kwT[qsl][:, hk, 512:1024], KTs[2, hk, :, 0:512], (), [("kwT", qsl)], f"kwT{qsl}")
                    else:
                        dma("sync", kwT[qsl][:, hk, :], KTs[2, hk, :, t0:t0 + 1024], (), [("kwT", qsl)], f"kwT{qsl}")
                if m == 0:
                    dma("sync", vwA[qsl][:, 4:8, :, :].rearrange("p b h c -> p b (h c)"),
                        VSc[1, 0:512, :, :].rearrange("(b p) h c -> p b (h c)", p=128), (), [("vwA", qsl)], f"vwA{qsl}")
                else:
                    dma("sync", vwA[qsl].rearrange("p b h c -> p b (h c)"),
                        VSc[1, t0:t0 + 1024, :, :].rearrange("(b p) h c -> p b (h c)", p=128), (), [("vwA", qsl)], f"vwA{qsl}")
                dma("sync", cbc_f[qsl], cb_cmp_d[m].rearrange("p (a b) -> p a b", a=2), (), [("cbc_f", qsl)], f"cbcf{qsl}")
                dma("sync", cbT[qsl], cbT_cmp_d[m], (), [("cbT", qsl)], f"cbT{qsl}")

            issue_loads(0)
            for m in range(16):
                qsl = m % 2
                nwin0 = 4 if m == 0 else 0
                if m + 1 < 16:
                    issue_loads(m + 1)
                vcopy(cbc[qsl], cbc_f[qsl].unsqueeze(2).to_broadcast([128, 2, 4, 128]), [("cbc_f", qsl)], [("cbc", qsl)])
                Nm = 32 * (m + 1)
                ns = 8 * (m + 1)
                NBc = m // 4 + 1
                zlo = max(0, 32 * m - 32)
                zc0 = zlo - (32 * m - 32)
                for hk in range(2):
                    for g_ in range(4):
                        h = 4 * hk + g_
                        tbk = 6 + (h % 2)
                        esl = h % 2
                        mm(ps[:, tbk, 0:Nm], qT[qsl][:, h, :], kcT[:, hk, 0:Nm], True, True, [("qT", qsl), "kcT"], [("ps", tbk)])
                        vtt(ps[:, tbk, zlo:Nm], ps[:, tbk, zlo:Nm], cbT[qsl][:, zc0:64], ALU.add,
                            [("ps", tbk), ("cbT", qsl)], [("ps", tbk)])
                        act(ee[esl][:, 0:Nm], ps[:, tbk, 0:Nm], AF.Exp, [("ps", tbk)], [("ee", esl), ("Dh", h)],
                            scale=SCALE, accum_out=Dh[:, h:h + 1])
                        vts(Dh[:, h:h + 1], Dh[:, h:h + 1], 1e-30, None, ALU.max, None, [("Dh", h)], [("Dh", h)])
                        vrecip(Dh[:, h:h + 1], Dh[:, h:h + 1], [("Dh", h)], [("Dh", h)])
                        if g_ == 0:
                            vts(psum_[:, 0:Nm], ee[esl][:, 0:Nm], Dh[:, h:h + 1], None, ALU.mult, None,
                                [("ee", esl), ("Dh", h)], ["psum"])
                        else:
                            vstt(psum_[:, 0:Nm], ee[esl][:, 0:Nm], Dh[:, h:h + 1], psum_[:, 0:Nm], ALU.mult, ALU.add,
                                 [("ee", esl), ("Dh", h), "psum"], ["psum"])
                    P.op("vector", lambda e, Nm=Nm, ns=ns: e.tensor_reduce(
                        out=imp[:, 0:ns], in_=psum_[:, 0:Nm].rearrange("p (s f) -> p s f", f=4), axis=AX.X, op=ALU.add),
                        ["psum"], ["imp"])
                    vtt(imp[:, 1:ns], imp[:, 1:ns], psum_[:, 0:Nm].rearrange("p (s f) -> p s f", f=4)[:, 0:ns - 1, 3], ALU.add,
                        ["imp", "psum"], ["imp"])
                    vmemset(imp[:, 0:1], 10.0, ["imp"])
                    if m == 0:
                        zs, zt = imp[:, 0:8], slice(1, 9)
                    else:
                        zs, zt = imp[:, 8 * m - 1:8 * m + 8], slice(0, 9)
                    vtt(zs, zs, sfz[:, zt], ALU.max, ["imp", "sfz"], ["imp"])
                    vtt(zs, zs, suz[:, zt], ALU.min, ["imp", "suz"], ["imp"])
                    P.op("vector", lambda e: e.max(out=m8[:, 0:8], in_=imp[:, :]), ["imp"], ["m8a"])
                    P.op("vector", lambda e: e.match_replace(out=imp2[:, :], in_to_replace=m8[:, 0:8], in_values=imp[:, :],
                                                             imm_value=-2.0), ["imp", "m8a"], ["imp2"])
                    P.op("vector", lambda e: e.max(out=m8[:, 8:16], in_=imp2[:, :]), ["imp2"], ["m8b"])
                    vts(selb, imp, m8[:, 15:16], 1.0, ALU.is_ge, ALU.subtract, ["imp", "m8b"], ["selb"])
                    tr(ps[:, 6, 0:128], selb, identF[:], ["selb", "identF"], [("ps", 6)])
                    act(sbT[hk], ps[:, 6, 0:128].unsqueeze(1).to_broadcast([128, 4, 128]), AF.Copy, [("ps", 6)], [("sbT", hk)],
                        scale=-NEG)
                idB = identB[:]
                o_started = set()
                for hk in range(2):
                    visits = []
                    for nb in range(NBc):
                        biases = []
                        w_ = nb - (NBc - 2)
                        if w_ >= 0:
                            biases.append((idB, cbc[qsl][:, w_, :, :].rearrange("p g q -> p (g q)"), ["identB", ("cbc", qsl)]))
                        visits.append((kcT[:, hk, nb * 128:(nb + 1) * 128], ["kcT"], vcc[:, nb, hk, 0:129], ["vcc", "vcc1"], biases))
                    attend(m, hk, visits, o_started)
                finalize(m, 0, True)
                o_started = set()
                for hk in range(2):
                    visits = []
                    for jj in range(nwin0, 8):
                        biases = [(idB, cbw[:, jj, :, :].rearrange("p g q -> p (g q)"), ["identB", "cbw"])]
                        visits.append((kwT[qsl][:, hk, jj * 128:(jj + 1) * 128], [("kwT", qsl)], vwA[qsl][:, jj, hk, 0:129],
                                       [("vwA", qsl)], biases))
                    attend(m, hk, visits, o_started)
                finalize(m, 2, False)
                o_started = set()
                for hk in range(2):
                    visits = []
                    for kb in range(4 * m + 4):
                        biases = [(Eall[:, kb, :], sbT[hk].rearrange("p g q -> p (g q)"), [("Eall", kb // 16), ("sbT", hk)])]
                        if kb >= 4 * m:
                            biases.append((idB, cbs[:, kb - 4 * m, :, :].rearrange("p g q -> p (g q)"), ["identB", "cbs"]))
                        visits.append((ksT[:, hk, kb * 128:(kb + 1) * 128], [("ksT", hk)], vsA[:, kb, hk, 0:129],
                                       [("vsA", kb // 16)], biases))
                    attend(m, hk, visits, o_started)
                finalize(m, 1, False)
                yflat = yat.rearrange("p h x -> p (h x)")
                rs, krs = rms_rstd(yflat, 1024, 16, "yat")
                vstt(yan, yflat, rs, gattn, ALU.mult, ALU.mult, ["yat", krs, "gattn"], ["yan"])
                pty = bank_bf(7).rearrange("p a (c t) -> p (a c) t", t=128)
                for c in range(8):
                    tr(pty[:, c, :], yan[:, c * 128:(c + 1) * 128], identB[:], ["yan", "identB"], [("ps", 7)])
                act(ymT2[qsl], pty, AF.Copy, [("ps", 7)], [("ymT2", qsl)])
                dma("sync", ymixT[m, :, 8:16, :], ymT2[qsl], [("ymT2", qsl)], [("ymixT", m, 1)], f"ymT2{qsl}")
            P.barrier()
            if KSTOP == "B":
                break

            AR.reset()
            gbuf = AR.alloc([128, 2048], F32)
            ymx = [AR.alloc([128, 16, 128], BF16) for _ in range(2)]
            hh_ = AR.alloc([128, 4, 2048], F32)
            xb = AR.alloc([128, 2048], BF16)
            fT = AR.alloc([128, 16, 512], BF16)
            actT = AR.alloc([128, 44, 512], BF16)
            wst = [AR.alloc([128, 16, 256], BF16) for _ in range(3)]
            stg = [AR.alloc([128, 2816], F32) for _ in range(2)]
            wdn = [AR.alloc([128, 11, 256], BF16) for _ in range(2)]
            sg = [AR.alloc([128, 512], F32) for _ in range(2)]
            wc2 = [0]

            cst = [0]

            def load_cast(dst, src, kdst, a, n):
                i = cst[0] % 2
                cst[0] += 1
                st = stg[i][:, 0:a * n].rearrange("p (a n) -> p a n", a=a)
                dma("sync", st, src, (), [("stg", i)], f"stg{i}")
                if i == 0:
                    vcopy(dst, st, [("stg", i)], [kdst])
                else:
                    act(dst, st, AF.Copy, [("stg", i)], [kdst])

            def next_w2(src_cols):
                i = wc2[0] % 3
                wc2[0] += 1
                sv = src_cols.rearrange("(kc p) n -> p kc n", p=128)
                for hf_ in range(2):
                    load_cast(wst[i][:, hf_ * 8:(hf_ + 1) * 8, :], sv[:, hf_ * 8:(hf_ + 1) * 8, :], ("wst", i), 8, 256)
                return wst[i], ("wst", i)

            dctr = [0]
            octr = [0]
            for tt in range(4):
                for tb in range(4):
                    m = tt * 4 + tb
                    dma("sync", hh_[:, tb, :], xown[m * 128:(m + 1) * 128, :], (), [("hh", tb)], f"hh{tb}")
                for oc in range(8):
                    Wo, kWo = next_w2(w_out[:, oc * 256:(oc + 1) * 256])
                    for tb in range(4):
                        m = tt * 4 + tb
                        sl = tb % 2
                        dma("sync", ymx[sl], ymixT[m], (), [("ymx", sl)], f"ymx{sl}")
                        ob = octr[0] % 2
                        octr[0] += 1
                        for k in range(16):
                            mm(ps[:, ob, 0:256], ymx[sl][:, k, :], Wo[:, k, :], k == 0, k == 15, [("ymx", sl), kWo], [("ps", ob)])
                        vtt(hh_[:, tb, oc * 256:(oc + 1) * 256], ps[:, ob, 0:256], hh_[:, tb, oc * 256:(oc + 1) * 256], ALU.add,
                            [("ps", ob), ("hh", tb)], [("hh", tb)])
                bc_load(gbuf, g_ffn, "gbuf", "c_gbuf")
                for tb in range(4):
                    norm_transpose(hh_[:, tb, :], ("hh", tb), gbuf, "gbuf", xb, "xb", 2 if tb % 2 == 0 else 4,
                                   fT[:, :, tb * 128:(tb + 1) * 128], ("fT", tb), 20)
                fkeys = [("fT", tb) for tb in range(4)]
                for hp in range(22):
                    Wg, kWg = next_w2(w_gate[:, hp * 256:(hp + 1) * 256])
                    Wu, kWu = next_w2(w_up[:, hp * 256:(hp + 1) * 256])
                    for c2 in range(2):
                        hc = hp * 2 + c2
                        gb = 0 + 2 * (hc % 2)
                        ub = gb + 1
                        for k in range(16):
                            mm(bank(gb), Wg[:, k, c2 * 128:(c2 + 1) * 128], fT[:, k, :], k == 0, k == 15, [kWg] + fkeys, [("ps", gb)])
                        for k in range(16):
                            mm(bank(ub), Wu[:, k, c2 * 128:(c2 + 1) * 128], fT[:, k, :], k == 0, k == 15, [kWu] + fkeys, [("ps", ub)])
                        act(sg[hc % 2], bank(gb), AF.Silu, [("ps", gb)], [("sg", hc % 2)])
                        vtt(actT[:, hc, :], bank(ub), sg[hc % 2], ALU.mult, [("ps", ub), ("sg", hc % 2)], [("actT", hc)])
                wdv = w_down.rearrange("(hc p) n -> p hc n", p=128)
                for cg in range(8):
                    for hq in range(4):
                        di = dctr[0] % 2
                        dctr[0] += 1
                        load_cast(wdn[di], wdv[:, hq * 11:(hq + 1) * 11, cg * 256:(cg + 1) * 256], ("wdn", di), 11, 256)
                        for tb in range(4):
                            for c in range(11):
                                hc = hq * 11 + c
                                mm(ps[:, 4 + tb, 0:256], actT[:, hc, tb * 128:(tb + 1) * 128], wdn[di][:, c, :], hc == 0, hc == 43,
                                   [("actT", hc), ("wdn", di)], [("ps", 4 + tb)])
                    for tb in range(4):
                        vtt(hh_[:, tb, cg * 256:(cg + 1) * 256], ps[:, 4 + tb, 0:256], hh_[:, tb, cg * 256:(cg + 1) * 256], ALU.add,
                            [("ps", 4 + tb), ("hh", tb)], [("hh", tb)])
                bc_load(gbuf, g_fin, "gbuf", "c_gbuf")
                for tb in range(4):
                    m = tt * 4 + tb
                    rs, krs = rms_rstd(hh_[:, tb, :], 2048, 24, ("hh", tb))
                    vstt(hh_[:, tb, :], hh_[:, tb, :], rs, gbuf, ALU.mult, ALU.mult, [("hh", tb), krs, "gbuf"], [("hh", tb)])
                    dma("sync", y[m * 128:(m + 1) * 128, :], hh_[:, tb, :], [("hh", tb)], [("y", m)], f"yout{tb}")
            o_ = P.op("sync", lambda e: e.nop(), [("y", m) for m in range(16)], ())
            o_.is_nop = True
            P.barrier()
        P.emit(nc, ctx)
    return nc


def _tables(j):
    f32 = np.float32
    k = np.arange(128)[:, None]
    q = np.arange(128)[None, :]
    cb_sel = np.zeros((128, 4, 128), f32)
    for jj in range(4):
        if jj < j:
            v = np.ones((128, 128), bool)
        elif jj == j:
            v = k <= q
        else:
            v = np.zeros((128, 128), bool)
        cb_sel[:, jj, :] = np.where(v, 0.0, NEG)
    cb_win = np.zeros((128, 8, 128), f32)
    for jj in range(8):
        delta = 128 * (j + 4 - jj) + q - k
        cb_win[:, jj, :] = np.where((delta >= 0) & (delta < 512), 0.0, NEG)
    cb_cmp = np.zeros((16, 128, 2, 128), f32)
    cbT_cmp = np.zeros((16, 128, 64), f32)
    for m in range(16):
        t = 128 * (4 * m + j) + np.arange(128)
        NBc = m // 4 + 1
        for w in range(2):
            nb = NBc - 2 + w
            n = 128 * nb + np.arange(128)
            valid = (16 * n[:, None] + 31 <= t[None, :]) & (n[:, None] >= 0)
            cb_cmp[m, :, w, :] = np.where(valid, 0.0, NEG)
        n = 32 * m - 32 + np.arange(64)
        valid = 16 * n[None, :] + 31 <= t[:, None]
        cbT_cmp[m] = np.where(valid, 0.0, NEG)
    qq = np.arange(128)[:, None]
    c = np.arange(9)[None, :]
    rel = c - 1 - 2 * j - (qq >= 64)
    sel_force = np.where((rel == 0) | (rel == -1), 10.0, -1e9).astype(f32)
    sel_future = np.where(rel > 0, -1.0, 1e9).astype(f32)
    return dict(cb_sel=cb_sel.reshape(128, 512), cb_win=cb_win.reshape(128, 1024),
                cb_cmp=cb_cmp.reshape(16, 128, 256), cbT_cmp=cbT_cmp, sel_force=sel_force, sel_future=sel_future)


def _shared_tables():
    f32 = np.float32
    half = 64
    inv = 1.0 / (10000.0 ** (np.arange(half, dtype=np.float32) / half))
    ang = np.arange(S, dtype=np.float32)[:, None] * inv[None, :]
    cs_all = np.concatenate([np.cos(ang), np.sin(ang)], axis=1).astype(f32)
    s = np.arange(128)[:, None, None]
    kb = np.arange(64)[None, :, None]
    kk = np.arange(128)[None, None, :]
    E_all = (s == 2 * kb + kk // 64).astype(f32).reshape(128, 64 * 128)
    kr = np.arange(128)[:, None]
    mc = np.arange(128)[None, :]
    Sh = np.stack([(kr == mc - 1), (kr == mc - 2)], axis=1).astype(f32).reshape(128, 256)
    Hh = np.zeros((32, 272), f32)
    r = np.arange(32)
    Hh[r % 2 == 1, 0] = 1.0
    Hh[r % 2 == 0, 128 + 0] = 1.0
    Hh[r % 2 == 1, 128 + 1] = 1.0
    for m in range(16):
        Hh[2 * m:2 * m + 2, 256 + m] = 1.0
    return dict(cs_all=cs_all, E_all=E_all, Sh=Sh, Hh=Hh, ident=np.eye(128, dtype=f32))


_NC_CACHE = {}


def kernel(**inputs):
    x = np.asarray(inputs["x"], dtype=np.float32)
    sh = _shared_tables()
    base = {
        "w_in": np.ascontiguousarray(inputs["w_in"][0]),
        "w_out": np.ascontiguousarray(inputs["w_out"][0]),
        "w_gate": np.ascontiguousarray(inputs["w_gate"][0]),
        "w_up": np.ascontiguousarray(inputs["w_up"][0]),
        "w_down": np.ascontiguousarray(inputs["w_down"][0]),
        "conv_w": np.ascontiguousarray(inputs["conv_w"][0]),
        "cmp_pe_k": np.ascontiguousarray(inputs["cmp_pe_k"][0]),
        "cmp_w1_k": np.ascontiguousarray(inputs["cmp_w1_k"][0]),
        "cmp_w2_k": np.ascontiguousarray(inputs["cmp_w2_k"][0]),
        "cmp_pe_v": np.ascontiguousarray(inputs["cmp_pe_v"][0]),
        "cmp_w1_v": np.ascontiguousarray(inputs["cmp_w1_v"][0]),
        "cmp_w2_v": np.ascontiguousarray(inputs["cmp_w2_v"][0]),
        "norm_mix": np.ascontiguousarray(inputs["norm_mix"][0]),
        "norm_conv_out": np.ascontiguousarray(inputs["norm_conv_out"][0]),
        "norm_attn_out": np.ascontiguousarray(inputs["norm_attn_out"][0]),
        "norm_ffn": np.ascontiguousarray(inputs["norm_ffn"][0]),
        "norm_final": np.ascontiguousarray(inputs["norm_final"]),
    }
    base = {k: np.asarray(v, dtype=np.float32) for k, v in base.items()}
    base.update(sh)
    in_maps = []
    for c in range(8):
        b, j = c // 4, c % 4
        blocks = [4 * m + j for m in range(16)]
        xb_ = x[b]
        xown = np.concatenate([xb_[128 * qb:128 * qb + 128] for qb in blocks], axis=0)
        xhalo = np.zeros((32, D), np.float32)
        for m, qb in enumerate(blocks):
            if qb > 0:
                xhalo[2 * m:2 * m + 2] = xb_[128 * qb - 2:128 * qb]
        cs_own = np.concatenate([sh["cs_all"][128 * qb:128 * qb + 128] for qb in blocks], axis=0)
        im = dict(base)
        im.update(_tables(j))
        im.update(xall=np.ascontiguousarray(xb_[:KS]), xown=np.ascontiguousarray(xown), xhalo=xhalo,
                  cs_own=np.ascontiguousarray(cs_own))
        im["cs_all"] = np.ascontiguousarray(im["cs_all"][:KS])
        if SHRINK_KEEP is not None:
            im = {k: (v if k in SHRINK_KEEP else np.zeros((1, 1), np.float32)) for k, v in im.items()}
        in_maps.append(im)
    if "nc" not in _NC_CACHE:
        _NC_CACHE["nc"] = build_nc()
    nc = _NC_CACHE["nc"]
    res = run_bass_kernel_spmd(nc, in_maps[:KCORES], core_ids=list(range(KCORES)))
    out = np.zeros((2, S, D), np.float32)
    for c in range(KCORES):
        b, j = c // 4, c % 4
        yv = res.results[c]["y"]
        for m in range(16):
            qb = 4 * m + j
            out[b, 128 * qb:128 * qb + 128] = yv[128 * m:128 * m + 128]
    if DEBUG:
        kernel.last_results = res.results
    return out
```

```python
import os
import numpy as np
import concourse.bass as bass
import concourse.mybir as mybir
from concourse.bass_utils import run_bass_kernel_spmd
from contextlib import ExitStack

F32 = mybir.dt.float32
BF16 = mybir.dt.bfloat16
U8 = mybir.dt.uint8
AF = mybir.ActivationFunctionType
ALU = mybir.AluOpType
AX = mybir.AxisListType

D = 2048
S = 8192
DP = 5656
FF = 5632
NEG = -30000.0
SCALE = 128 ** -0.5
EPS = 1e-6
ENGS = ["tensor", "vector", "scalar", "gpsimd", "sync"]
DEBUG = bool(int(os.environ.get("KDEBUG", "0")))
KSTOP = os.environ.get("KSTOP", "")
KCORES = int(os.environ.get("KCORES", "8"))
KS = int(os.environ.get("KS", "8192"))
KSKIP = set(os.environ.get("KSKIP", "").split(","))
_UA = {"xall", "w_in", "norm_mix", "cs_all", "ident"}
_UA2 = _UA | {"cmp_pe_k", "cmp_w1_k", "cmp_w2_k", "cmp_pe_v", "cmp_w1_v", "cmp_w2_v"}
_U0 = _UA2 | {"xown", "xhalo", "conv_w", "norm_conv_out", "cs_own", "Sh", "Hh"}
_UB = _U0 | {"cb_sel", "cb_win", "cb_cmp", "cbT_cmp", "sel_force", "sel_future", "E_all", "norm_attn_out"}
_USED = {"A": _UA, "A2": _UA2, "0": _U0, "B": _UB}
SHRINK_KEEP = _USED.get(KSTOP)


class Op:
    __slots__ = ("eng", "fn", "deps", "needs_inc", "count", "grp", "is_nop")


class DmaGroup:
    __slots__ = ("stream", "n", "final", "last")


class Prog:
    def __init__(self):
        self.q = {e: [] for e in ENGS}
        self.keys = {}
        self.streams = {}

    def dma_group(self, stream):
        g = DmaGroup()
        g.stream = stream
        g.n = 0
        g.final = None
        g.last = None
        self.streams.setdefault(stream, []).append(g)
        return g

    def op(self, eng, fn, reads=(), writes=(), grp=None, extra=()):
        o = Op()
        o.eng = eng
        o.fn = fn
        o.needs_inc = False
        o.count = None
        o.grp = grp
        o.is_nop = False
        if grp is not None:
            grp.n += 1
            grp.last = o
        ident = eng if grp is None else ("dma", id(grp))
        deps = set(extra)
        writes = list(writes) + [k for k in reads if isinstance(k, tuple) and k[0] == "ps" and k not in writes]
        reads = [k for k in reads if not (isinstance(k, tuple) and k[0] == "ps")]
        for k in reads:
            st = self.keys.setdefault(k, ({}, {}))
            deps.update(st[0].values())
            st[1][ident] = o
        for k in writes:
            st = self.keys.setdefault(k, ({}, {}))
            deps.update(st[0].values())
            deps.update(st[1].values())
            if st[1]:
                st[0].clear()
                st[1].clear()
            st[0][ident] = o
        deps.discard(o)
        o.deps = deps
        self.q[eng].append(o)
        return o

    def barrier(self):
        arr = []
        for e in ENGS:
            for o in reversed(self.q[e]):
                if o.grp is None and not getattr(o, "is_nop", False):
                    arr.append(o)
                    break
        lasts = [g[-1].last for g in self.streams.values() if g and g[-1].last is not None]
        for e in ENGS:
            o = self.op(e, lambda eng: eng.nop(), extra=arr + lasts)
            o.is_nop = True
        self.keys = {}

    def emit(self, nc, ctx):
        engsem = {e: ctx.enter_context(nc.semaphore("es_" + e)) for e in ENGS}
        ssem = {}
        for s, groups in self.streams.items():
            ssem[s] = ctx.enter_context(nc.semaphore("ds_" + str(s)))
            cum = 0
            for g in groups:
                cum += 16 * g.n
                g.final = cum
        for e in ENGS:
            for o in self.q[e]:
                for d in o.deps:
                    if d.grp is None and not (d.eng == e and e == "tensor"):
                        d.needs_inc = True
        for e in ENGS:
            c = 0
            for o in self.q[e]:
                if o.grp is None and o.needs_inc:
                    c += 1
                    o.count = c
        block = ctx.enter_context(nc.Block())
        prog = self

        def run(e):
            def body(eng):
                waited = {}
                for o in prog.q[e]:
                    needs = {}
                    for d in o.deps:
                        if d.grp is not None:
                            s, v, key = ssem[d.grp.stream], d.grp.final, ("s", d.grp.stream)
                        else:
                            if d.eng == e and e == "tensor":
                                continue
                            s, v, key = engsem[d.eng], d.count, ("e", d.eng)
                        if key not in needs or needs[key][1] < v:
                            needs[key] = (s, v)
                    for key, (s, v) in needs.items():
                        if waited.get(key, 0) < v:
                            eng.wait_ge(s, v)
                            waited[key] = v
                    ins = o.fn(eng)
                    if o.grp is not None:
                        ins.then_inc(ssem[o.grp.stream], 16)
                    elif o.needs_inc:
                        ins.then_inc(engsem[e], 1)
            return body

        block.tensor(run("tensor"))
        block.vector(run("vector"))
        block.scalar(run("scalar"))
        block.gpsimd(run("gpsimd"))
        block.sync(run("sync"))


class Arena:
    def __init__(self, t, size):
        self.t = t
        self.size = size
        self.off = 0

    def reset(self):
        self.off = 0

    def alloc(self, shape, dt):
        esz = 4 if dt == F32 else (2 if dt == BF16 else 1)
        n = 1
        for s in shape[1:]:
            n *= s
        nb = (n * esz + 63) // 64 * 64
        assert self.off + nb <= self.size, ("arena overflow", self.off, nb, self.size)
        ap = self.t[0:shape[0], self.off:self.off + n * esz].bitcast(dt)
        self.off += nb
        if len(shape) == 3:
            ap = ap.rearrange("p (a b) -> p a b", a=shape[1])
        elif len(shape) == 4:
            ap = ap.rearrange("p (a b c) -> p a b c", a=shape[1], b=shape[2])
        elif len(shape) == 5:
            ap = ap.rearrange("p (a b c d) -> p a b c d", a=shape[1], b=shape[2], c=shape[3])
        return ap


def build_nc():
    nc = bass.Bass("TRN2", target_bir_lowering=False)

    def din(name, shape):
        if SHRINK_KEEP is not None and name not in SHRINK_KEEP:
            shape = [1, 1]
        return nc.dram_tensor(name, list(shape), F32, kind="ExternalInput").ap()

    xall = din("xall", [KS, D])
    xown = din("xown", [2048, D])
    xhalo = din("xhalo", [32, D])
    w_in = din("w_in", [D, DP])
    w_out = din("w_out", [D, D])
    w_gate = din("w_gate", [D, FF])
    w_up = din("w_up", [D, FF])
    w_down = din("w_down", [FF, D])
    conv_w = din("conv_w", [3, 1024])
    pe_k = din("cmp_pe_k", [32, 128])
    w1_k = din("cmp_w1_k", [4096, 128])
    w2_k = din("cmp_w2_k", [128, 128])
    pe_v = din("cmp_pe_v", [32, 128])
    w1_v = din("cmp_w1_v", [4096, 128])
    w2_v = din("cmp_w2_v", [128, 128])
    g_mix = din("norm_mix", [D])
    g_conv = din("norm_conv_out", [1024])
    g_attn = din("norm_attn_out", [1024])
    g_ffn = din("norm_ffn", [D])
    g_fin = din("norm_final", [D])
    cs_all = din("cs_all", [KS, 128])
    cs_own = din("cs_own", [2048, 128])
    cb_sel_d = din("cb_sel", [128, 4 * 128])
    cb_win_d = din("cb_win", [128, 8 * 128])
    cb_cmp_d = din("cb_cmp", [16, 128, 2 * 128])
    cbT_cmp_d = din("cbT_cmp", [16, 128, 64])
    self_force_d = din("sel_force", [128, 9])
    self_future_d = din("sel_future", [128, 9])
    E_all_d = din("E_all", [128, 64 * 128])
    Sh_d = din("Sh", [128, 2 * 128])
    Hh_d = din("Hh", [32, 272])
    ident_d = din("ident", [128, 128])
    y = nc.dram_tensor("y", [2048, D], F32, kind="ExternalOutput").ap()

    skind = dict(kind="ExternalOutput") if DEBUG else {}
    KTs = nc.dram_tensor("KTs", [4, 2, 128, KS], BF16, **skind).ap()
    VSc = nc.dram_tensor("VSc", [2, KS, 2, 130], BF16, **skind).ap()
    qTs = nc.dram_tensor("qTs", [16, 128, 8, 128], BF16, **skind).ap()
    ymixT = nc.dram_tensor("ymixT", [16, 128, 16, 128], BF16, **skind).ap()
    if DEBUG:
        dbg_kc = nc.dram_tensor("dbg_kc", [128, 2 * 512], BF16, kind="ExternalOutput").ap()
        dbg_vc = nc.dram_tensor("dbg_vc", [128, 4 * 2 * 130], BF16, kind="ExternalOutput").ap()
        dbg_gates = nc.dram_tensor("dbg_gates", [128, 16 * 24], F32, kind="ExternalOutput").ap()

    ctx = ExitStack()
    with ctx:
        P = Prog()
        AR_SIZE = 184 * 1024
        arena_t = ctx.enter_context(nc.sbuf_tensor("arena", [128, AR_SIZE], U8))
        AR = Arena(arena_t, AR_SIZE)

        def sbt(name, shape, dt):
            return ctx.enter_context(nc.sbuf_tensor(name, shape, dt))

        ps = ctx.enter_context(nc.psum_tensor("ps", [128, 8, 512], F32))

        def bank(i):
            return ps[:, i, :]

        def bank_bf(i, n=1):
            return ps[:, i:i + n, :].bitcast(BF16)

        identF = sbt("identF", [128, 128], F32)
        identB = sbt("identB", [128, 128], BF16)
        eps_t = sbt("eps_t", [128, 1], F32)
        kcT = sbt("kcT", [128, 2, 512], BF16)
        vcc = sbt("vcc", [128, 4, 2, 130], BF16)
        gates = sbt("gates", [128, 16, 24], F32)
        u_halo = sbt("u_halo", [32, 1024], F32)
        aT_halo = sbt("aT_halo", [128, 16, 32], BF16)
        junk = sbt("junk", [128, 2048], BF16)
        stat = sbt("stat", [128, 64], F32)

        def dma(queue, out, in_, reads, writes, stream, grp=None):
            g = grp if grp is not None else P.dma_group(stream)
            P.op(queue, lambda e: e.dma_start(out=out, in_=in_), reads, writes, grp=g)
            return g

        def mm(out, lhsT, rhs, start, stop, reads, writes, skip=False):
            P.op("tensor", lambda e: e.matmul(out, lhsT=lhsT, rhs=rhs, start=start, stop=stop,
                                              skip_group_check=skip), reads, writes)

        def tr(out, in_, ident, reads, writes):
            P.op("tensor", lambda e: e.transpose(out=out, in_=in_, identity=ident), reads, writes)

        def act(out, in_, func, reads, writes, **kw):
            P.op("scalar", lambda e: e.activation(out=out, in_=in_, func=func, **kw), reads, writes)

        def vtt(out, in0, in1, op, reads, writes):
            P.op("vector", lambda e: e.tensor_tensor(out=out, in0=in0, in1=in1, op=op), reads, writes)

        def vts(out, in0, s1, s2, op0, op1, reads, writes):
            if op1 is None:
                P.op("vector", lambda e: e.tensor_scalar(out=out, in0=in0, scalar1=s1, scalar2=None, op0=op0),
                     reads, writes)
            else:
                P.op("vector", lambda e: e.tensor_scalar(out=out, in0=in0, scalar1=s1, scalar2=s2, op0=op0, op1=op1),
                     reads, writes)

        def vstt(out, in0, scalar, in1, op0, op1, reads, writes):
            P.op("vector", lambda e: e.scalar_tensor_tensor(out=out, in0=in0, scalar=scalar, in1=in1,
                                                            op0=op0, op1=op1), reads, writes)

        def vcopy(out, in_, reads, writes):
            P.op("vector", lambda e: e.tensor_copy(out=out, in_=in_), reads, writes)

        def vrecip(out, in_, reads, writes):
            P.op("vector", lambda e: e.reciprocal(out=out, in_=in_), reads, writes)

        def vmemset(ap, val, writes):
            P.op("vector", lambda e: e.memset(ap, val), (), writes)

        def bc_load(dst, src_row, key, stream):
            dma("sync", dst, src_row.partition_broadcast(128), (), [key], stream)

        def rms_rstd(src, n, col, kin, np_=128):
            ss = stat[0:np_, col:col + 1]
            rt = stat[0:np_, col + 1:col + 2]
            rs = stat[0:np_, col + 2:col + 3]
            act(junk[0:np_, 0:n], src, AF.Square, [kin], ["junk", ("stat", col)], accum_out=ss)
            act(rt, ss, AF.Sqrt, [("stat", col)], [("stat", col + 1)], scale=1.0 / n, bias=eps_t[0:np_, :])
            vrecip(rs, rt, [("stat", col + 1)], [("stat", col + 2)])
            return rs, ("stat", col + 2)

        def norm_transpose(src, ksrc, gain_bc, kgain, xb, kxb, pbank, dst, kdst, col, np_=128):
            rs, krs = rms_rstd(src, 2048, col, ksrc, np_)
            vstt(xb[0:np_, :], src, rs, gain_bc[0:np_, :], ALU.mult, ALU.mult, [ksrc, krs, kgain], [kxb])
            pt = bank_bf(pbank, 2).rearrange("p a (c t) -> p (a c) t", t=128)
            idn = identB[0:np_, 0:np_]
            for k in range(16):
                tr(pt[:, k, 0:np_], xb[0:np_, k * 128:(k + 1) * 128], idn, [kxb, "identB"],
                   [("ps", pbank), ("ps", pbank + 1)])
            act(dst, pt[:, :, 0:np_], AF.Copy, [("ps", pbank), ("ps", pbank + 1)], [kdst])

        def load_w(dst, src_cols, key, stream):
            dma("gpsimd", dst, src_cols.rearrange("(kc p) n -> p kc n", p=128), (), [key], stream)

        dma("sync", identF[:], ident_d, (), ["identF"], "c_id")
        vcopy(identB[:], identF[:], ["identF"], ["identB"])
        vmemset(eps_t[:], EPS, ["eps"])
        P.barrier()
        for _once in (0,):

            AR.reset()
            wkv = AR.alloc([128, 16, 1536], BF16)
            gmix = AR.alloc([128, 2048], F32)
            xs = [AR.alloc([128, 2048], F32) for _ in range(2)]
            xb = AR.alloc([128, 2048], BF16)
            aT = [AR.alloc([128, 16, 128], BF16) for _ in range(2)]
            cs = [AR.alloc([128, 2, 64], F32) for _ in range(2)]
            rt1 = AR.alloc([128, 3, 2, 64], F32)
            rt2 = AR.alloc([128, 3, 2, 64], F32)
            krope = AR.alloc([128, 3, 2, 2, 64], BF16)
            vcb = AR.alloc([128, 2, 128], BF16)
            vst = [AR.alloc([128, 2, 2, 130], BF16) for _ in range(2)]
            kts = [AR.alloc([128, 8, 128], BF16) for _ in range(2)]

            for g in range(3):
                load_w(wkv[:, :, g * 512:(g + 1) * 512], w_in[:, 4096 + g * 512:4096 + (g + 1) * 512], ("wkv", g), f"wkv{g}")
            bc_load(gmix, g_mix, "gmix", "c_gmix")
            for s_ in range(2):
                if "ms" not in KSKIP:
                    vmemset(vst[s_][:, :, :, 128:130], 1.0, [("vst1", s_)])
            NB_A = int(os.environ.get("KNBA", "64"))
            dma("sync", xs[0], xall[0:128, :], (), [("xs", 0)], "xs0")
            if NB_A > 1:
                dma("sync", xs[1], xall[128:256, :], (), [("xs", 1)], "xs1")
            for i in range(NB_A):
                sl = i % 2
                dma("sync", cs[sl], cs_all[i * 128:(i + 1) * 128, :].rearrange("p (a b) -> p a b", a=2), (), [("cs", sl)], f"cs{sl}")
                if i == 0:
                    norm_transpose(xs[0], ("xs", 0), gmix, "gmix", xb, "xb", 0, aT[0], ("aT", 0), 0)
                for g in range(3 if "mm" not in KSKIP else 0):
                    for k in range(16):
                        mm(bank(2 + g), aT[sl][:, k, :], wkv[:, k, g * 512:(g + 1) * 512], k == 0, k == 15,
                           [("aT", sl), ("wkv", g)], [("ps", 2 + g)])
                if i + 1 < NB_A:
                    norm_transpose(xs[1 - sl], ("xs", 1 - sl), gmix, "gmix", xb, "xb", 6 if sl == 0 else 0,
                                   aT[1 - sl], ("aT", 1 - sl), 0)
                    if i + 2 < NB_A:
                        dma("sync", xs[sl], xall[(i + 2) * 128:(i + 3) * 128, :], (), [("xs", sl)], f"xs{sl}")
                if "rope" not in KSKIP:
                    z = ps[:, 2:5, 0:256].rearrange("p t (h f x) -> p t h f x", h=2, f=2)
                    cosb = cs[sl][:, 0, :].unsqueeze(1).unsqueeze(1).to_broadcast([128, 3, 2, 64])
                    sinb = cs[sl][:, 1, :].unsqueeze(1).unsqueeze(1).to_broadcast([128, 3, 2, 64])
                    pk = [("ps", 2), ("ps", 3), ("ps", 4)]
                    vtt(rt1, z[:, :, :, 0, :], cosb, ALU.mult, pk + [("cs", sl)], ["rt1"])
                    vtt(rt2, z[:, :, :, 1, :], sinb, ALU.mult, pk + [("cs", sl)], ["rt2"])
                    vtt(krope[:, :, :, 0, :], rt1, rt2, ALU.subtract, ["rt1", "rt2"], ["krope0"])
                    vtt(rt1, z[:, :, :, 1, :], cosb, ALU.mult, pk + [("cs", sl)], ["rt1"])
                    vtt(rt2, z[:, :, :, 0, :], sinb, ALU.mult, pk + [("cs", sl)], ["rt2"])
                    vtt(krope[:, :, :, 1, :], rt1, rt2, ALU.add, ["rt1", "rt2"], ["krope1"])
                if "vcopy" not in KSKIP:
                    act(vcb, ps[:, 2, 256:512].rearrange("p (h x) -> p h x", h=2), AF.Copy, [("ps", 2)], ["vcb"])
                    for t_ in range(2):
                        act(vst[sl][:, t_, :, 0:128], ps[:, 3 + t_, 256:512].rearrange("p (h x) -> p h x", h=2), AF.Copy,
                            [("ps", 3 + t_)], [("vst", sl)])
                if "tr5" not in KSKIP:
                    pk5 = bank_bf(5).rearrange("p a (c t) -> p (a c) t", t=128)
                    for t in range(3):
                        for hk in range(2):
                            tr(pk5[:, t * 2 + hk, :], krope[:, t, hk, :, :].rearrange("p f x -> p (f x)"), identB[:],
                               ["krope0", "krope1", "identB"], [("ps", 5)])
                    for hk in range(2):
                        tr(pk5[:, 6 + hk, :], vcb[:, hk, :], identB[:], ["vcb", "identB"], [("ps", 5)])
                    vcopy(kts[sl], pk5, [("ps", 5)], [("kts", sl)])
                if "st" not in KSKIP:
                    dma("sync", KTs[:, :, :, i * 128:(i + 1) * 128].rearrange("t h d s -> d (t h) s"), kts[sl],
                        [("kts", sl)], [("KTs", i)], f"kst{sl}")
                    dma("sync", VSc[:, i * 128:(i + 1) * 128, :, :].rearrange("t s h c -> s t (h c)"),
                        vst[sl].rearrange("p t h c -> p t (h c)"), [("vst", sl), ("vst1", sl)], [("VSc", i)], f"vst{sl}")
            P.barrier()
            if KSTOP == "A":
                break

            AR.reset()
            w1b = [AR.alloc([128, 32, 128], BF16) for _ in range(2)]
            w2b = [AR.alloc([128, 128], BF16) for _ in range(2)]
            pe_f = [AR.alloc([32, 128], F32) for _ in range(2)]
            peT = [AR.alloc([128, 32], BF16) for _ in range(2)]
            c0 = [AR.alloc([128, 1], F32) for _ in range(2)]
            raw = [AR.alloc([128, S], BF16) for _ in range(2)]
            h1T = AR.alloc([128, 512], BF16)
            for ti, (w1d, w2d, ped) in enumerate([(w1_k, w2_k, pe_k), (w1_v, w2_v, pe_v)]):
                dma("gpsimd", w1b[ti], w1d.rearrange("(l d) e -> d l e", d=128), (), [("w1b", ti)], f"w1b{ti}")
                dma("gpsimd", w2b[ti], w2d, (), [("w2b", ti)], f"w2b{ti}")
                dma("sync", pe_f[ti], ped, (), [("pe_f", ti)], f"pef{ti}")
                tr(ps[:, 0, 0:32], pe_f[ti], identF[0:32, 0:32], [("pe_f", ti), "identF"], [("ps", 0)])
                vcopy(peT[ti], ps[:, 0, 0:32], [("ps", 0)], [("peT", ti)])
                for l in range(32):
                    mm(ps[:, 1, 0:1], w1b[ti][:, l, :], peT[ti][:, l:l + 1], l == 0, l == 31,
                       [("w1b", ti), ("peT", ti)], [("ps", 1)])
                vcopy(c0[ti], ps[:, 1, 0:1], [("ps", 1)], [("c0", ti)])
            vmemset(vcc[:, :, :, 128:130], 1.0, ["vcc1"])
            it = 0
            for ti in range(2):
                for hk in range(2):
                    sl = it % 2
                    it += 1
                    dma("sync", raw[sl], KTs[0 if ti == 0 else 3, hk, :, :], (), [("raw", sl)], f"raw{sl}")
                    r16 = raw[sl].rearrange("p (n s) -> p n s", s=16)
                    pb = 2 + sl
                    for l in range(32):
                        rhs = r16[:, 0:511, l] if l < 16 else r16[:, 1:512, l - 16]
                        mm(ps[:, pb, 0:511], w1b[ti][:, l, :], rhs, l == 0, l == 31, [("w1b", ti), ("raw", sl)], [("ps", pb)])
                    vmemset(h1T[:, 511:512], 0.0, ["h1T"])
                    act(h1T[:, 0:511], ps[:, pb, 0:511], AF.Silu, [("ps", pb), ("c0", ti)], ["h1T"], bias=c0[ti])
                    if ti == 0:
                        mm(bank(4), w2b[0], h1T, True, True, [("w2b", 0), "h1T"], [("ps", 4)])
                        vcopy(kcT[:, hk, :], bank(4), [("ps", 4)], ["kcT"])
                    else:
                        for nb in range(4):
                            mm(ps[:, 5, nb * 128:(nb + 1) * 128], h1T[:, nb * 128:(nb + 1) * 128], w2b[1], True, True,
                               [("w2b", 1), "h1T"], [("ps", 5)])
                        vcopy(vcc[:, :, hk, 0:128], ps[:, 5, :].rearrange("p (n x) -> p n x", n=4), [("ps", 5)], ["vcc"])
            if DEBUG:
                dma("sync", dbg_kc, kcT[:].rearrange("p a b -> p (a b)"), ["kcT"], ["dbg_kc"], "dbg1")
                dma("sync", dbg_vc, vcc[:].rearrange("p a b c -> p (a b c)"), ["vcc", "vcc1"], ["dbg_vc"], "dbg2")
            P.barrier()
            if KSTOP == "A2":
                break

            AR.reset()
            gmix = AR.alloc([128, 2048], F32)
            gconv = AR.alloc([128, 1024], F32)
            wcv = AR.alloc([128, 3, 1024], F32)
            shf = AR.alloc([128, 2, 128], F32)
            hfix = AR.alloc([32, 2, 128], F32)
            hmask = AR.alloc([32, 16], F32)
            uh = [AR.alloc([32, 256], F32) for _ in range(2)]
            xs = [AR.alloc([128, 2048], F32) for _ in range(2)]
            xb = AR.alloc([128, 2048], BF16)
            aTo = AR.alloc([128, 16, 1024], BF16)
            wch = [AR.alloc([128, 16, 256], BF16) for _ in range(6)]
            wgt = AR.alloc([128, 16, 24], BF16)
            ycb = AR.alloc([128, 8, 1024], BF16)
            cso = AR.alloc([128, 8, 2, 64], F32)
            ccs = [AR.alloc([128, 256], F32) for _ in range(2)]
            uu = [AR.alloc([128, 256], F32) for _ in range(2)]
            aa = [AR.alloc([128, 256], F32) for _ in range(2)]
            tb_ = [AR.alloc([128, 256], F32) for _ in range(2)]
            qr1 = AR.alloc([128, 2, 64], F32)
            qr2 = AR.alloc([128, 2, 64], F32)
            qrope = [AR.alloc([128, 2, 2, 64], BF16) for _ in range(2)]
            qts = [AR.alloc([128, 2, 128], BF16) for _ in range(2)]
            ycn = AR.alloc([128, 1024], BF16)
            ymT = [AR.alloc([128, 8, 128], BF16) for _ in range(2)]
            ssq = AR.alloc([128, 8, 4], F32)

            bc_load(gmix, g_mix, "gmix", "c_gmix")
            bc_load(gconv, g_conv, "gconv", "c_gconv")
            for kk in range(3):
                bc_load(wcv[:, kk, :], conv_w[kk, :], ("wcv", kk), f"c_wcv{kk}")
            dma("sync", shf, Sh_d.rearrange("p (a b) -> p a b", a=2), (), ["shf"], "c_sh")
            dma("sync", hfix, Hh_d[:, 0:256].rearrange("p (a b) -> p a b", a=2), (), ["hfix"], "c_hh")
            dma("sync", hmask, Hh_d[:, 256:272], (), ["hmask"], "c_hm")
            wcnt = [0]

            def next_w(src_cols, ncols=256):
                i = wcnt[0] % 6
                wcnt[0] += 1
                load_w(wch[i][:, :, 0:ncols], src_cols, ("wch", i), f"wch{i}")
                return wch[i], ("wch", i)

            for hf in range(2):
                if hf == 0:
                    dma("sync", xs[1][0:32, :], xhalo, (), [("xs", 1)], "xs1")
                    norm_transpose(xs[1][0:32, :], ("xs", 1), gmix, "gmix", xb, "xb", 0, aT_halo[:], "aT_halo", 0, np_=32)
                for mi in range(8):
                    m = hf * 8 + mi
                    sl = mi % 2
                    dma("sync", xs[sl], xown[m * 128:(m + 1) * 128, :], (), [("xs", sl)], f"xs{sl}")
                    norm_transpose(xs[sl], ("xs", sl), gmix, "gmix", xb, "xb", 0 if sl == 0 else 6,
                                   aTo[:, :, mi * 128:(mi + 1) * 128], ("aTo", mi), 0)
                dma("sync", cso, cs_own[hf * 1024:(hf + 1) * 1024, :].rearrange("(m p) (a b) -> p m a b", p=128, a=2),
                    (), ["cso"], "cso")
                for cg in range(4):
                    c0_, c1_ = cg * 256, (cg + 1) * 256
                    Wb, kWb = next_w(w_in[:, c0_:c1_])
                    Wc, kWc = next_w(w_in[:, 1024 + c0_:1024 + c1_])
                    Wh, kWh = next_w(w_in[:, 2048 + c0_:2048 + c1_])
                    if hf == 0:
                        for k in range(16):
                            mm(ps[0:32, 2, 0:256], aT_halo[:, k, :], Wc[:, k, :], k == 0, k == 15, ["aT_halo", kWc], [("ps", 2)])
                        for k in range(16):
                            mm(ps[0:32, 2, 256:512], aT_halo[:, k, :], Wh[:, k, :], k == 0, k == 15, ["aT_halo", kWh], [("ps", 2)])
                        act(ccs[0][0:32, :], ps[0:32, 2, 0:256], AF.Copy, [("ps", 2)], [("ccs", 0)])
                        vtt(u_halo[:, c0_:c1_], ps[0:32, 2, 256:512], ccs[0][0:32, :], ALU.mult, [("ps", 2), ("ccs", 0)], ["u_halo"])
                    for mi in range(8):
                        m = hf * 8 + mi
                        sl = mi % 2
                        bA, bB, bC = (2, 3, 4) if sl == 0 else (5, 6, 7)
                        at = aTo[:, :, mi * 128:(mi + 1) * 128]
                        for k in range(16):
                            mm(ps[:, bA, 0:256], at[:, k, :], Wc[:, k, :], k == 0, k == 15, [("aTo", mi), kWc], [("ps", bA)])
                        for k in range(16):
                            mm(ps[:, bA, 256:512], at[:, k, :], Wh[:, k, :], k == 0, k == 15, [("aTo", mi), kWh], [("ps", bA)])
                        for k in range(16):
                            mm(ps[:, bB, 0:256], at[:, k, :], Wb[:, k, :], k == 0, k == 15, [("aTo", mi), kWb], [("ps", bB)])
                        act(ccs[sl], ps[:, bA, 0:256], AF.Copy, [("ps", bA)], [("ccs", sl)])
                        vtt(uu[sl], ps[:, bA, 256:512], ccs[sl], ALU.mult, [("ps", bA), ("ccs", sl)], [("uu", sl)])
                        vts(uh[sl], u_halo[:, c0_:c1_], hmask[:, m:m + 1], None, ALU.mult, None, ["u_halo", "hmask"], [("uh", sl)])
                        mm(ps[:, bC, 0:256], shf[:, 0, :], uu[sl], True, False, ["shf", ("uu", sl)], [("ps", bC)])
                        mm(ps[:, bC, 0:256], hfix[:, 0, :], uh[sl], False, True, ["hfix", ("uh", sl)], [("ps", bC)])
                        mm(ps[:, bC, 256:512], shf[:, 1, :], uu[sl], True, False, ["shf", ("uu", sl)], [("ps", bC)])
                        mm(ps[:, bC, 256:512], hfix[:, 1, :], uh[sl], False, True, ["hfix", ("uh", sl)], [("ps", bC)])
                        vtt(aa[sl], uu[sl], wcv[:, 2, c0_:c1_], ALU.mult, [("uu", sl), ("wcv", 2)], [("aa", sl)])
                        vtt(tb_[sl], ps[:, bC, 0:256], wcv[:, 1, c0_:c1_], ALU.mult, [("ps", bC), ("wcv", 1)], [("tb", sl)])
                        vtt(aa[sl], aa[sl], tb_[sl], ALU.add, [("aa", sl), ("tb", sl)], [("aa", sl)])
                        vtt(tb_[sl], ps[:, bC, 256:512], wcv[:, 0, c0_:c1_], ALU.mult, [("ps", bC), ("wcv", 0)], [("tb", sl)])
                        vtt(aa[sl], aa[sl], tb_[sl], ALU.add, [("aa", sl), ("tb", sl)], [("aa", sl)])
                        vtt(ycb[:, mi, c0_:c1_], ps[:, bB, 0:256], aa[sl], ALU.mult, [("ps", bB), ("aa", sl)], [("ycb", mi, cg)])
                        act(junk[:, 0:256], ycb[:, mi, c0_:c1_], AF.Square, [("ycb", mi, cg)], ["junk", ("ssq", mi, cg)],
                            accum_out=ssq[:, mi, cg:cg + 1])
                for qc in range(4):
                    Wq, kWq = next_w(w_in[:, 3072 + qc * 256:3072 + (qc + 1) * 256])
                    for mi in range(8):
                        m = hf * 8 + mi
                        sl = mi % 2
                        bq = 2 if sl == 0 else 5
                        at = aTo[:, :, mi * 128:(mi + 1) * 128]
                        for k in range(16):
                            mm(ps[:, bq, 0:256], at[:, k, :], Wq[:, k, :], k == 0, k == 15, [("aTo", mi), kWq], [("ps", bq)])
                        z = ps[:, bq, 0:256].rearrange("p (h f x) -> p h f x", h=2, f=2)
                        cosb = cso[:, mi, 0, :].unsqueeze(1).to_broadcast([128, 2, 64])
                        sinb = cso[:, mi, 1, :].unsqueeze(1).to_broadcast([128, 2, 64])
                        kz = [("ps", bq), "cso"]
                        vtt(qr1, z[:, :, 0, :], cosb, ALU.mult, kz, ["qr1"])
                        vtt(qr2, z[:, :, 1, :], sinb, ALU.mult, kz, ["qr2"])
                        vtt(qrope[sl][:, :, 0, :], qr1, qr2, ALU.subtract, ["qr1", "qr2"], [("qrope0", sl)])
                        vtt(qr1, z[:, :, 1, :], cosb, ALU.mult, kz, ["qr1"])
                        vtt(qr2, z[:, :, 0, :], sinb, ALU.mult, kz, ["qr2"])
                        vtt(qrope[sl][:, :, 1, :], qr1, qr2, ALU.add, ["qr1", "qr2"], [("qrope1", sl)])
                        bt = 3 if sl == 0 else 6
                        ptq = bank_bf(bt)[:, 0, 0:256].rearrange("p (h t) -> p h t", h=2)
                        for hh in range(2):
                            tr(ptq[:, hh, :], qrope[sl][:, hh, :, :].rearrange("p f x -> p (f x)"), identB[:],
                               [("qrope0", sl), ("qrope1", sl), "identB"], [("ps", bt)])
                        act(qts[sl], ptq, AF.Copy, [("ps", bt)], [("qts", sl)])
                        dma("sync", qTs[m, :, qc * 2:qc * 2 + 2, :], qts[sl], [("qts", sl)], [("qTs", m, qc)], f"qts{sl}")
                load_w(wgt, w_in[:, 5632:5656], "wgt", "wgt")
                for mi in range(8):
                    m = hf * 8 + mi
                    bq = 4 if mi % 2 == 0 else 7
                    at = aTo[:, :, mi * 128:(mi + 1) * 128]
                    for k in range(16):
                        mm(ps[:, bq, 0:24], at[:, k, :], wgt[:, k, :], k == 0, k == 15, [("aTo", mi), "wgt"], [("ps", bq)])
                    act(gates[:, m, :], ps[:, bq, 0:24], AF.Sigmoid, [("ps", bq)], [("gates", m)])
                for mi in range(8):
                    m = hf * 8 + mi
                    sl = mi % 2
                    P.op("vector", lambda e, mi=mi: e.tensor_reduce(out=stat[:, 8:9], in_=ssq[:, mi, :], axis=AX.X, op=ALU.add),
                         [("ssq", mi, c) for c in range(4)], [("stat", 8)])
                    act(stat[:, 9:10], stat[:, 8:9], AF.Sqrt, [("stat", 8)], [("stat", 9)], scale=1.0 / 1024, bias=eps_t[:])
                    vrecip(stat[:, 10:11], stat[:, 9:10], [("stat", 9)], [("stat", 10)])
                    vstt(ycn, ycb[:, mi, :], stat[:, 10:11], gconv, ALU.mult, ALU.mult,
                         [("ycb", mi, c) for c in range(4)] + [("stat", 10), "gconv"], ["ycn"])
                    bt = 3 if sl == 0 else 6
                    pty = bank_bf(bt).rearrange("p a (c t) -> p (a c) t", t=128)
                    for c in range(8):
                        tr(pty[:, c, :], ycn[:, c * 128:(c + 1) * 128], identB[:], ["ycn", "identB"], [("ps", bt)])
                    act(ymT[sl], pty, AF.Copy, [("ps", bt)], [("ymT", sl)])
                    dma("sync", ymixT[m, :, 0:8, :], ymT[sl], [("ymT", sl)], [("ymixT", m, 0)], f"ymT{sl}")
            if DEBUG:
                dma("sync", dbg_gates, gates[:].rearrange("p a b -> p (a b)"), [("gates", m) for m in range(16)], ["dbg_g"], "dbg3")
            P.barrier()
            if KSTOP == "0":
                break

            AR.reset()
            ksT = AR.alloc([128, 2, S], BF16)
            vsA = AR.alloc([128, 64, 2, 130], BF16)
            Eall = AR.alloc([128, 64, 128], BF16)
            cbs = AR.alloc([128, 4, 4, 128], BF16)
            cbw = AR.alloc([128, 8, 4, 128], BF16)
            cbs_f = AR.alloc([128, 8, 128], F32)
            gattn = AR.alloc([128, 1024], F32)
            sfz = AR.alloc([128, 9], F32)
            suz = AR.alloc([128, 9], F32)
            qT = [AR.alloc([128, 8, 128], BF16) for _ in range(2)]
            kwT = [AR.alloc([128, 2, 1024], BF16) for _ in range(2)]
            vwA = [AR.alloc([128, 8, 2, 130], BF16) for _ in range(2)]
            cbc_f = [AR.alloc([128, 2, 128], F32) for _ in range(2)]
            cbc = [AR.alloc([128, 2, 4, 128], BF16) for _ in range(2)]
            cbT = [AR.alloc([128, 64], F32) for _ in range(2)]
            ee = [AR.alloc([128, 512], F32) for _ in range(2)]
            psum_ = AR.alloc([128, 512], F32)
            imp = AR.alloc([128, 128], F32)
            imp2 = AR.alloc([128, 128], F32)
            m8 = AR.alloc([128, 16], F32)
            selb = AR.alloc([128, 128], F32)
            sbT = [AR.alloc([128, 4, 128], BF16) for _ in range(2)]
            pT = [AR.alloc([128, 512], BF16) for _ in range(3)]
            yat = AR.alloc([128, 8, 128], F32)
            ytmp = AR.alloc([128, 8, 128], F32)
            coef = AR.alloc([128, 8], F32)
            dden = AR.alloc([128, 8], F32)
            yan = AR.alloc([128, 1024], BF16)
            ymT2 = [AR.alloc([128, 8, 128], BF16) for _ in range(2)]
            Dh = AR.alloc([128, 8], F32)

            for hk in range(2):
                dma("sync", ksT[:, hk, :], KTs[1, hk, :, :], (), [("ksT", hk)], f"ksT{hk}")
            for qd in range(4):
                dma("sync", vsA[:, qd * 16:(qd + 1) * 16, :, :].rearrange("p b h c -> p b (h c)"),
                    VSc[0, qd * 2048:(qd + 1) * 2048, :, :].rearrange("(b p) h c -> p b (h c)", p=128), (), [("vsA", qd)], f"vsA{qd}")
            for qd in range(8):
                dma("gpsimd", Eall[:, qd * 8:(qd + 1) * 8, :], E_all_d[:, qd * 1024:(qd + 1) * 1024].rearrange("p (a b) -> p a b", a=8),
                    (), [("Eall", qd // 2)], f"Eall{qd}")
            dma("sync", cbs_f[:, 0:4, :], cb_sel_d.rearrange("p (a b) -> p a b", a=4), (), ["cbs_f"], "c_cbs")
            vcopy(cbs, cbs_f[:, 0:4, :].unsqueeze(2).to_broadcast([128, 4, 4, 128]), ["cbs_f"], ["cbs"])
            dma("sync", cbs_f, cb_win_d.rearrange("p (a b) -> p a b", a=8), ["cbs_f"], ["cbs_f"], "c_cbs")
            vcopy(cbw, cbs_f.unsqueeze(2).to_broadcast([128, 8, 4, 128]), ["cbs_f"], ["cbw"])
            bc_load(gattn, g_attn, "gattn", "c_gattn")
            dma("sync", sfz, self_force_d, (), ["sfz"], "c_sfz")
            dma("sync", suz, self_future_d, (), ["suz"], "c_suz")
            vmemset(imp, -1.0, ["imp"])

            sctr = [0]
            pctr = [0]
            S_BANKS = [0, 1]
            O_BANK0 = 2

            def o_view(h):
                return ps[:, O_BANK0 + h // 2, (h % 2) * 256:(h % 2) * 256 + 129]

            def attend(m, hk, visits, o_started):
                qsl = m % 2
                qrhs = qT[qsl][:, 4 * hk:4 * hk + 4, :].rearrange("p g q -> p (g q)")
                def scores(vi):
                    kT_ap, kkeys, v_ap, vkeys, biases = visits[vi]
                    sb_ = S_BANKS[sctr[0] % 2]
                    sctr[0] += 1
                    nb = len(biases)
                    mm(bank(sb_), kT_ap, qrhs, True, nb == 0, kkeys + [("qT", qsl)], [("ps", sb_)])
                    for bi, (bl, br, bk) in enumerate(biases):
                        mm(bank(sb_), bl, br, False, bi == nb - 1, bk, [("ps", sb_)])
                    return sb_

                cur = scores(0)
                for vi in range(len(visits)):
                    kT_ap, kkeys, v_ap, vkeys, biases = visits[vi]
                    last = vi == len(visits) - 1
                    nxt = scores(vi + 1) if not last else None
                    sb_ = cur
                    pi = pctr[0] % 3
                    pctr[0] += 1
                    act(pT[pi], bank(sb_), AF.Exp, [("ps", sb_)], [("pT", pi)], scale=SCALE)
                    for g_ in range(4):
                        h = 4 * hk + g_
                        bnk = O_BANK0 + h // 2
                        st = bnk not in o_started
                        o_started.add(bnk)
                        mm(o_view(h), pT[pi][:, g_ * 128:(g_ + 1) * 128], v_ap, st, last, [("pT", pi)] + vkeys,
                           [("ps", bnk)], skip=True)
                    cur = nxt

            def finalize(m, br, first):
                ov = ps[:, O_BANK0:O_BANK0 + 4, :].rearrange("p b (h x) -> p (b h) x", h=2)
                okeys = [("ps", O_BANK0 + b_) for b_ in range(4)]
                vts(dden, ov[:, :, 128], 1e-30, None, ALU.max, None, okeys, ["dden"])
                vrecip(dden, dden, ["dden"], ["dden"])
                gv = gates[:, m, :].rearrange("p (h b) -> p h b", b=3)[:, :, br]
                vtt(coef, dden, gv, ALU.mult, ["dden", ("gates", m)], ["coef"])
                cb_ = coef[:, :].unsqueeze(2).to_broadcast([128, 8, 128])
                if first:
                    vtt(yat, ov[:, :, 0:128], cb_, ALU.mult, okeys + ["coef"], ["yat"])
                else:
                    vtt(ytmp, ov[:, :, 0:128], cb_, ALU.mult, okeys + ["coef"], ["ytmp"])
                    vtt(yat, yat, ytmp, ALU.add, ["yat", "ytmp"], ["yat"])

            def issue_loads(m):
                qsl = m % 2
                dma("sync", qT[qsl], qTs[m], (), [("qT", qsl)], f"qT{qsl}")
                t0 = 512 * (m - 1)
                for hk in range(2):
                    if m == 0:
                        dma("sync", kwT[qsl][:, hk, 512:1024], KTs[2, hk, :, 0:512], (), [("kwT", qsl)], f"kwT{qsl}")
                    else:
                        dma("sync", kwT[qsl][:, hk, :], KTs[2, hk, :, t0:t0 + 1024], (), [("kwT", qsl)], f"kwT{qsl}")
                if m == 0:
                    dma("sync", vwA[qsl][:, 4:8, :, :].rearrange("p b h c -> p b (h c)"),
                        VSc[1, 0:512, :, :].rearrange("(b p) h c -> p b (h c)", p=128), (), [("vwA", qsl)], f"vwA{qsl}")
                else:
                    dma("sync", vwA[qsl].rearrange("p b h c -> p b (h c)"),
                        VSc[1, t0:t0 + 1024, :, :].rearrange("(b p) h c -> p b (h c)", p=128), (), [("vwA", qsl)], f"vwA{qsl}")
                dma("sync", cbc_f[qsl], cb_cmp_d[m].rearrange("p (a b) -> p a b", a=2), (), [("cbc_f", qsl)], f"cbcf{qsl}")
                dma("sync", cbT[qsl], cbT_cmp_d[m], (), [("cbT", qsl)], f"cbT{qsl}")

            issue_loads(0)
            for m in range(16):
                qsl = m % 2
                nwin0 = 4 if m == 0 else 0
                if m + 1 < 16:
                    issue_loads(m + 1)
                vcopy(cbc[qsl], cbc_f[qsl].unsqueeze(2).to_broadcast([128, 2, 4, 128]), [("cbc_f", qsl)], [("cbc", qsl)])
                Nm = 32 * (m + 1)
                ns = 8 * (m + 1)
                NBc = m // 4 + 1
                zlo = max(0, 32 * m - 32)
                zc0 = zlo - (32 * m - 32)
                for hk in range(2):
                    for g_ in range(4):
                        h = 4 * hk + g_
                        tbk = 6 + (h % 2)
                        esl = h % 2
                        mm(ps[:, tbk, 0:Nm], qT[qsl][:, h, :], kcT[:, hk, 0:Nm], True, True, [("qT", qsl), "kcT"], [("ps", tbk)])
                        vtt(ps[:, tbk, zlo:Nm], ps[:, tbk, zlo:Nm], cbT[qsl][:, zc0:64], ALU.add,
                            [("ps", tbk), ("cbT", qsl)], [("ps", tbk)])
                        act(ee[esl][:, 0:Nm], ps[:, tbk, 0:Nm], AF.Exp, [("ps", tbk)], [("ee", esl), ("Dh", h)],
                            scale=SCALE, accum_out=Dh[:, h:h + 1])
                        vts(Dh[:, h:h + 1], Dh[:, h:h + 1], 1e-30, None, ALU.max, None, [("Dh", h)], [("Dh", h)])
                        vrecip(Dh[:, h:h + 1], Dh[:, h:h + 1], [("Dh", h)], [("Dh", h)])
                        if g_ == 0:
                            vts(psum_[:, 0:Nm], ee[esl][:, 0:Nm], Dh[:, h:h + 1], None, ALU.mult, None,
                                [("ee", esl), ("Dh", h)], ["psum"])
                        else:
                            vstt(psum_[:, 0:Nm], ee[esl][:, 0:Nm], Dh[:, h:h + 1], psum_[:, 0:Nm], ALU.mult, ALU.add,
                                 [("ee", esl), ("Dh", h), "psum"], ["psum"])
                    P.op("vector", lambda e, Nm=Nm, ns=ns: e.tensor_reduce(
                        out=imp[:, 0:ns], in_=psum_[:, 0:Nm].rearrange("p (s f) -> p s f", f=4), axis=AX.X, op=ALU.add),
                        ["psum"], ["imp"])
                    vtt(imp[:, 1:ns], imp[:, 1:ns], psum_[:, 0:Nm].rearrange("p (s f) -> p s f", f=4)[:, 0:ns - 1, 3], ALU.add,
                        ["imp", "psum"], ["imp"])
                    vmemset(imp[:, 0:1], 10.0, ["imp"])
                    if m == 0:
                        zs, zt = imp[:, 0:8], slice(1, 9)
                    else:
                        zs, zt = imp[:, 8 * m - 1:8 * m + 8], slice(0, 9)
                    vtt(zs, zs, sfz[:, zt], ALU.max, ["imp", "sfz"], ["imp"])
                    vtt(zs, zs, suz[:, zt], ALU.min, ["imp", "suz"], ["imp"])
                    P.op("vector", lambda e: e.max(out=m8[:, 0:8], in_=imp[:, :]), ["imp"], ["m8a"])
                    P.op("vector", lambda e: e.match_replace(out=imp2[:, :], in_to_replace=m8[:, 0:8], in_values=imp[:, :],
                                                             imm_value=-2.0), ["imp", "m8a"], ["imp2"])
                    P.op("vector", lambda e: e.max(out=m8[:, 8:16], in_=imp2[:, :]), ["imp2"], ["m8b"])
                    vts(selb, imp, m8[:, 15:16], 1.0, ALU.is_ge, ALU.subtract, ["imp", "m8b"], ["selb"])
                    tr(ps[:, 6, 0:128], selb, identF[:], ["selb", "identF"], [("ps", 6)])
                    act(sbT[hk], ps[:, 6, 0:128].unsqueeze(1).to_broadcast([128, 4, 128]), AF.Copy, [("ps", 6)], [("sbT", hk)],
                        scale=-NEG)
                idB = identB[:]
                o_started = set()
                for hk in range(2):
                    visits = []
                    for nb in range(NBc):
                        biases = []
                        w_ = nb - (NBc - 2)
                        if w_ >= 0:
                            biases.append((idB, cbc[qsl][:, w_, :, :].rearrange("p g q -> p (g q)"), ["identB", ("cbc", qsl)]))
                        visits.append((kcT[:, hk, nb * 128:(nb + 1) * 128], ["kcT"], vcc[:, nb, hk, 0:129], ["vcc", "vcc1"], biases))
                    attend(m, hk, visits, o_started)
                finalize(m, 0, True)
                o_started = set()
                for hk in range(2):
                    visits = []
                    for jj in range(nwin0, 8):
                        biases = [(idB, cbw[:, jj, :, :].rearrange("p g q -> p (g q)"), ["identB", "cbw"])]
                        visits.append((kwT[qsl][:, hk, jj * 128:(jj + 1) * 128], [("kwT", qsl)], vwA[qsl][:, jj, hk, 0:129],
                                       [("vwA", qsl)], biases))
                    attend(m, hk, visits, o_started)
                finalize(m, 2, False)
                o_started = set()
                for hk in range(2):
                    visits = []
                    for kb in range(4 * m + 4):
                        biases = [(Eall[:, kb, :], sbT[hk].rearrange("p g q -> p (g q)"), [("Eall", kb // 16), ("sbT", hk)])]
                        if kb >= 4 * m:
                            biases.append((idB, cbs[:, kb - 4 * m, :, :].rearrange("p g q -> p (g q)"), ["identB", "cbs"]))
                        visits.append((ksT[:, hk, kb * 128:(kb + 1) * 128], [("ksT", hk)], vsA[:, kb, hk, 0:129],
                                       [("vsA", kb // 16)], biases))
                    attend(m, hk, visits, o_started)
                finalize(m, 1, False)
                yflat = yat.rearrange("p h x -> p (h x)")
                rs, krs = rms_rstd(yflat, 1024, 16, "yat")
                vstt(yan, yflat, rs, gattn, ALU.mult, ALU.mult, ["yat", krs, "gattn"], ["yan"])
                pty = bank_bf(7).rearrange("p a (c t) -> p (a c) t", t=128)
                for c in range(8):
                    tr(pty[:, c, :], yan[:, c * 128:(c + 1) * 128], identB[:], ["yan", "identB"], [("ps", 7)])
                act(ymT2[qsl], pty, AF.Copy, [("ps", 7)], [("ymT2", qsl)])
                dma("sync", ymixT[m, :, 8:16, :], ymT2[qsl], [("ymT2", qsl)], [("ymixT", m, 1)], f"ymT2{qsl}")
            P.barrier()
            if KSTOP == "B":
                break

            AR.reset()
            gbuf = AR.alloc([128, 2048], F32)
            ymx = [AR.alloc([128, 16, 128], BF16) for _ in range(4)]
            hh_ = AR.alloc([128, 4, 2048], F32)
            xb = AR.alloc([128, 2048], BF16)
            fT = AR.alloc([128, 16, 512], BF16)
            actT = AR.alloc([128, 44, 512], BF16)
            wst = [AR.alloc([128, 16, 256], BF16) for _ in range(3)]
            stg = [AR.alloc([128, 2816], F32) for _ in range(2)]
            wdn = [AR.alloc([128, 11, 256], BF16) for _ in range(2)]
            sg = [AR.alloc([128, 512], F32) for _ in range(2)]
            wc2 = [0]

            cst = [0]

            def load_cast(dst, src, kdst, a, n):
                i = cst[0] % 2
                cst[0] += 1
                st = stg[i][:, 0:a * n].rearrange("p (a n) -> p a n", a=a)
                dma("sync", st, src, (), [("stg", i)], f"stg{i}")
                if i == 0:
                    vcopy(dst, st, [("stg", i)], [kdst])
                else:
                    act(dst, st, AF.Copy, [("stg", i)], [kdst])

            def next_w2(src_cols):
                i = wc2[0] % 3
                wc2[0] += 1
                sv = src_cols.rearrange("(kc p) n -> p kc n", p=128)
                for hf_ in range(2):
                    load_cast(wst[i][:, hf_ * 8:(hf_ + 1) * 8, :], sv[:, hf_ * 8:(hf_ + 1) * 8, :], ("wst", i), 8, 256)
                return wst[i], ("wst", i)

            dctr = [0]
            octr = [0]
            for tt in range(4):
                for tb in range(4):
                    m = tt * 4 + tb
                    dma("sync", hh_[:, tb, :], xown[m * 128:(m + 1) * 128, :], (), [("hh", tb)], f"hh{tb}")
                    dma("sync", ymx[tb], ymixT[m], (), [("ymx", tb)], f"ymx{tb}")
                for oc in range(8):
                    Wo, kWo = next_w2(w_out[:, oc * 256:(oc + 1) * 256])
                    for tb in range(4):
                        m = tt * 4 + tb
                        sl = tb
                        ob = octr[0] % 2
                        octr[0] += 1
                        for k in range(16):
                            mm(ps[:, ob, 0:256], ymx[sl][:, k, :], Wo[:, k, :], k == 0, k == 15, [("ymx", sl), kWo], [("ps", ob)])
                        vtt(hh_[:, tb, oc * 256:(oc + 1) * 256], ps[:, ob, 0:256], hh_[:, tb, oc * 256:(oc + 1) * 256], ALU.add,
                            [("ps", ob), ("hh", tb)], [("hh", tb)])
                bc_load(gbuf, g_ffn, "gbuf", "c_gbuf")
                for tb in range(4):
                    norm_transpose(hh_[:, tb, :], ("hh", tb), gbuf, "gbuf", xb, "xb", 2 if tb % 2 == 0 else 4,
                                   fT[:, :, tb * 128:(tb + 1) * 128], ("fT", tb), 20)
                fkeys = [("fT", tb) for tb in range(4)]
                for hp in range(22):
                    Wg, kWg = next_w2(w_gate[:, hp * 256:(hp + 1) * 256])
                    Wu, kWu = next_w2(w_up[:, hp * 256:(hp + 1) * 256])
                    for c2 in range(2):
                        hc = hp * 2 + c2
                        gb = 0 + 2 * (hc % 2)
                        ub = gb + 1
                        for k in range(16):
                            mm(bank(gb), Wg[:, k, c2 * 128:(c2 + 1) * 128], fT[:, k, :], k == 0, k == 15, [kWg] + fkeys, [("ps", gb)])
                        for k in range(16):
                            mm(bank(ub), Wu[:, k, c2 * 128:(c2 + 1) * 128], fT[:, k, :], k == 0, k == 15, [kWu] + fkeys, [("ps", ub)])
                        act(sg[hc % 2], bank(gb), AF.Silu, [("ps", gb)], [("sg", hc % 2)])
                        vtt(actT[:, hc, :], bank(ub), sg[hc % 2], ALU.mult, [("ps", ub), ("sg", hc % 2)], [("actT", hc)])
                wdv = w_down.rearrange("(hc p) n -> p hc n", p=128)
                for cg in range(8):
                    for hq in range(4):
                        di = dctr[0] % 2
                        dctr[0] += 1
                        load_cast(wdn[di], wdv[:, hq * 11:(hq + 1) * 11, cg * 256:(cg + 1) * 256], ("wdn", di), 11, 256)
                        for tb in range(4):
                            for c in range(11):
                                hc = hq * 11 + c
                                mm(ps[:, 4 + tb, 0:256], actT[:, hc, tb * 128:(tb + 1) * 128], wdn[di][:, c, :], hc == 0, hc == 43,
                                   [("actT", hc), ("wdn", di)], [("ps", 4 + tb)])
                    for tb in range(4):
                        vtt(hh_[:, tb, cg * 256:(cg + 1) * 256], ps[:, 4 + tb, 0:256], hh_[:, tb, cg * 256:(cg + 1) * 256], ALU.add,
                            [("ps", 4 + tb), ("hh", tb)], [("hh", tb)])
                bc_load(gbuf, g_fin, "gbuf", "c_gbuf")
                for tb in range(4):
                    m = tt * 4 + tb
                    rs, krs = rms_rstd(hh_[:, tb, :], 2048, 24, ("hh", tb))
                    vstt(hh_[:, tb, :], hh_[:, tb, :], rs, gbuf, ALU.mult, ALU.mult, [("hh", tb), krs, "gbuf"], [("hh", tb)])
                    dma("sync", y[m * 128:(m + 1) * 128, :], hh_[:, tb, :], [("hh", tb)], [("y", m)], f"yout{tb}")
            o_ = P.op("sync", lambda e: e.nop(), [("y", m) for m in range(16)], ())
            o_.is_nop = True
            P.barrier()
        P.emit(nc, ctx)
    return nc


def _tables(j):
    f32 = np.float32
    k = np.arange(128)[:, None]
    q = np.arange(128)[None, :]
    cb_sel = np.zeros((128, 4, 128), f32)
    for jj in range(4):
        if jj < j:
            v = np.ones((128, 128), bool)
        elif jj == j:
            v = k <= q
        else:
            v = np.zeros((128, 128), bool)
        cb_sel[:, jj, :] = np.where(v, 0.0, NEG)
    cb_win = np.zeros((128, 8, 128), f32)
    for jj in range(8):
        delta = 128 * (j + 4 - jj) + q - k
        cb_win[:, jj, :] = np.where((delta >= 0) & (delta < 512), 0.0, NEG)
    cb_cmp = np.zeros((16, 128, 2, 128), f32)
    cbT_cmp = np.zeros((16, 128, 64), f32)
    for m in range(16):
        t = 128 * (4 * m + j) + np.arange(128)
        NBc = m // 4 + 1
        for w in range(2):
            nb = NBc - 2 + w
            n = 128 * nb + np.arange(128)
            valid = (16 * n[:, None] + 31 <= t[None, :]) & (n[:, None] >= 0)
            cb_cmp[m, :, w, :] = np.where(valid, 0.0, NEG)
        n = 32 * m - 32 + np.arange(64)
        valid = 16 * n[None, :] + 31 <= t[:, None]
        cbT_cmp[m] = np.where(valid, 0.0, NEG)
    qq = np.arange(128)[:, None]
    c = np.arange(9)[None, :]
    rel = c - 1 - 2 * j - (qq >= 64)
    sel_force = np.where((rel == 0) | (rel == -1), 10.0, -1e9).astype(f32)
    sel_future = np.where(rel > 0, -1.0, 1e9).astype(f32)
    return dict(cb_sel=cb_sel.reshape(128, 512), cb_win=cb_win.reshape(128, 1024),
                cb_cmp=cb_cmp.reshape(16, 128, 256), cbT_cmp=cbT_cmp, sel_force=sel_force, sel_future=sel_future)


def _shared_tables():
    f32 = np.float32
    half = 64
    inv = 1.0 / (10000.0 ** (np.arange(half, dtype=np.float32) / half))
    ang = np.arange(S, dtype=np.float32)[:, None] * inv[None, :]
    cs_all = np.concatenate([np.cos(ang), np.sin(ang)], axis=1).astype(f32)
    s = np.arange(128)[:, None, None]
    kb = np.arange(64)[None, :, None]
    kk = np.arange(128)[None, None, :]
    E_all = (s == 2 * kb + kk // 64).astype(f32).reshape(128, 64 * 128)
    kr = np.arange(128)[:, None]
    mc = np.arange(128)[None, :]
    Sh = np.stack([(kr == mc - 1), (kr == mc - 2)], axis=1).astype(f32).reshape(128, 256)
    Hh = np.zeros((32, 272), f32)
    r = np.arange(32)
    Hh[r % 2 == 1, 0] = 1.0
    Hh[r % 2 == 0, 128 + 0] = 1.0
    Hh[r % 2 == 1, 128 + 1] = 1.0
    for m in range(16):
        Hh[2 * m:2 * m + 2, 256 + m] = 1.0
    return dict(cs_all=cs_all, E_all=E_all, Sh=Sh, Hh=Hh, ident=np.eye(128, dtype=f32))


_NC_CACHE = {}


def kernel(**inputs):
    x = np.asarray(inputs["x"], dtype=np.float32)
    sh = _shared_tables()
    base = {
        "w_in": np.ascontiguousarray(inputs["w_in"][0]),
        "w_out": np.ascontiguousarray(inputs["w_out"][0]),
        "w_gate": np.ascontiguousarray(inputs["w_gate"][0]),
        "w_up": np.ascontiguousarray(inputs["w_up"][0]),
        "w_down": np.ascontiguousarray(inputs["w_down"][0]),
        "conv_w": np.ascontiguousarray(inputs["conv_w"][0]),
        "cmp_pe_k": np.ascontiguousarray(inputs["cmp_pe_k"][0]),
        "cmp_w1_k": np.ascontiguousarray(inputs["cmp_w1_k"][0]),
        "cmp_w2_k": np.ascontiguousarray(inputs["cmp_w2_k"][0]),
        "cmp_pe_v": np.ascontiguousarray(inputs["cmp_pe_v"][0]),
        "cmp_w1_v": np.ascontiguousarray(inputs["cmp_w1_v"][0]),
        "cmp_w2_v": np.ascontiguousarray(inputs["cmp_w2_v"][0]),
        "norm_mix": np.ascontiguousarray(inputs["norm_mix"][0]),
        "norm_conv_out": np.ascontiguousarray(inputs["norm_conv_out"][0]),
        "norm_attn_out": np.ascontiguousarray(inputs["norm_attn_out"][0]),
        "norm_ffn": np.ascontiguousarray(inputs["norm_ffn"][0]),
        "norm_final": np.ascontiguousarray(inputs["norm_final"]),
    }
    base = {k: np.asarray(v, dtype=np.float32) for k, v in base.items()}
    base.update(sh)
    in_maps = []
    for c in range(8):
        b, j = c // 4, c % 4
        blocks = [4 * m + j for m in range(16)]
        xb_ = x[b]
        xown = np.concatenate([xb_[128 * qb:128 * qb + 128] for qb in blocks], axis=0)
        xhalo = np.zeros((32, D), np.float32)
        for m, qb in enumerate(blocks):
            if qb > 0:
                xhalo[2 * m:2 * m + 2] = xb_[128 * qb - 2:128 * qb]
        cs_own = np.concatenate([sh["cs_all"][128 * qb:128 * qb + 128] for qb in blocks], axis=0)
        im = dict(base)
        im.update(_tables(j))
        im.update(xall=np.ascontiguousarray(xb_[:KS]), xown=np.ascontiguousarray(xown), xhalo=xhalo,
                  cs_own=np.ascontiguousarray(cs_own))
        im["cs_all"] = np.ascontiguousarray(im["cs_all"][:KS])
        if SHRINK_KEEP is not None:
            im = {k: (v if k in SHRINK_KEEP else np.zeros((1, 1), np.float32)) for k, v in im.items()}
        in_maps.append(im)
    if "nc" not in _NC_CACHE:
        _NC_CACHE["nc"] = build_nc()
    nc = _NC_CACHE["nc"]
    res = run_bass_kernel_spmd(nc, in_maps[:KCORES], core_ids=list(range(KCORES)))
    out = np.zeros((2, S, D), np.float32)
    for c in range(KCORES):
        b, j = c // 4, c % 4
        yv = res.results[c]["y"]
        for m in range(16):
            qb = 4 * m + j
            out[b, 128 * qb:128 * qb + 128] = yv[128 * m:128 * m + 128]
    if DEBUG:
        kernel.last_results = res.results
    return out
```

```python
import os
import numpy as np
import concourse.bass as bass
import concourse.mybir as mybir
from concourse.bass_utils import run_bass_kernel_spmd
from contextlib import ExitStack

F32 = mybir.dt.float32
BF16 = mybir.dt.bfloat16
U8 = mybir.dt.uint8
AF = mybir.ActivationFunctionType
ALU = mybir.AluOpType
AX = mybir.AxisListType

D = 2048
S = 8192
DP = 5656
FF = 5632
NEG = -30000.0
SCALE = 128 ** -0.5
EPS = 1e-6
ENGS = ["tensor", "vector", "scalar", "gpsimd", "sync"]
DEBUG = bool(int(os.environ.get("KDEBUG", "0")))
KSTOP = os.environ.get("KSTOP", "")
KCORES = int(os.environ.get("KCORES", "8"))
KS = int(os.environ.get("KS", "8192"))
KSKIP = set(os.environ.get("KSKIP", "").split(","))
_UA = {"xall", "w_in", "norm_mix", "cs_all", "ident"}
_UA2 = _UA | {"cmp_pe_k", "cmp_w1_k", "cmp_w2_k", "cmp_pe_v", "cmp_w1_v", "cmp_w2_v"}
_U0 = _UA2 | {"xown", "xhalo", "conv_w", "norm_conv_out", "cs_own", "Sh", "Hh"}
_UB = _U0 | {"cb_sel", "cb_win", "cb_cmp", "cbT_cmp", "sel_force", "sel_future", "E_all", "norm_attn_out"}
_USED = {"A": _UA, "A2": _UA2, "0": _U0, "B": _UB}
SHRINK_KEEP = _USED.get(KSTOP)


class Op:
    __slots__ = ("eng", "fn", "deps", "needs_inc", "count", "grp", "is_nop")


class DmaGroup:
    __slots__ = ("stream", "n", "final", "last")


class Prog:
    def __init__(self):
        self.q = {e: [] for e in ENGS}
        self.keys = {}
        self.streams = {}

    def dma_group(self, stream):
        g = DmaGroup()
        g.stream = stream
        g.n = 0
        g.final = None
        g.last = None
        self.streams.setdefault(stream, []).append(g)
        return g

    def op(self, eng, fn, reads=(), writes=(), grp=None, extra=()):
        o = Op()
        o.eng = eng
        o.fn = fn
        o.needs_inc = False
        o.count = None
        o.grp = grp
        o.is_nop = False
        if grp is not None:
            grp.n += 1
            grp.last = o
        ident = eng if grp is None else ("dma", id(grp))
        deps = set(extra)
        writes = list(writes) + [k for k in reads if isinstance(k, tuple) and k[0] == "ps" and k not in writes]
        reads = [k for k in reads if not (isinstance(k, tuple) and k[0] == "ps")]
        for k in reads:
            st = self.keys.setdefault(k, ({}, {}))
            deps.update(st[0].values())
            st[1][ident] = o
        for k in writes:
            st = self.keys.setdefault(k, ({}, {}))
            deps.update(st[0].values())
            deps.update(st[1].values())
            if st[1]:
                st[0].clear()
                st[1].clear()
            st[0][ident] = o
        deps.discard(o)
        o.deps = deps
        self.q[eng].append(o)
        return o

    def barrier(self):
        arr = []
        for e in ENGS:
            for o in reversed(self.q[e]):
                if o.grp is None and not getattr(o, "is_nop", False):
                    arr.append(o)
                    break
        lasts = [g[-1].last for g in self.streams.values() if g and g[-1].last is not None]
        for e in ENGS:
            o = self.op(e, lambda eng: eng.nop(), extra=arr + lasts)
            o.is_nop = True
        self.keys = {}

    def emit(self, nc, ctx):
        engsem = {e: ctx.enter_context(nc.semaphore("es_" + e)) for e in ENGS}
        ssem = {}
        for s, groups in self.streams.items():
            ssem[s] = ctx.enter_context(nc.semaphore("ds_" + str(s)))
            cum = 0
            for g in groups:
                cum += 16 * g.n
                g.final = cum
        for e in ENGS:
            for o in self.q[e]:
                for d in o.deps:
                    if d.grp is None and not (d.eng == e and e == "tensor"):
                        d.needs_inc = True
        for e in ENGS:
            c = 0
            for o in self.q[e]:
                if o.grp is None and o.needs_inc:
                    c += 1
                    o.count = c
        block = ctx.enter_context(nc.Block())
        prog = self

        def run(e):
            def body(eng):
                waited = {}
                for o in prog.q[e]:
                    needs = {}
                    for d in o.deps:
                        if d.grp is not None:
                            s, v, key = ssem[d.grp.stream], d.grp.final, ("s", d.grp.stream)
                        else:
                            if d.eng == e and e == "tensor":
                                continue
                            s, v, key = engsem[d.eng], d.count, ("e", d.eng)
                        if key not in needs or needs[key][1] < v:
                            needs[key] = (s, v)
                    for key, (s, v) in needs.items():
                        if waited.get(key, 0) < v:
                            eng.wait_ge(s, v)
                            waited[key] = v
                    ins = o.fn(eng)
                    if o.grp is not None:
                        ins.then_inc(ssem[o.grp.stream], 16)
                    elif o.needs_inc:
                        ins.then_inc(engsem[e], 1)
            return body

        block.tensor(run("tensor"))
        block.vector(run("vector"))
        block.scalar(run("scalar"))
        block.gpsimd(run("gpsimd"))
        block.sync(run("sync"))


class Arena:
    def __init__(self, t, size):
        self.t = t
        self.size = size
        self.off = 0

    def reset(self):
        self.off = 0

    def alloc(self, shape, dt):
        esz = 4 if dt == F32 else (2 if dt == BF16 else 1)
        n = 1
        for s in shape[1:]:
            n *= s
        nb = (n * esz + 63) // 64 * 64
        assert self.off + nb <= self.size, ("arena overflow", self.off, nb, self.size)
        ap = self.t[0:shape[0], self.off:self.off + n * esz].bitcast(dt)
        self.off += nb
        if len(shape) == 3:
            ap = ap.rearrange("p (a b) -> p a b", a=shape[1])
        elif len(shape) == 4:
            ap = ap.rearrange("p (a b c) -> p a b c", a=shape[1], b=shape[2])
        elif len(shape) == 5:
            ap = ap.rearrange("p (a b c d) -> p a b c d", a=shape[1], b=shape[2], c=shape[3])
        return ap


def build_nc():
    nc = bass.Bass("TRN2", target_bir_lowering=False)

    def din(name, shape):
        if SHRINK_KEEP is not None and name not in SHRINK_KEEP:
            shape = [1, 1]
        return nc.dram_tensor(name, list(shape), F32, kind="ExternalInput").ap()

    xall = din("xall", [KS, D])
    xown = din("xown", [2048, D])
    xhalo = din("xhalo", [32, D])
    w_in = din("w_in", [D, DP])
    w_out = din("w_out", [D, D])
    w_gate = din("w_gate", [D, FF])
    w_up = din("w_up", [D, FF])
    w_down = din("w_down", [FF, D])
    conv_w = din("conv_w", [3, 1024])
    pe_k = din("cmp_pe_k", [32, 128])
    w1_k = din("cmp_w1_k", [4096, 128])
    w2_k = din("cmp_w2_k", [128, 128])
    pe_v = din("cmp_pe_v", [32, 128])
    w1_v = din("cmp_w1_v", [4096, 128])
    w2_v = din("cmp_w2_v", [128, 128])
    g_mix = din("norm_mix", [D])
    g_conv = din("norm_conv_out", [1024])
    g_attn = din("norm_attn_out", [1024])
    g_ffn = din("norm_ffn", [D])
    g_fin = din("norm_final", [D])
    cs_all = din("cs_all", [KS, 128])
    cs_own = din("cs_own", [2048, 128])
    cb_sel_d = din("cb_sel", [128, 4 * 128])
    cb_win_d = din("cb_win", [128, 8 * 128])
    cb_cmp_d = din("cb_cmp", [16, 128, 2 * 128])
    cbT_cmp_d = din("cbT_cmp", [16, 128, 64])
    self_force_d = din("sel_force", [128, 9])
    self_future_d = din("sel_future", [128, 9])
    E_all_d = din("E_all", [128, 64 * 128])
    Sh_d = din("Sh", [128, 2 * 128])
    Hh_d = din("Hh", [32, 272])
    ident_d = din("ident", [128, 128])
    y = nc.dram_tensor("y", [2048, D], F32, kind="ExternalOutput").ap()

    skind = dict(kind="ExternalOutput") if DEBUG else {}
    KTs = nc.dram_tensor("KTs", [4, 2, 128, KS], BF16, **skind).ap()
    VSc = nc.dram_tensor("VSc", [2, KS, 2, 130], BF16, **skind).ap()
    qTs = nc.dram_tensor("qTs", [16, 128, 8, 128], BF16, **skind).ap()
    ymixT = nc.dram_tensor("ymixT", [16, 128, 16, 128], BF16, **skind).ap()
    if DEBUG:
        dbg_kc = nc.dram_tensor("dbg_kc", [128, 2 * 512], BF16, kind="ExternalOutput").ap()
        dbg_vc = nc.dram_tensor("dbg_vc", [128, 4 * 2 * 130], BF16, kind="ExternalOutput").ap()
        dbg_gates = nc.dram_tensor("dbg_gates", [128, 16 * 24], F32, kind="ExternalOutput").ap()

    ctx = ExitStack()
    with ctx:
        P = Prog()
        AR_SIZE = 184 * 1024
        arena_t = ctx.enter_context(nc.sbuf_tensor("arena", [128, AR_SIZE], U8))
        AR = Arena(arena_t, AR_SIZE)

        def sbt(name, shape, dt):
            return ctx.enter_context(nc.sbuf_tensor(name, shape, dt))

        ps = ctx.enter_context(nc.psum_tensor("ps", [128, 8, 512], F32))

        def bank(i):
            return ps[:, i, :]

        def bank_bf(i, n=1):
            return ps[:, i:i + n, :].bitcast(BF16)

        identF = sbt("identF", [128, 128], F32)
        identB = sbt("identB", [128, 128], BF16)
        eps_t = sbt("eps_t", [128, 1], F32)
        kcT = sbt("kcT", [128, 2, 512], BF16)
        vcc = sbt("vcc", [128, 4, 2, 130], BF16)
        gates = sbt("gates", [128, 16, 24], F32)
        u_halo = sbt("u_halo", [32, 1024], F32)
        aT_halo = sbt("aT_halo", [128, 16, 32], BF16)
        junk = sbt("junk", [128, 2048], BF16)
        stat = sbt("stat", [128, 64], F32)

        def dma(queue, out, in_, reads, writes, stream, grp=None):
            g = grp if grp is not None else P.dma_group(stream)
            P.op(queue, lambda e: e.dma_start(out=out, in_=in_), reads, writes, grp=g)
            return g

        def mm(out, lhsT, rhs, start, stop, reads, writes, skip=False):
            P.op("tensor", lambda e: e.matmul(out, lhsT=lhsT, rhs=rhs, start=start, stop=stop,
                                              skip_group_check=skip), reads, writes)

        def tr(out, in_, ident, reads, writes):
            P.op("tensor", lambda e: e.transpose(out=out, in_=in_, identity=ident), reads, writes)

        def act(out, in_, func, reads, writes, **kw):
            P.op("scalar", lambda e: e.activation(out=out, in_=in_, func=func, **kw), reads, writes)

        def vtt(out, in0, in1, op, reads, writes):
            P.op("vector", lambda e: e.tensor_tensor(out=out, in0=in0, in1=in1, op=op), reads, writes)

        def vts(out, in0, s1, s2, op0, op1, reads, writes):
            if op1 is None:
                P.op("vector", lambda e: e.tensor_scalar(out=out, in0=in0, scalar1=s1, scalar2=None, op0=op0),
                     reads, writes)
            else:
                P.op("vector", lambda e: e.tensor_scalar(out=out, in0=in0, scalar1=s1, scalar2=s2, op0=op0, op1=op1),
                     reads, writes)

        def vstt(out, in0, scalar, in1, op0, op1, reads, writes):
            P.op("vector", lambda e: e.scalar_tensor_tensor(out=out, in0=in0, scalar=scalar, in1=in1,
                                                            op0=op0, op1=op1), reads, writes)

        def vcopy(out, in_, reads, writes):
            P.op("vector", lambda e: e.tensor_copy(out=out, in_=in_), reads, writes)

        def vrecip(out, in_, reads, writes):
            P.op("vector", lambda e: e.reciprocal(out=out, in_=in_), reads, writes)

        def vmemset(ap, val, writes):
            P.op("vector", lambda e: e.memset(ap, val), (), writes)

        def bc_load(dst, src_row, key, stream):
            dma("sync", dst, src_row.partition_broadcast(128), (), [key], stream)

        def rms_rstd(src, n, col, kin, np_=128):
            ss = stat[0:np_, col:col + 1]
            rt = stat[0:np_, col + 1:col + 2]
            rs = stat[0:np_, col + 2:col + 3]
            act(junk[0:np_, 0:n], src, AF.Square, [kin], ["junk", ("stat", col)], accum_out=ss)
            act(rt, ss, AF.Sqrt, [("stat", col)], [("stat", col + 1)], scale=1.0 / n, bias=eps_t[0:np_, :])
            vrecip(rs, rt, [("stat", col + 1)], [("stat", col + 2)])
            return rs, ("stat", col + 2)

        def norm_transpose(src, ksrc, gain_bc, kgain, xb, kxb, pbank, dst, kdst, col, np_=128):
            rs, krs = rms_rstd(src, 2048, col, ksrc, np_)
            vstt(xb[0:np_, :], src, rs, gain_bc[0:np_, :], ALU.mult, ALU.mult, [ksrc, krs, kgain], [kxb])
            pt = bank_bf(pbank, 2).rearrange("p a (c t) -> p (a c) t", t=128)
            idn = identB[0:np_, 0:np_]
            for k in range(16):
                tr(pt[:, k, 0:np_], xb[0:np_, k * 128:(k + 1) * 128], idn, [kxb, "identB"],
                   [("ps", pbank), ("ps", pbank + 1)])
            act(dst, pt[:, :, 0:np_], AF.Copy, [("ps", pbank), ("ps", pbank + 1)], [kdst])

        def load_w(dst, src_cols, key, stream):
            dma("gpsimd", dst, src_cols.rearrange("(kc p) n -> p kc n", p=128), (), [key], stream)

        dma("sync", identF[:], ident_d, (), ["identF"], "c_id")
        vcopy(identB[:], identF[:], ["identF"], ["identB"])
        vmemset(eps_t[:], EPS, ["eps"])
        P.barrier()
        for _once in (0,):

            AR.reset()
            wkv = AR.alloc([128, 16, 1536], BF16)
            gmix = AR.alloc([128, 2048], F32)
            xs = [AR.alloc([128, 2048], F32) for _ in range(2)]
            xb = AR.alloc([128, 2048], BF16)
            aT = [AR.alloc([128, 16, 128], BF16) for _ in range(2)]
            cs = [AR.alloc([128, 2, 64], F32) for _ in range(2)]
            rt1 = AR.alloc([128, 3, 2, 64], F32)
            rt2 = AR.alloc([128, 3, 2, 64], F32)
            krope = AR.alloc([128, 3, 2, 2, 64], BF16)
            vcb = AR.alloc([128, 2, 128], BF16)
            vst = [AR.alloc([128, 2, 2, 130], BF16) for _ in range(2)]
            kts = [AR.alloc([128, 8, 128], BF16) for _ in range(2)]

            for g in range(3):
                load_w(wkv[:, :, g * 512:(g + 1) * 512], w_in[:, 4096 + g * 512:4096 + (g + 1) * 512], ("wkv", g), f"wkv{g}")
            bc_load(gmix, g_mix, "gmix", "c_gmix")
            for s_ in range(2):
                if "ms" not in KSKIP:
                    vmemset(vst[s_][:, :, :, 128:130], 1.0, [("vst1", s_)])
            NB_A = int(os.environ.get("KNBA", "64"))
            dma("sync", xs[0], xall[0:128, :], (), [("xs", 0)], "xs0")
            if NB_A > 1:
                dma("sync", xs[1], xall[128:256, :], (), [("xs", 1)], "xs1")
            for i in range(NB_A):
                sl = i % 2
                dma("sync", cs[sl], cs_all[i * 128:(i + 1) * 128, :].rearrange("p (a b) -> p a b", a=2), (), [("cs", sl)], f"cs{sl}")
                if i == 0:
                    norm_transpose(xs[0], ("xs", 0), gmix, "gmix", xb, "xb", 0, aT[0], ("aT", 0), 0)
                for g in range(3 if "mm" not in KSKIP else 0):
                    for k in range(16):
                        mm(bank(2 + g), aT[sl][:, k, :], wkv[:, k, g * 512:(g + 1) * 512], k == 0, k == 15,
                           [("aT", sl), ("wkv", g)], [("ps", 2 + g)])
                if i + 1 < NB_A:
                    norm_transpose(xs[1 - sl], ("xs", 1 - sl), gmix, "gmix", xb, "xb", 6 if sl == 0 else 0,
                                   aT[1 - sl], ("aT", 1 - sl), 0)
                    if i + 2 < NB_A:
                        dma("sync", xs[sl], xall[(i + 2) * 128:(i + 3) * 128, :], (), [("xs", sl)], f"xs{sl}")
                if "rope" not in KSKIP:
                    z = ps[:, 2:5, 0:256].rearrange("p t (h f x) -> p t h f x", h=2, f=2)
                    cosb = cs[sl][:, 0, :].unsqueeze(1).unsqueeze(1).to_broadcast([128, 3, 2, 64])
                    sinb = cs[sl][:, 1, :].unsqueeze(1).unsqueeze(1).to_broadcast([128, 3, 2, 64])
                    pk = [("ps", 2), ("ps", 3), ("ps", 4)]
                    vtt(rt1, z[:, :, :, 0, :], cosb, ALU.mult, pk + [("cs", sl)], ["rt1"])
                    vtt(rt2, z[:, :, :, 1, :], sinb, ALU.mult, pk + [("cs", sl)], ["rt2"])
                    vtt(krope[:, :, :, 0, :], rt1, rt2, ALU.subtract, ["rt1", "rt2"], ["krope0"])
                    vtt(rt1, z[:, :, :, 1, :], cosb, ALU.mult, pk + [("cs", sl)], ["rt1"])
                    vtt(rt2, z[:, :, :, 0, :], sinb, ALU.mult, pk + [("cs", sl)], ["rt2"])
                    vtt(krope[:, :, :, 1, :], rt1, rt2, ALU.add, ["rt1", "rt2"], ["krope1"])
                if "vcopy" not in KSKIP:
                    act(vcb, ps[:, 2, 256:512].rearrange("p (h x) -> p h x", h=2), AF.Copy, [("ps", 2)], ["vcb"])
                    for t_ in range(2):
                        act(vst[sl][:, t_, :, 0:128], ps[:, 3 + t_, 256:512].rearrange("p (h x) -> p h x", h=2), AF.Copy,
                            [("ps", 3 + t_)], [("vst", sl)])
                if "tr5" not in KSKIP:
                    pk5 = bank_bf(5).rearrange("p a (c t) -> p (a c) t", t=128)
                    for t in range(3):
                        for hk in range(2):
                            tr(pk5[:, t * 2 + hk, :], krope[:, t, hk, :, :].rearrange("p f x -> p (f x)"), identB[:],
                               ["krope0", "krope1", "identB"], [("ps", 5)])
                    for hk in range(2):
                        tr(pk5[:, 6 + hk, :], vcb[:, hk, :], identB[:], ["vcb", "identB"], [("ps", 5)])
                    vcopy(kts[sl], pk5, [("ps", 5)], [("kts", sl)])
                if "st" not in KSKIP:
                    dma("sync", KTs[:, :, :, i * 128:(i + 1) * 128].rearrange("t h d s -> d (t h) s"), kts[sl],
                        [("kts", sl)], [("KTs", i)], f"kst{sl}")
                    dma("sync", VSc[:, i * 128:(i + 1) * 128, :, :].rearrange("t s h c -> s t (h c)"),
                        vst[sl].rearrange("p t h c -> p t (h c)"), [("vst", sl), ("vst1", sl)], [("VSc", i)], f"vst{sl}")
            P.barrier()
            if KSTOP == "A":
                break

            AR.reset()
            w1b = [AR.alloc([128, 32, 128], BF16) for _ in range(2)]
            w2b = [AR.alloc([128, 128], BF16) for _ in range(2)]
            pe_f = [AR.alloc([32, 128], F32) for _ in range(2)]
            peT = [AR.alloc([128, 32], BF16) for _ in range(2)]
            c0 = [AR.alloc([128, 1], F32) for _ in range(2)]
            raw = [AR.alloc([128, S], BF16) for _ in range(2)]
            h1T = AR.alloc([128, 512], BF16)
            for ti, (w1d, w2d, ped) in enumerate([(w1_k, w2_k, pe_k), (w1_v, w2_v, pe_v)]):
                dma("gpsimd", w1b[ti], w1d.rearrange("(l d) e -> d l e", d=128), (), [("w1b", ti)], f"w1b{ti}")
                dma("gpsimd", w2b[ti], w2d, (), [("w2b", ti)], f"w2b{ti}")
                dma("sync", pe_f[ti], ped, (), [("pe_f", ti)], f"pef{ti}")
                tr(ps[:, 0, 0:32], pe_f[ti], identF[0:32, 0:32], [("pe_f", ti), "identF"], [("ps", 0)])
                vcopy(peT[ti], ps[:, 0, 0:32], [("ps", 0)], [("peT", ti)])
                for l in range(32):
                    mm(ps[:, 1, 0:1], w1b[ti][:, l, :], peT[ti][:, l:l + 1], l == 0, l == 31,
                       [("w1b", ti), ("peT", ti)], [("ps", 1)])
                vcopy(c0[ti], ps[:, 1, 0:1], [("ps", 1)], [("c0", ti)])
            vmemset(vcc[:, :, :, 128:130], 1.0, ["vcc1"])
            it = 0
            for ti in range(2):
                for hk in range(2):
                    sl = it % 2
                    it += 1
                    dma("sync", raw[sl], KTs[0 if ti == 0 else 3, hk, :, :], (), [("raw", sl)], f"raw{sl}")
                    r16 = raw[sl].rearrange("p (n s) -> p n s", s=16)
                    pb = 2 + sl
                    for l in range(32):
                        rhs = r16[:, 0:511, l] if l < 16 else r16[:, 1:512, l - 16]
                        mm(ps[:, pb, 0:511], w1b[ti][:, l, :], rhs, l == 0, l == 31, [("w1b", ti), ("raw", sl)], [("ps", pb)])
                    vmemset(h1T[:, 511:512], 0.0, ["h1T"])
                    act(h1T[:, 0:511], ps[:, pb, 0:511], AF.Silu, [("ps", pb), ("c0", ti)], ["h1T"], bias=c0[ti])
                    if ti == 0:
                        mm(bank(4), w2b[0], h1T, True, True, [("w2b", 0), "h1T"], [("ps", 4)])
                        vcopy(kcT[:, hk, :], bank(4), [("ps", 4)], ["kcT"])
                    else:
                        for nb in range(4):
                            mm(ps[:, 5, nb * 128:(nb + 1) * 128], h1T[:, nb * 128:(nb + 1) * 128], w2b[1], True, True,
                               [("w2b", 1), "h1T"], [("ps", 5)])
                        vcopy(vcc[:, :, hk, 0:128], ps[:, 5, :].rearrange("p (n x) -> p n x", n=4), [("ps", 5)], ["vcc"])
            if DEBUG:
                dma("sync", dbg_kc, kcT[:].rearrange("p a b -> p (a b)"), ["kcT"], ["dbg_kc"], "dbg1")
                dma("sync", dbg_vc, vcc[:].rearrange("p a b c -> p (a b c)"), ["vcc", "vcc1"], ["dbg_vc"], "dbg2")
            P.barrier()
            if KSTOP == "A2":
                break

            AR.reset()
            gmix = AR.alloc([128, 2048], F32)
            gconv = AR.alloc([128, 1024], F32)
            wcv = AR.alloc([128, 3, 1024], F32)
            shf = AR.alloc([128, 2, 128], F32)
            hfix = AR.alloc([32, 2, 128], F32)
            hmask = AR.alloc([32, 16], F32)
            uh = [AR.alloc([32, 256], F32) for _ in range(2)]
            xs = [AR.alloc([128, 2048], F32) for _ in range(2)]
            xb = AR.alloc([128, 2048], BF16)
            aTo = AR.alloc([128, 16, 1024], BF16)
            wch = [AR.alloc([128, 16, 256], BF16) for _ in range(6)]
            wgt = AR.alloc([128, 16, 24], BF16)
            ycb = AR.alloc([128, 8, 1024], BF16)
            cso = AR.alloc([128, 8, 2, 64], F32)
            ccs = [AR.alloc([128, 256], F32) for _ in range(2)]
            uu = [AR.alloc([128, 256], F32) for _ in range(2)]
            aa = [AR.alloc([128, 256], F32) for _ in range(2)]
            tb_ = [AR.alloc([128, 256], F32) for _ in range(2)]
            qr1 = AR.alloc([128, 2, 64], F32)
            qr2 = AR.alloc([128, 2, 64], F32)
            qrope = [AR.alloc([128, 2, 2, 64], BF16) for _ in range(2)]
            qts = [AR.alloc([128, 2, 128], BF16) for _ in range(2)]
            ycn = AR.alloc([128, 1024], BF16)
            ymT = [AR.alloc([128, 8, 128], BF16) for _ in range(2)]
            ssq = AR.alloc([128, 8, 4], F32)

            bc_load(gmix, g_mix, "gmix", "c_gmix")
            bc_load(gconv, g_conv, "gconv", "c_gconv")
            for kk in range(3):
                bc_load(wcv[:, kk, :], conv_w[kk, :], ("wcv", kk), f"c_wcv{kk}")
            dma("sync", shf, Sh_d.rearrange("p (a b) -> p a b", a=2), (), ["shf"], "c_sh")
            dma("sync", hfix, Hh_d[:, 0:256].rearrange("p (a b) -> p a b", a=2), (), ["hfix"], "c_hh")
            dma("sync", hmask, Hh_d[:, 256:272], (), ["hmask"], "c_hm")
            wcnt = [0]

            def next_w(src_cols, ncols=256):
                i = wcnt[0] % 6
                wcnt[0] += 1
                load_w(wch[i][:, :, 0:ncols], src_cols, ("wch", i), f"wch{i}")
                return wch[i], ("wch", i)

            for hf in range(2):
                if hf == 0:
                    dma("sync", xs[1][0:32, :], xhalo, (), [("xs", 1)], "xs1")
                    norm_transpose(xs[1][0:32, :], ("xs", 1), gmix, "gmix", xb, "xb", 0, aT_halo[:], "aT_halo", 0, np_=32)
                for mi in range(8):
                    m = hf * 8 + mi
                    sl = mi % 2
                    dma("sync", xs[sl], xown[m * 128:(m + 1) * 128, :], (), [("xs", sl)], f"xs{sl}")
                    norm_transpose(xs[sl], ("xs", sl), gmix, "gmix", xb, "xb", 0 if sl == 0 else 6,
                                   aTo[:, :, mi * 128:(mi + 1) * 128], ("aTo", mi), 0)
                dma("sync", cso, cs_own[hf * 1024:(hf + 1) * 1024, :].rearrange("(m p) (a b) -> p m a b", p=128, a=2),
                    (), ["cso"], "cso")
                for cg in range(4):
                    c0_, c1_ = cg * 256, (cg + 1) * 256
                    Wb, kWb = next_w(w_in[:, c0_:c1_])
                    Wc, kWc = next_w(w_in[:, 1024 + c0_:1024 + c1_])
                    Wh, kWh = next_w(w_in[:, 2048 + c0_:2048 + c1_])
                    if hf == 0:
                        for k in range(16):
                            mm(ps[0:32, 2, 0:256], aT_halo[:, k, :], Wc[:, k, :], k == 0, k == 15, ["aT_halo", kWc], [("ps", 2)])
                        for k in range(16):
                            mm(ps[0:32, 2, 256:512], aT_halo[:, k, :], Wh[:, k, :], k == 0, k == 15, ["aT_halo", kWh], [("ps", 2)])
                        act(ccs[0][0:32, :], ps[0:32, 2, 0:256], AF.Copy, [("ps", 2)], [("ccs", 0)])
                        vtt(u_halo[:, c0_:c1_], ps[0:32, 2, 256:512], ccs[0][0:32, :], ALU.mult, [("ps", 2), ("ccs", 0)], ["u_halo"])
                    for mi in range(8):
                        m = hf * 8 + mi
                        sl = mi % 2
                        bA, bB, bC = (2, 3, 4) if sl == 0 else (5, 6, 7)
                        at = aTo[:, :, mi * 128:(mi + 1) * 128]
                        for k in range(16):
                            mm(ps[:, bA, 0:256], at[:, k, :], Wc[:, k, :], k == 0, k == 15, [("aTo", mi), kWc], [("ps", bA)])
                        for k in range(16):
                            mm(ps[:, bA, 256:512], at[:, k, :], Wh[:, k, :], k == 0, k == 15, [("aTo", mi), kWh], [("ps", bA)])
                        for k in range(16):
                            mm(ps[:, bB, 0:256], at[:, k, :], Wb[:, k, :], k == 0, k == 15, [("aTo", mi), kWb], [("ps", bB)])
                        act(ccs[sl], ps[:, bA, 0:256], AF.Copy, [("ps", bA)], [("ccs", sl)])
                        vtt(uu[sl], ps[:, bA, 256:512], ccs[sl], ALU.mult, [("ps", bA), ("ccs", sl)], [("uu", sl)])
                        vts(uh[sl], u_halo[:, c0_:c1_], hmask[:, m:m + 1], None, ALU.mult, None, ["u_halo", "hmask"], [("uh", sl)])
                        mm(ps[:, bC, 0:256], shf[:, 0, :], uu[sl], True, False, ["shf", ("uu", sl)], [("ps", bC)])
                        mm(ps[:, bC, 0:256], hfix[:, 0, :], uh[sl], False, True, ["hfix", ("uh", sl)], [("ps", bC)])
                        mm(ps[:, bC, 256:512], shf[:, 1, :], uu[sl], True, False, ["shf", ("uu", sl)], [("ps", bC)])
                        mm(ps[:, bC, 256:512], hfix[:, 1, :], uh[sl], False, True, ["hfix", ("uh", sl)], [("ps", bC)])
                        vtt(aa[sl], uu[sl], wcv[:, 2, c0_:c1_], ALU.mult, [("uu", sl), ("wcv", 2)], [("aa", sl)])
                        vtt(tb_[sl], ps[:, bC, 0:256], wcv[:, 1, c0_:c1_], ALU.mult, [("ps", bC), ("wcv", 1)], [("tb", sl)])
                        vtt(aa[sl], aa[sl], tb_[sl], ALU.add, [("aa", sl), ("tb", sl)], [("aa", sl)])
                        vtt(tb_[sl], ps[:, bC, 256:512], wcv[:, 0, c0_:c1_], ALU.mult, [("ps", bC), ("wcv", 0)], [("tb", sl)])
                        vtt(aa[sl], aa[sl], tb_[sl], ALU.add, [("aa", sl), ("tb", sl)], [("aa", sl)])
                        vtt(ycb[:, mi, c0_:c1_], ps[:, bB, 0:256], aa[sl], ALU.mult, [("ps", bB), ("aa", sl)], [("ycb", mi, cg)])
                        act(junk[:, 0:256], ycb[:, mi, c0_:c1_], AF.Square, [("ycb", mi, cg)], ["junk", ("ssq", mi, cg)],
                            accum_out=ssq[:, mi, cg:cg + 1])
                for qc in range(4):
                    Wq, kWq = next_w(w_in[:, 3072 + qc * 256:3072 + (qc + 1) * 256])
                    for mi in range(8):
                        m = hf * 8 + mi
                        sl = mi % 2
                        bq = 2 if sl == 0 else 5
                        at = aTo[:, :, mi * 128:(mi + 1) * 128]
                        for k in range(16):
                            mm(ps[:, bq, 0:256], at[:, k, :], Wq[:, k, :], k == 0, k == 15, [("aTo", mi), kWq], [("ps", bq)])
                        z = ps[:, bq, 0:256].rearrange("p (h f x) -> p h f x", h=2, f=2)
                        cosb = cso[:, mi, 0, :].unsqueeze(1).to_broadcast([128, 2, 64])
                        sinb = cso[:, mi, 1, :].unsqueeze(1).to_broadcast([128, 2, 64])
                        kz = [("ps", bq), "cso"]
                        vtt(qr1, z[:, :, 0, :], cosb, ALU.mult, kz, ["qr1"])
                        vtt(qr2, z[:, :, 1, :], sinb, ALU.mult, kz, ["qr2"])
                        vtt(qrope[sl][:, :, 0, :], qr1, qr2, ALU.subtract, ["qr1", "qr2"], [("qrope0", sl)])
                        vtt(qr1, z[:, :, 1, :], cosb, ALU.mult, kz, ["qr1"])
                        vtt(qr2, z[:, :, 0, :], sinb, ALU.mult, kz, ["qr2"])
                        vtt(qrope[sl][:, :, 1, :], qr1, qr2, ALU.add, ["qr1", "qr2"], [("qrope1", sl)])
                        bt = 3 if sl == 0 else 6
                        ptq = bank_bf(bt)[:, 0, 0:256].rearrange("p (h t) -> p h t", h=2)
                        for hh in range(2):
                            tr(ptq[:, hh, :], qrope[sl][:, hh, :, :].rearrange("p f x -> p (f x)"), identB[:],
                               [("qrope0", sl), ("qrope1", sl), "identB"], [("ps", bt)])
                        act(qts[sl], ptq, AF.Copy, [("ps", bt)], [("qts", sl)])
                        dma("sync", qTs[m, :, qc * 2:qc * 2 + 2, :], qts[sl], [("qts", sl)], [("qTs", m, qc)], f"qts{sl}")
                load_w(wgt, w_in[:, 5632:5656], "wgt", "wgt")
                for mi in range(8):
                    m = hf * 8 + mi
                    bq = 4 if mi % 2 == 0 else 7
                    at = aTo[:, :, mi * 128:(mi + 1) * 128]
                    for k in range(16):
                        mm(ps[:, bq, 0:24], at[:, k, :], wgt[:, k, :], k == 0, k == 15, [("aTo", mi), "wgt"], [("ps", bq)])
                    act(gates[:, m, :], ps[:, bq, 0:24], AF.Sigmoid, [("ps", bq)], [("gates", m)])
                for mi in range(8):
                    m = hf * 8 + mi
                    sl = mi % 2
                    P.op("vector", lambda e, mi=mi: e.tensor_reduce(out=stat[:, 8:9], in_=ssq[:, mi, :], axis=AX.X, op=ALU.add),
                         [("ssq", mi, c) for c in range(4)], [("stat", 8)])
                    act(stat[:, 9:10], stat[:, 8:9], AF.Sqrt, [("stat", 8)], [("stat", 9)], scale=1.0 / 1024, bias=eps_t[:])
                    vrecip(stat[:, 10:11], stat[:, 9:10], [("stat", 9)], [("stat", 10)])
                    vstt(ycn, ycb[:, mi, :], stat[:, 10:11], gconv, ALU.mult, ALU.mult,
                         [("ycb", mi, c) for c in range(4)] + [("stat", 10), "gconv"], ["ycn"])
                    bt = 3 if sl == 0 else 6
                    pty = bank_bf(bt).rearrange("p a (c t) -> p (a c) t", t=128)
                    for c in range(8):
                        tr(pty[:, c, :], ycn[:, c * 128:(c + 1) * 128], identB[:], ["ycn", "identB"], [("ps", bt)])
                    act(ymT[sl], pty, AF.Copy, [("ps", bt)], [("ymT", sl)])
                    dma("sync", ymixT[m, :, 0:8, :], ymT[sl], [("ymT", sl)], [("ymixT", m, 0)], f"ymT{sl}")
            if DEBUG:
                dma("sync", dbg_gates, gates[:].rearrange("p a b -> p (a b)"), [("gates", m) for m in range(16)], ["dbg_g"], "dbg3")
            P.barrier()
            if KSTOP == "0":
                break

            AR.reset()
            ksT = AR.alloc([128, 2, S], BF16)
            vsA = AR.alloc([128, 64, 2, 130], BF16)
            Eall = AR.alloc([128, 64, 128], BF16)
            cbs = AR.alloc([128, 4, 4, 128], BF16)
            cbw = AR.alloc([128, 8, 4, 128], BF16)
            cbs_f = AR.alloc([128, 8, 128], F32)
            gattn = AR.alloc([128, 1024], F32)
            sfz = AR.alloc([128, 9], F32)
            suz = AR.alloc([128, 9], F32)
            qT = [AR.alloc([128, 8, 128], BF16) for _ in range(2)]
            kwT = [AR.alloc([128, 2, 1024], BF16) for _ in range(2)]
            vwA = [AR.alloc([128, 8, 2, 130], BF16) for _ in range(2)]
            cbc_f = [AR.alloc([128, 2, 128], F32) for _ in range(2)]
            cbc = [AR.alloc([128, 2, 4, 128], BF16) for _ in range(2)]
            cbT = [AR.alloc([128, 64], F32) for _ in range(2)]
            ee = [AR.alloc([128, 512], F32) for _ in range(2)]
            psum_ = AR.alloc([128, 512], F32)
            imp = AR.alloc([128, 128], F32)
            imp2 = AR.alloc([128, 128], F32)
            m8 = AR.alloc([128, 16], F32)
            selb = AR.alloc([128, 128], F32)
            sbT = [AR.alloc([128, 4, 128], BF16) for _ in range(2)]
            pT = [AR.alloc([128, 512], BF16) for _ in range(3)]
            yat = AR.alloc([128, 8, 128], F32)
            ytmp = AR.alloc([128, 8, 128], F32)
            coef = AR.alloc([128, 8], F32)
            dden = AR.alloc([128, 8], F32)
            yan = AR.alloc([128, 1024], BF16)
            ymT2 = [AR.alloc([128, 8, 128], BF16) for _ in range(2)]
            Dh = AR.alloc([128, 8], F32)

            for hk in range(2):
                dma("sync", ksT[:, hk, :], KTs[1, hk, :, :], (), [("ksT", hk)], f"ksT{hk}")
            for qd in range(4):
                dma("sync", vsA[:, qd * 16:(qd + 1) * 16, :, :].rearrange("p b h c -> p b (h c)"),
                    VSc[0, qd * 2048:(qd + 1) * 2048, :, :].rearrange("(b p) h c -> p b (h c)", p=128), (), [("vsA", qd)], f"vsA{qd}")
            for qd in range(8):
                dma("gpsimd", Eall[:, qd * 8:(qd + 1) * 8, :], E_all_d[:, qd * 1024:(qd + 1) * 1024].rearrange("p (a b) -> p a b", a=8),
                    (), [("Eall", qd // 2)], f"Eall{qd}")
            dma("sync", cbs_f[:, 0:4, :], cb_sel_d.rearrange("p (a b) -> p a b", a=4), (), ["cbs_f"], "c_cbs")
            vcopy(cbs, cbs_f[:, 0:4, :].unsqueeze(2).to_broadcast([128, 4, 4, 128]), ["cbs_f"], ["cbs"])
            dma("sync", cbs_f, cb_win_d.rearrange("p (a b) -> p a b", a=8), ["cbs_f"], ["cbs_f"], "c_cbs")
            vcopy(cbw, cbs_f.unsqueeze(2).to_broadcast([128, 8, 4, 128]), ["cbs_f"], ["cbw"])
            bc_load(gattn, g_attn, "gattn", "c_gattn")
            dma("sync", sfz, self_force_d, (), ["sfz"], "c_sfz")
            dma("sync", suz, self_future_d, (), ["suz"], "c_suz")
            vmemset(imp, -1.0, ["imp"])

            sctr = [0]
            pctr = [0]
            S_BANKS = [0, 1]
            O_BANK0 = 2

            def o_view(h):
                return ps[:, O_BANK0 + h // 2, (h % 2) * 256:(h % 2) * 256 + 129]

            def attend(m, hk, visits, o_started):
                qsl = m % 2
                qrhs = qT[qsl][:, 4 * hk:4 * hk + 4, :].rearrange("p g q -> p (g q)")
                def scores(vi):
                    kT_ap, kkeys, v_ap, vkeys, biases = visits[vi]
                    sb_ = S_BANKS[sctr[0] % 2]
                    sctr[0] += 1
                    nb = len(biases)
                    mm(bank(sb_), kT_ap, qrhs, True, nb == 0, kkeys + [("qT", qsl)], [("ps", sb_)])
                    for bi, (bl, br, bk) in enumerate(biases):
                        mm(bank(sb_), bl, br, False, bi == nb - 1, bk, [("ps", sb_)])
                    return sb_

                cur = scores(0)
                for vi in range(len(visits)):
                    kT_ap, kkeys, v_ap, vkeys, biases = visits[vi]
                    last = vi == len(visits) - 1
                    nxt = scores(vi + 1) if not last else None
                    sb_ = cur
                    pi = pctr[0] % 3
                    pctr[0] += 1
                    act(pT[pi], bank(sb_), AF.Exp, [("ps", sb_)], [("pT", pi)], scale=SCALE)
                    for g_ in range(4):
                        h = 4 * hk + g_
                        bnk = O_BANK0 + h // 2
                        st = bnk not in o_started
                        o_started.add(bnk)
                        mm(o_view(h), pT[pi][:, g_ * 128:(g_ + 1) * 128], v_ap, st, last, [("pT", pi)] + vkeys,
                           [("ps", bnk)], skip=True)
                    cur = nxt

            def finalize(m, br, first):
                ov = ps[:, O_BANK0:O_BANK0 + 4, :].rearrange("p b (h x) -> p (b h) x", h=2)
                okeys = [("ps", O_BANK0 + b_) for b_ in range(4)]
                vts(dden, ov[:, :, 128], 1e-30, None, ALU.max, None, okeys, ["dden"])
                vrecip(dden, dden, ["dden"], ["dden"])
                gv = gates[:, m, :].rearrange("p (h b) -> p h b", b=3)[:, :, br]
                vtt(coef, dden, gv, ALU.mult, ["dden", ("gates", m)], ["coef"])
                cb_ = coef[:, :].unsqueeze(2).to_broadcast([128, 8, 128])
                if first:
                    vtt(yat, ov[:, :, 0:128], cb_, ALU.mult, okeys + ["coef"], ["yat"])
                else:
                    vtt(ytmp, ov[:, :, 0:128], cb_, ALU.mult, okeys + ["coef"], ["ytmp"])
                    vtt(yat, yat, ytmp, ALU.add, ["yat", "ytmp"], ["yat"])

            def issue_loads(m):
                qsl = m % 2
                dma("sync", qT[qsl], qTs[m], (), [("qT", qsl)], f"qT{qsl}")
                t0 = 512 * (m - 1)
                for hk in range(2):
                    if m == 0:
                        dma("sync", kwT[qsl][:, hk, 512:1024], KTs[2, hk, :, 0:512], (), [("kwT", qsl)], f"kwT{qsl}")
                    else:
                        dma("sync", kwT[qsl][:, hk, :], KTs[2, hk, :, t0:t0 + 1024], (), [("kwT", qsl)], f"kwT{qsl}")
                if m == 0:
                    dma("sync", vwA[qsl][:, 4:8, :, :].rearrange("p b h c -> p b (h c)"),
                        VSc[1, 0:512, :, :].rearrange("(b p) h c -> p b (h c)", p=128), (), [("vwA", qsl)], f"vwA{qsl}")
                else:
                    dma("sync", vwA[qsl].rearrange("p b h c -> p b (h c)"),
                        VSc[1, t0:t0 + 1024, :, :].rearrange("(b p) h c -> p b (h c)", p=128), (), [("vwA", qsl)], f"vwA{qsl}")
                dma("sync", cbc_f[qsl], cb_cmp_d[m].rearrange("p (a b) -> p a b", a=2), (), [("cbc_f", qsl)], f"cbcf{qsl}")
                dma("sync", cbT[qsl], cbT_cmp_d[m], (), [("cbT", qsl)], f"cbT{qsl}")

            issue_loads(0)
            for m in range(16):
                qsl = m % 2
                nwin0 = 4 if m == 0 else 0
                if m + 1 < 16:
                    issue_loads(m + 1)
                vcopy(cbc[qsl], cbc_f[qsl].unsqueeze(2).to_broadcast([128, 2, 4, 128]), [("cbc_f", qsl)], [("cbc", qsl)])
                Nm = 32 * (m + 1)
                ns = 8 * (m + 1)
                NBc = m // 4 + 1
                zlo = max(0, 32 * m - 32)
                zc0 = zlo - (32 * m - 32)
                for hk in range(2):
                    for g_ in range(4):
                        h = 4 * hk + g_
                        tbk = 6 + (h % 2)
                        esl = h % 2
                        mm(ps[:, tbk, 0:Nm], qT[qsl][:, h, :], kcT[:, hk, 0:Nm], True, True, [("qT", qsl), "kcT"], [("ps", tbk)])
                        vtt(ps[:, tbk, zlo:Nm], ps[:, tbk, zlo:Nm], cbT[qsl][:, zc0:64], ALU.add,
                            [("ps", tbk), ("cbT", qsl)], [("ps", tbk)])
                        act(ee[esl][:, 0:Nm], ps[:, tbk, 0:Nm], AF.Exp, [("ps", tbk)], [("ee", esl), ("Dh", h)],
                            scale=SCALE, accum_out=Dh[:, h:h + 1])
                        vts(Dh[:, h:h + 1], Dh[:, h:h + 1], 1e-30, None, ALU.max, None, [("Dh", h)], [("Dh", h)])
                        vrecip(Dh[:, h:h + 1], Dh[:, h:h + 1], [("Dh", h)], [("Dh", h)])
                        if g_ == 0:
                            vts(psum_[:, 0:Nm], ee[esl][:, 0:Nm], Dh[:, h:h + 1], None, ALU.mult, None,
                                [("ee", esl), ("Dh", h)], ["psum"])
                        else:
                            vstt(psum_[:, 0:Nm], ee[esl][:, 0:Nm], Dh[:, h:h + 1], psum_[:, 0:Nm], ALU.mult, ALU.add,
                                 [("ee", esl), ("Dh", h), "psum"], ["psum"])
                    P.op("vector", lambda e, Nm=Nm, ns=ns: e.tensor_reduce(
                        out=imp[:, 0:ns], in_=psum_[:, 0:Nm].rearrange("p (s f) -> p s f", f=4), axis=AX.X, op=ALU.add),
                        ["psum"], ["imp"])
                    vtt(imp[:, 1:ns], imp[:, 1:ns], psum_[:, 0:Nm].rearrange("p (s f) -> p s f", f=4)[:, 0:ns - 1, 3], ALU.add,
                        ["imp", "psum"], ["imp"])
                    vmemset(imp[:, 0:1], 10.0, ["imp"])
                    if m == 0:
                        zs, zt = imp[:, 0:8], slice(1, 9)
                    else:
                        zs, zt = imp[:, 8 * m - 1:8 * m + 8], slice(0, 9)
                    vtt(zs, zs, sfz[:, zt], ALU.max, ["imp", "sfz"], ["imp"])
                    vtt(zs, zs, suz[:, zt], ALU.min, ["imp", "suz"], ["imp"])
                    P.op("vector", lambda e: e.max(out=m8[:, 0:8], in_=imp[:, :]), ["imp"], ["m8a"])
                    P.op("vector", lambda e: e.match_replace(out=imp2[:, :], in_to_replace=m8[:, 0:8], in_values=imp[:, :],
                                                             imm_value=-2.0), ["imp", "m8a"], ["imp2"])
                    P.op("vector", lambda e: e.max(out=m8[:, 8:16], in_=imp2[:, :]), ["imp2"], ["m8b"])
                    vts(selb, imp, m8[:, 15:16], 1.0, ALU.is_ge, ALU.subtract, ["imp", "m8b"], ["selb"])
                    tr(ps[:, 6, 0:128], selb, identF[:], ["selb", "identF"], [("ps", 6)])
                    act(sbT[hk], ps[:, 6, 0:128].unsqueeze(1).to_broadcast([128, 4, 128]), AF.Copy, [("ps", 6)], [("sbT", hk)],
                        scale=-NEG)
                idB = identB[:]
                o_started = set()
                for hk in range(2):
                    visits = []
                    for nb in range(NBc):
                        biases = []
                        w_ = nb - (NBc - 2)
                        if w_ >= 0:
                            biases.append((idB, cbc[qsl][:, w_, :, :].rearrange("p g q -> p (g q)"), ["identB", ("cbc", qsl)]))
                        visits.append((kcT[:, hk, nb * 128:(nb + 1) * 128], ["kcT"], vcc[:, nb, hk, 0:129], ["vcc", "vcc1"], biases))
                    attend(m, hk, visits, o_started)
                finalize(m, 0, True)
                o_started = set()
                for hk in range(2):
                    visits = []
                    for jj in range(nwin0, 8):
                        biases = [(idB, cbw[:, jj, :, :].rearrange("p g q -> p (g q)"), ["identB", "cbw"])]
                        visits.append((kwT[qsl][:, hk, jj * 128:(jj + 1) * 128], [("kwT", qsl)], vwA[qsl][:, jj, hk, 0:129],
                                       [("vwA", qsl)], biases))
                    attend(m, hk, visits, o_started)
                finalize(m, 2, False)
                o_started = set()
                for hk in range(2):
                    visits = []
                    for kb in range(4 * m + 4):
                        biases = [(Eall[:, kb, :], sbT[hk].rearrange("p g q -> p (g q)"), [("Eall", kb // 16), ("sbT", hk)])]
                        if kb >= 4 * m:
                            biases.append((idB, cbs[:, kb - 4 * m, :, :].rearrange("p g q -> p (g q)"), ["identB", "cbs"]))
                        visits.append((ksT[:, hk, kb * 128:(kb + 1) * 128], [("ksT", hk)], vsA[:, kb, hk, 0:129],
                                       [("vsA", kb // 16)], biases))
                    attend(m, hk, visits, o_started)
                finalize(m, 1, False)
                yflat = yat.rearrange("p h x -> p (h x)")
                rs, krs = rms_rstd(yflat, 1024, 16, "yat")
                vstt(yan, yflat, rs, gattn, ALU.mult, ALU.mult, ["yat", krs, "gattn"], ["yan"])
                pty = bank_bf(7).rearrange("p a (c t) -> p (a c) t", t=128)
                for c in range(8):
                    tr(pty[:, c, :], yan[:, c * 128:(c + 1) * 128], identB[:], ["yan", "identB"], [("ps", 7)])
                act(ymT2[qsl], pty, AF.Copy, [("ps", 7)], [("ymT2", qsl)])
                dma("sync", ymixT[m, :, 8:16, :], ymT2[qsl], [("ymT2", qsl)], [("ymixT", m, 1)], f"ymT2{qsl}")
            P.barrier()
            if KSTOP == "B":
                break

            AR.reset()
            gbuf = AR.alloc([128, 2048], F32)
            ymx = [AR.alloc([128, 16, 128], BF16) for _ in range(4)]
            hh_ = AR.alloc([128, 4, 2048], F32)
            xb = AR.alloc([128, 2048], BF16)
            fT = AR.alloc([128, 16, 512], BF16)
            actT = AR.alloc([128, 44, 512], BF16)
            wst = [AR.alloc([128, 16, 256], BF16) for _ in range(3)]
            stg = [AR.alloc([128, 2816], F32) for _ in range(2)]
            wdn = [AR.alloc([128, 11, 256], BF16) for _ in range(2)]
            sg = [AR.alloc([128, 512], F32) for _ in range(2)]
            wc2 = [0]

            cst = [0]

            def load_cast(dst, src, kdst, a, n):
                i = cst[0] % 2
                cst[0] += 1
                st = stg[i][:, 0:a * n].rearrange("p (a n) -> p a n", a=a)
                dma("sync", st, src, (), [("stg", i)], f"stg{i}")
                if i == 0:
                    vcopy(dst, st, [("stg", i)], [kdst])
                else:
                    act(dst, st, AF.Copy, [("stg", i)], [kdst])

            def next_w2(src_cols):
                i = wc2[0] % 3
                wc2[0] += 1
                sv = src_cols.rearrange("(kc p) n -> p kc n", p=128)
                for hf_ in range(2):
                    load_cast(wst[i][:, hf_ * 8:(hf_ + 1) * 8, :], sv[:, hf_ * 8:(hf_ + 1) * 8, :], ("wst", i), 8, 256)
                return wst[i], ("wst", i)

            dctr = [0]
            octr = [0]
            for tt in range(4):
                for tb in range(4):
                    m = tt * 4 + tb
                    dma("sync", hh_[:, tb, :], xown[m * 128:(m + 1) * 128, :], (), [("hh", tb)], f"hh{tb}")
                    dma("sync", ymx[tb], ymixT[m], (), [("ymx", tb)], f"ymx{tb}")
                for oc in range(8):
                    Wo, kWo = next_w2(w_out[:, oc * 256:(oc + 1) * 256])
                    for tb in range(4):
                        m = tt * 4 + tb
                        sl = tb
                        ob = octr[0] % 2
                        octr[0] += 1
                        for k in range(16):
                            mm(ps[:, ob, 0:256], ymx[sl][:, k, :], Wo[:, k, :], k == 0, k == 15, [("ymx", sl), kWo], [("ps", ob)])
                        vtt(hh_[:, tb, oc * 256:(oc + 1) * 256], ps[:, ob, 0:256], hh_[:, tb, oc * 256:(oc + 1) * 256], ALU.add,
                            [("ps", ob), ("hh", tb)], [("hh", tb)])
                bc_load(gbuf, g_ffn, "gbuf", "c_gbuf")
                for tb in range(4):
                    norm_transpose(hh_[:, tb, :], ("hh", tb), gbuf, "gbuf", xb, "xb", 2 if tb % 2 == 0 else 4,
                                   fT[:, :, tb * 128:(tb + 1) * 128], ("fT", tb), 20)
                fkeys = [("fT", tb) for tb in range(4)]
                chunks = []
                for hp in range(22):
                    chunks.append(w_gate[:, hp * 256:(hp + 1) * 256])
                    chunks.append(w_up[:, hp * 256:(hp + 1) * 256])
                nxt_w = next_w2(chunks[0])
                for n_ in range(44):
                    W_, kW_ = nxt_w
                    if n_ + 1 < 44:
                        nxt_w = next_w2(chunks[n_ + 1])
                    hp = n_ // 2
                    base = 0 if hp % 2 == 0 else 4
                    if n_ % 2 == 0:
                        for c2 in range(2):
                            bk_ = base + 2 * c2
                            for k in range(16):
                                mm(bank(bk_), W_[:, k, c2 * 128:(c2 + 1) * 128], fT[:, k, :], k == 0, k == 15,
                                   [kW_] + fkeys, [("ps", bk_)])
                    else:
                        for c2 in range(2):
                            bk_ = base + 2 * c2 + 1
                            for k in range(16):
                                mm(bank(bk_), W_[:, k, c2 * 128:(c2 + 1) * 128], fT[:, k, :], k == 0, k == 15,
                                   [kW_] + fkeys, [("ps", bk_)])
                        for c2 in range(2):
                            hc = hp * 2 + c2
                            gb = base + 2 * c2
                            act(sg[c2], bank(gb), AF.Silu, [("ps", gb)], [("sg", c2)])
                            vtt(actT[:, hc, :], bank(gb + 1), sg[c2], ALU.mult, [("ps", gb + 1), ("sg", c2)], [("actT", hc)])
                wdv = w_down.rearrange("(hc p) n -> p hc n", p=128)
                for cg in range(8):
                    for hq in range(4):
                        di = dctr[0] % 2
                        dctr[0] += 1
                        load_cast(wdn[di], wdv[:, hq * 11:(hq + 1) * 11, cg * 256:(cg + 1) * 256], ("wdn", di), 11, 256)
                        for tb in range(4):
                            for c in range(11):
                                hc = hq * 11 + c
                                mm(ps[:, 4 + tb, 0:256], actT[:, hc, tb * 128:(tb + 1) * 128], wdn[di][:, c, :], hc == 0, hc == 43,
                                   [("actT", hc), ("wdn", di)], [("ps", 4 + tb)])
                    for tb in range(4):
                        vtt(hh_[:, tb, cg * 256:(cg + 1) * 256], ps[:, 4 + tb, 0:256], hh_[:, tb, cg * 256:(cg + 1) * 256], ALU.add,
                            [("ps", 4 + tb), ("hh", tb)], [("hh", tb)])
                bc_load(gbuf, g_fin, "gbuf", "c_gbuf")
                for tb in range(4):
                    m = tt * 4 + tb
                    rs, krs = rms_rstd(hh_[:, tb, :], 2048, 24, ("hh", tb))
                    vstt(hh_[:, tb, :], hh_[:, tb, :], rs, gbuf, ALU.mult, ALU.mult, [("hh", tb), krs, "gbuf"], [("hh", tb)])
                    dma("sync", y[m * 128:(m + 1) * 128, :], hh_[:, tb, :], [("hh", tb)], [("y", m)], f"yout{tb}")
            o_ = P.op("sync", lambda e: e.nop(), [("y", m) for m in range(16)], ())
            o_.is_nop = True
            P.barrier()
        P.emit(nc, ctx)
    return nc


def _tables(j):
    f32 = np.float32
    k = np.arange(128)[:, None]
    q = np.arange(128)[None, :]
    cb_sel = np.zeros((128, 4, 128), f32)
    for jj in range(4):
        if jj < j:
            v = np.ones((128, 128), bool)
        elif jj == j:
            v = k <= q
        else:
            v = np.zeros((128, 128), bool)
        cb_sel[:, jj, :] = np.where(v, 0.0, NEG)
    cb_win = np.zeros((128, 8, 128), f32)
    for jj in range(8):
        delta = 128 * (j + 4 - jj) + q - k
        cb_win[:, jj, :] = np.where((delta >= 0) & (delta < 512), 0.0, NEG)
    cb_cmp = np.zeros((16, 128, 2, 128), f32)
    cbT_cmp = np.zeros((16, 128, 64), f32)
    for m in range(16):
        t = 128 * (4 * m + j) + np.arange(128)
        NBc = m // 4 + 1
        for w in range(2):
            nb = NBc - 2 + w
            n = 128 * nb + np.arange(128)
            valid = (16 * n[:, None] + 31 <= t[None, :]) & (n[:, None] >= 0)
            cb_cmp[m, :, w, :] = np.where(valid, 0.0, NEG)
        n = 32 * m - 32 + np.arange(64)
        valid = 16 * n[None, :] + 31 <= t[:, None]
        cbT_cmp[m] = np.where(valid, 0.0, NEG)
    qq = np.arange(128)[:, None]
    c = np.arange(9)[None, :]
    rel = c - 1 - 2 * j - (qq >= 64)
    sel_force = np.where((rel == 0) | (rel == -1), 10.0, -1e9).astype(f32)
    sel_future = np.where(rel > 0, -1.0, 1e9).astype(f32)
    return dict(cb_sel=cb_sel.reshape(128, 512), cb_win=cb_win.reshape(128, 1024),
                cb_cmp=cb_cmp.reshape(16, 128, 256), cbT_cmp=cbT_cmp, sel_force=sel_force, sel_future=sel_future)


def _shared_tables():
    f32 = np.float32
    half = 64
    inv = 1.0 / (10000.0 ** (np.arange(half, dtype=np.float32) / half))
    ang = np.arange(S, dtype=np.float32)[:, None] * inv[None, :]
    cs_all = np.concatenate([np.cos(ang), np.sin(ang)], axis=1).astype(f32)
    s = np.arange(128)[:, None, None]
    kb = np.arange(64)[None, :, None]
    kk = np.arange(128)[None, None, :]
    E_all = (s == 2 * kb + kk // 64).astype(f32).reshape(128, 64 * 128)
    kr = np.arange(128)[:, None]
    mc = np.arange(128)[None, :]
    Sh = np.stack([(kr == mc - 1), (kr == mc - 2)], axis=1).astype(f32).reshape(128, 256)
    Hh = np.zeros((32, 272), f32)
    r = np.arange(32)
    Hh[r % 2 == 1, 0] = 1.0
    Hh[r % 2 == 0, 128 + 0] = 1.0
    Hh[r % 2 == 1, 128 + 1] = 1.0
    for m in range(16):
        Hh[2 * m:2 * m + 2, 256 + m] = 1.0
    return dict(cs_all=cs_all, E_all=E_all, Sh=Sh, Hh=Hh, ident=np.eye(128, dtype=f32))


_NC_CACHE = {}


def kernel(**inputs):
    x = np.asarray(inputs["x"], dtype=np.float32)
    sh = _shared_tables()
    base = {
        "w_in": np.ascontiguousarray(inputs["w_in"][0]),
        "w_out": np.ascontiguousarray(inputs["w_out"][0]),
        "w_gate": np.ascontiguousarray(inputs["w_gate"][0]),
        "w_up": np.ascontiguousarray(inputs["w_up"][0]),
        "w_down": np.ascontiguousarray(inputs["w_down"][0]),
        "conv_w": np.ascontiguousarray(inputs["conv_w"][0]),
        "cmp_pe_k": np.ascontiguousarray(inputs["cmp_pe_k"][0]),
        "cmp_w1_k": np.ascontiguousarray(inputs["cmp_w1_k"][0]),
        "cmp_w2_k": np.ascontiguousarray(inputs["cmp_w2_k"][0]),
        "cmp_pe_v": np.ascontiguousarray(inputs["cmp_pe_v"][0]),
        "cmp_w1_v": np.ascontiguousarray(inputs["cmp_w1_v"][0]),
        "cmp_w2_v": np.ascontiguousarray(inputs["cmp_w2_v"][0]),
        "norm_mix": np.ascontiguousarray(inputs["norm_mix"][0]),
        "norm_conv_out": np.ascontiguousarray(inputs["norm_conv_out"][0]),
        "norm_attn_out": np.ascontiguousarray(inputs["norm_attn_out"][0]),
        "norm_ffn": np.ascontiguousarray(inputs["norm_ffn"][0]),
        "norm_final": np.ascontiguousarray(inputs["norm_final"]),
    }
    base = {k: np.asarray(v, dtype=np.float32) for k, v in base.items()}
    base.update(sh)
    in_maps = []
    for c in range(8):
        b, j = c // 4, c % 4
        blocks = [4 * m + j for m in range(16)]
        xb_ = x[b]
        xown = np.concatenate([xb_[128 * qb:128 * qb + 128] for qb in blocks], axis=0)
        xhalo = np.zeros((32, D), np.float32)
        for m, qb in enumerate(blocks):
            if qb > 0:
                xhalo[2 * m:2 * m + 2] = xb_[128 * qb - 2:128 * qb]
        cs_own = np.concatenate([sh["cs_all"][128 * qb:128 * qb + 128] for qb in blocks], axis=0)
        im = dict(base)
        im.update(_tables(j))
        im.update(xall=np.ascontiguousarray(xb_[:KS]), xown=np.ascontiguousarray(xown), xhalo=xhalo,
                  cs_own=np.ascontiguousarray(cs_own))
        im["cs_all"] = np.ascontiguousarray(im["cs_all"][:KS])
        if SHRINK_KEEP is not None:
            im = {k: (v if k in SHRINK_KEEP else np.zeros((1, 1), np.float32)) for k, v in im.items()}
        in_maps.append(im)
    if "nc" not in _NC_CACHE:
        _NC_CACHE["nc"] = build_nc()
    nc = _NC_CACHE["nc"]
    res = run_bass_kernel_spmd(nc, in_maps[:KCORES], core_ids=list(range(KCORES)))
    out = np.zeros((2, S, D), np.float32)
    for c in range(KCORES):
        b, j = c // 4, c % 4
        yv = res.results[c]["y"]
        for m in range(16):
            qb = 4 * m + j
            out[b, 128 * qb:128 * qb + 128] = yv[128 * m:128 * m + 128]
    if DEBUG:
        kernel.last_results = res.results
    return out
```

```python
import os
import numpy as np
import concourse.bass as bass
import concourse.mybir as mybir
from concourse.bass_utils import run_bass_kernel_spmd
from contextlib import ExitStack

F32 = mybir.dt.float32
BF16 = mybir.dt.bfloat16
U8 = mybir.dt.uint8
AF = mybir.ActivationFunctionType
ALU = mybir.AluOpType
AX = mybir.AxisListType

D = 2048
S = 8192
DP = 5656
FF = 5632
NEG = -30000.0
SCALE = 128 ** -0.5
EPS = 1e-6
ENGS = ["tensor", "vector", "scalar", "gpsimd", "sync"]
DEBUG = bool(int(os.environ.get("KDEBUG", "0")))
KSTOP = os.environ.get("KSTOP", "")
KCORES = int(os.environ.get("KCORES", "8"))
KS = int(os.environ.get("KS", "8192"))
KSKIP = set(os.environ.get("KSKIP", "").split(","))
_UA = {"xall", "w_in", "norm_mix", "cs_all", "ident"}
_UA2 = _UA | {"cmp_pe_k", "cmp_w1_k", "cmp_w2_k", "cmp_pe_v", "cmp_w1_v", "cmp_w2_v"}
_U0 = _UA2 | {"xown", "xhalo", "conv_w", "norm_conv_out", "cs_own", "Sh", "Hh"}
_UB = _U0 | {"cb_sel", "cb_win", "cb_cmp", "cbT_cmp", "sel_force", "sel_future", "E_all", "norm_attn_out"}
_USED = {"A": _UA, "A2": _UA2, "0": _U0, "B": _UB}
SHRINK_KEEP = _USED.get(KSTOP)


class Op:
    __slots__ = ("eng", "fn", "deps", "needs_inc", "count", "grp", "is_nop")


class DmaGroup:
    __slots__ = ("stream", "n", "final", "last")


class Prog:
    def __init__(self):
        self.q = {e: [] for e in ENGS}
        self.keys = {}
        self.streams = {}

    def dma_group(self, stream):
        g = DmaGroup()
        g.stream = stream
        g.n = 0
        g.final = None
        g.last = None
        self.streams.setdefault(stream, []).append(g)
        return g

    def op(self, eng, fn, reads=(), writes=(), grp=None, extra=()):
        o = Op()
        o.eng = eng
        o.fn = fn
        o.needs_inc = False
        o.count = None
        o.grp = grp
        o.is_nop = False
        if grp is not None:
            grp.n += 1
            grp.last = o
        ident = eng if grp is None else ("dma", id(grp))
        deps = set(extra)
        writes = list(writes) + [k for k in reads if isinstance(k, tuple) and k[0] == "ps" and k not in writes]
        reads = [k for k in reads if not (isinstance(k, tuple) and k[0] == "ps")]
        for k in reads:
            st = self.keys.setdefault(k, ({}, {}))
            deps.update(st[0].values())
            st[1][ident] = o
        for k in writes:
            st = self.keys.setdefault(k, ({}, {}))
            deps.update(st[0].values())
            deps.update(st[1].values())
            if st[1]:
                st[0].clear()
                st[1].clear()
            st[0][ident] = o
        deps.discard(o)
        o.deps = deps
        self.q[eng].append(o)
        return o

    def barrier(self):
        arr = []
        for e in ENGS:
            for o in reversed(self.q[e]):
                if o.grp is None and not getattr(o, "is_nop", False):
                    arr.append(o)
                    break
        lasts = [g[-1].last for g in self.streams.values() if g and g[-1].last is not None]
        for e in ENGS:
            o = self.op(e, lambda eng: eng.nop(), extra=arr + lasts)
            o.is_nop = True
        self.keys = {}

    def emit(self, nc, ctx):
        engsem = {e: ctx.enter_context(nc.semaphore("es_" + e)) for e in ENGS}
        ssem = {}
        for s, groups in self.streams.items():
            ssem[s] = ctx.enter_context(nc.semaphore("ds_" + str(s)))
            cum = 0
            for g in groups:
                cum += 16 * g.n
                g.final = cum
        for e in ENGS:
            for o in self.q[e]:
                for d in o.deps:
                    if d.grp is None and not (d.eng == e and e == "tensor"):
                        d.needs_inc = True
        for e in ENGS:
            c = 0
            for o in self.q[e]:
                if o.grp is None and o.needs_inc:
                    c += 1
                    o.count = c
        block = ctx.enter_context(nc.Block())
        prog = self

        def run(e):
            def body(eng):
                waited = {}
                for o in prog.q[e]:
                    needs = {}
                    for d in o.deps:
                        if d.grp is not None:
                            s, v, key = ssem[d.grp.stream], d.grp.final, ("s", d.grp.stream)
                        else:
                            if d.eng == e and e == "tensor":
                                continue
                            s, v, key = engsem[d.eng], d.count, ("e", d.eng)
                        if key not in needs or needs[key][1] < v:
                            needs[key] = (s, v)
                    for key, (s, v) in needs.items():
                        if waited.get(key, 0) < v:
                            eng.wait_ge(s, v)
                            waited[key] = v
                    ins = o.fn(eng)
                    if o.grp is not None:
                        ins.then_inc(ssem[o.grp.stream], 16)
                    elif o.needs_inc:
                        ins.then_inc(engsem[e], 1)
            return body

        block.tensor(run("tensor"))
        block.vector(run("vector"))
        block.scalar(run("scalar"))
        block.gpsimd(run("gpsimd"))
        block.sync(run("sync"))


class Arena:
    def __init__(self, t, size):
        self.t = t
        self.size = size
        self.off = 0

    def reset(self):
        self.off = 0

    def alloc(self, shape, dt):
        esz = 4 if dt == F32 else (2 if dt == BF16 else 1)
        n = 1
        for s in shape[1:]:
            n *= s
        nb = (n * esz + 63) // 64 * 64
        assert self.off + nb <= self.size, ("arena overflow", self.off, nb, self.size)
        ap = self.t[0:shape[0], self.off:self.off + n * esz].bitcast(dt)
        self.off += nb
        if len(shape) == 3:
            ap = ap.rearrange("p (a b) -> p a b", a=shape[1])
        elif len(shape) == 4:
            ap = ap.rearrange("p (a b c) -> p a b c", a=shape[1], b=shape[2])
        elif len(shape) == 5:
            ap = ap.rearrange("p (a b c d) -> p a b c d", a=shape[1], b=shape[2], c=shape[3])
        return ap


def build_nc():
    nc = bass.Bass("TRN2", target_bir_lowering=False)

    def din(name, shape):
        if SHRINK_KEEP is not None and name not in SHRINK_KEEP:
            shape = [1, 1]
        return nc.dram_tensor(name, list(shape), F32, kind="ExternalInput").ap()

    xall = din("xall", [KS, D])
    xown = din("xown", [2048, D])
    xhalo = din("xhalo", [32, D])
    w_in = din("w_in", [D, DP])
    w_out = din("w_out", [D, D])
    w_gate = din("w_gate", [D, FF])
    w_up = din("w_up", [D, FF])
    w_down = din("w_down", [FF, D])
    conv_w = din("conv_w", [3, 1024])
    pe_k = din("cmp_pe_k", [32, 128])
    w1_k = din("cmp_w1_k", [4096, 128])
    w2_k = din("cmp_w2_k", [128, 128])
    pe_v = din("cmp_pe_v", [32, 128])
    w1_v = din("cmp_w1_v", [4096, 128])
    w2_v = din("cmp_w2_v", [128, 128])
    g_mix = din("norm_mix", [D])
    g_conv = din("norm_conv_out", [1024])
    g_attn = din("norm_attn_out", [1024])
    g_ffn = din("norm_ffn", [D])
    g_fin = din("norm_final", [D])
    cs_all = din("cs_all", [KS, 128])
    cs_own = din("cs_own", [2048, 128])
    cb_sel_d = din("cb_sel", [128, 4 * 128])
    cb_win_d = din("cb_win", [128, 8 * 128])
    cb_cmp_d = din("cb_cmp", [16, 128, 2 * 128])
    cbT_cmp_d = din("cbT_cmp", [16, 128, 64])
    self_force_d = din("sel_force", [128, 9])
    self_future_d = din("sel_future", [128, 9])
    E_all_d = din("E_all", [128, 64 * 128])
    Sh_d = din("Sh", [128, 2 * 128])
    Hh_d = din("Hh", [32, 272])
    ident_d = din("ident", [128, 128])
    y = nc.dram_tensor("y", [2048, D], F32, kind="ExternalOutput").ap()

    skind = dict(kind="ExternalOutput") if DEBUG else {}
    KTs = nc.dram_tensor("KTs", [4, 2, 128, KS], BF16, **skind).ap()
    VSc = nc.dram_tensor("VSc", [2, KS, 2, 130], BF16, **skind).ap()
    qTs = nc.dram_tensor("qTs", [16, 128, 8, 128], BF16, **skind).ap()
    ymixT = nc.dram_tensor("ymixT", [16, 128, 16, 128], BF16, **skind).ap()
    if DEBUG:
        dbg_kc = nc.dram_tensor("dbg_kc", [128, 2 * 512], BF16, kind="ExternalOutput").ap()
        dbg_vc = nc.dram_tensor("dbg_vc", [128, 4 * 2 * 130], BF16, kind="ExternalOutput").ap()
        dbg_gates = nc.dram_tensor("dbg_gates", [128, 16 * 24], F32, kind="ExternalOutput").ap()

    ctx = ExitStack()
    with ctx:
        P = Prog()
        AR_SIZE = 184 * 1024
        arena_t = ctx.enter_context(nc.sbuf_tensor("arena", [128, AR_SIZE], U8))
        AR = Arena(arena_t, AR_SIZE)

        def sbt(name, shape, dt):
            return ctx.enter_context(nc.sbuf_tensor(name, shape, dt))

        ps = ctx.enter_context(nc.psum_tensor("ps", [128, 8, 512], F32))

        def bank(i):
            return ps[:, i, :]

        def bank_bf(i, n=1):
            return ps[:, i:i + n, :].bitcast(BF16)

        identF = sbt("identF", [128, 128], F32)
        identB = sbt("identB", [128, 128], BF16)
        eps_t = sbt("eps_t", [128, 1], F32)
        kcT = sbt("kcT", [128, 2, 512], BF16)
        vcc = sbt("vcc", [128, 4, 2, 130], BF16)
        gates = sbt("gates", [128, 16, 24], F32)
        u_halo = sbt("u_halo", [32, 1024], F32)
        aT_halo = sbt("aT_halo", [128, 16, 32], BF16)
        junk = sbt("junk", [128, 2048], BF16)
        stat = sbt("stat", [128, 64], F32)

        def dma(queue, out, in_, reads, writes, stream, grp=None):
            g = grp if grp is not None else P.dma_group(stream)
            P.op(queue, lambda e: e.dma_start(out=out, in_=in_), reads, writes, grp=g)
            return g

        def mm(out, lhsT, rhs, start, stop, reads, writes, skip=False):
            P.op("tensor", lambda e: e.matmul(out, lhsT=lhsT, rhs=rhs, start=start, stop=stop,
                                              skip_group_check=skip), reads, writes)

        def tr(out, in_, ident, reads, writes):
            P.op("tensor", lambda e: e.transpose(out=out, in_=in_, identity=ident), reads, writes)

        def act(out, in_, func, reads, writes, **kw):
            P.op("scalar", lambda e: e.activation(out=out, in_=in_, func=func, **kw), reads, writes)

        def vtt(out, in0, in1, op, reads, writes):
            P.op("vector", lambda e: e.tensor_tensor(out=out, in0=in0, in1=in1, op=op), reads, writes)

        def vts(out, in0, s1, s2, op0, op1, reads, writes):
            if op1 is None:
                P.op("vector", lambda e: e.tensor_scalar(out=out, in0=in0, scalar1=s1, scalar2=None, op0=op0),
                     reads, writes)
            else:
                P.op("vector", lambda e: e.tensor_scalar(out=out, in0=in0, scalar1=s1, scalar2=s2, op0=op0, op1=op1),
                     reads, writes)

        def vstt(out, in0, scalar, in1, op0, op1, reads, writes):
            P.op("vector", lambda e: e.scalar_tensor_tensor(out=out, in0=in0, scalar=scalar, in1=in1,
                                                            op0=op0, op1=op1), reads, writes)

        def vcopy(out, in_, reads, writes):
            P.op("vector", lambda e: e.tensor_copy(out=out, in_=in_), reads, writes)

        def vrecip(out, in_, reads, writes):
            P.op("vector", lambda e: e.reciprocal(out=out, in_=in_), reads, writes)

        def vmemset(ap, val, writes):
            P.op("vector", lambda e: e.memset(ap, val), (), writes)

        def bc_load(dst, src_row, key, stream):
            dma("sync", dst, src_row.partition_broadcast(128), (), [key], stream)

        def rms_rstd(src, n, col, kin, np_=128):
            ss = stat[0:np_, col:col + 1]
            rt = stat[0:np_, col + 1:col + 2]
            rs = stat[0:np_, col + 2:col + 3]
            act(junk[0:np_, 0:n], src, AF.Square, [kin], ["junk", ("stat", col)], accum_out=ss)
            act(rt, ss, AF.Sqrt, [("stat", col)], [("stat", col + 1)], scale=1.0 / n, bias=eps_t[0:np_, :])
            vrecip(rs, rt, [("stat", col + 1)], [("stat", col + 2)])
            return rs, ("stat", col + 2)

        def norm_transpose(src, ksrc, gain_bc, kgain, xb, kxb, pbank, dst, kdst, col, np_=128):
            rs, krs = rms_rstd(src, 2048, col, ksrc, np_)
            vstt(xb[0:np_, :], src, rs, gain_bc[0:np_, :], ALU.mult, ALU.mult, [ksrc, krs, kgain], [kxb])
            pt = bank_bf(pbank, 2).rearrange("p a (c t) -> p (a c) t", t=128)
            idn = identB[0:np_, 0:np_]
            for k in range(16):
                tr(pt[:, k, 0:np_], xb[0:np_, k * 128:(k + 1) * 128], idn, [kxb, "identB"],
                   [("ps", pbank), ("ps", pbank + 1)])
            act(dst, pt[:, :, 0:np_], AF.Copy, [("ps", pbank), ("ps", pbank + 1)], [kdst])

        def load_w(dst, src_cols, key, stream):
            dma("gpsimd", dst, src_cols.rearrange("(kc p) n -> p kc n", p=128), (), [key], stream)

        dma("sync", identF[:], ident_d, (), ["identF"], "c_id")
        vcopy(identB[:], identF[:], ["identF"], ["identB"])
        vmemset(eps_t[:], EPS, ["eps"])
        P.barrier()
        for _once in (0,):

            AR.reset()
            wkv = AR.alloc([128, 16, 1536], BF16)
            gmix = AR.alloc([128, 2048], F32)
            xs = [AR.alloc([128, 2048], F32) for _ in range(2)]
            xb = AR.alloc([128, 2048], BF16)
            aT = [AR.alloc([128, 16, 128], BF16) for _ in range(2)]
            cs = [AR.alloc([128, 2, 64], F32) for _ in range(2)]
            rt1 = AR.alloc([128, 3, 2, 64], F32)
            rt2 = AR.alloc([128, 3, 2, 64], F32)
            krope = AR.alloc([128, 3, 2, 2, 64], BF16)
            vcb = AR.alloc([128, 2, 128], BF16)
            vst = [AR.alloc([128, 2, 2, 130], BF16) for _ in range(2)]
            kts = [AR.alloc([128, 8, 128], BF16) for _ in range(2)]

            for g in range(3):
                load_w(wkv[:, :, g * 512:(g + 1) * 512], w_in[:, 4096 + g * 512:4096 + (g + 1) * 512], ("wkv", g), f"wkv{g}")
            bc_load(gmix, g_mix, "gmix", "c_gmix")
            for s_ in range(2):
                if "ms" not in KSKIP:
                    vmemset(vst[s_][:, :, :, 128:130], 1.0, [("vst1", s_)])
            NB_A = int(os.environ.get("KNBA", "64"))
            dma("sync", xs[0], xall[0:128, :], (), [("xs", 0)], "xs0")
            if NB_A > 1:
                dma("sync", xs[1], xall[128:256, :], (), [("xs", 1)], "xs1")
            for i in range(NB_A):
                sl = i % 2
                dma("sync", cs[sl], cs_all[i * 128:(i + 1) * 128, :].rearrange("p (a b) -> p a b", a=2), (), [("cs", sl)], f"cs{sl}")
                if i == 0:
                    norm_transpose(xs[0], ("xs", 0), gmix, "gmix", xb, "xb", 0, aT[0], ("aT", 0), 0)
                for g in range(3 if "mm" not in KSKIP else 0):
                    for k in range(16):
                        mm(bank(2 + g), aT[sl][:, k, :], wkv[:, k, g * 512:(g + 1) * 512], k == 0, k == 15,
                           [("aT", sl), ("wkv", g)], [("ps", 2 + g)])
                if i + 1 < NB_A:
                    norm_transpose(xs[1 - sl], ("xs", 1 - sl), gmix, "gmix", xb, "xb", 6 if sl == 0 else 0,
                                   aT[1 - sl], ("aT", 1 - sl), 0)
                    if i + 2 < NB_A:
                        dma("sync", xs[sl], xall[(i + 2) * 128:(i + 3) * 128, :], (), [("xs", sl)], f"xs{sl}")
                if "rope" not in KSKIP:
                    z = ps[:, 2:5, 0:256].rearrange("p t (h f x) -> p t h f x", h=2, f=2)
                    cosb = cs[sl][:, 0, :].unsqueeze(1).unsqueeze(1).to_broadcast([128, 3, 2, 64])
                    sinb = cs[sl][:, 1, :].unsqueeze(1).unsqueeze(1).to_broadcast([128, 3, 2, 64])
                    pk = [("ps", 2), ("ps", 3), ("ps", 4)]
                    vtt(rt1, z[:, :, :, 0, :], cosb, ALU.mult, pk + [("cs", sl)], ["rt1"])
                    vtt(rt2, z[:, :, :, 1, :], sinb, ALU.mult, pk + [("cs", sl)], ["rt2"])
                    vtt(krope[:, :, :, 0, :], rt1, rt2, ALU.subtract, ["rt1", "rt2"], ["krope0"])
                    vtt(rt1, z[:, :, :, 1, :], cosb, ALU.mult, pk + [("cs", sl)], ["rt1"])
                    vtt(rt2, z[:, :, :, 0, :], sinb, ALU.mult, pk + [("cs", sl)], ["rt2"])
                    vtt(krope[:, :, :, 1, :], rt1, rt2, ALU.add, ["rt1", "rt2"], ["krope1"])
                if "vcopy" not in KSKIP:
                    act(vcb, ps[:, 2, 256:512].rearrange("p (h x) -> p h x", h=2), AF.Copy, [("ps", 2)], ["vcb"])
                    for t_ in range(2):
                        act(vst[sl][:, t_, :, 0:128], ps[:, 3 + t_, 256:512].rearrange("p (h x) -> p h x", h=2), AF.Copy,
                            [("ps", 3 + t_)], [("vst", sl)])
                if "tr5" not in KSKIP:
                    pk5 = bank_bf(5).rearrange("p a (c t) -> p (a c) t", t=128)
                    for t in range(3):
                        for hk in range(2):
                            tr(pk5[:, t * 2 + hk, :], krope[:, t, hk, :, :].rearrange("p f x -> p (f x)"), identB[:],
                               ["krope0", "krope1", "identB"], [("ps", 5)])
                    for hk in range(2):
                        tr(pk5[:, 6 + hk, :], vcb[:, hk, :], identB[:], ["vcb", "identB"], [("ps", 5)])
                    vcopy(kts[sl], pk5, [("ps", 5)], [("kts", sl)])
                if "st" not in KSKIP:
                    dma("sync", KTs[:, :, :, i * 128:(i + 1) * 128].rearrange("t h d s -> d (t h) s"), kts[sl],
                        [("kts", sl)], [("KTs", i)], f"kst{sl}")
                    dma("sync", VSc[:, i * 128:(i + 1) * 128, :, :].rearrange("t s h c -> s t (h c)"),
                        vst[sl].rearrange("p t h c -> p t (h c)"), [("vst", sl), ("vst1", sl)], [("VSc", i)], f"vst{sl}")
            P.barrier()
            if KSTOP == "A":
                break

            AR.reset()
            w1b = [AR.alloc([128, 32, 128], BF16) for _ in range(2)]
            w2b = [AR.alloc([128, 128], BF16) for _ in range(2)]
            pe_f = [AR.alloc([32, 128], F32) for _ in range(2)]
            peT = [AR.alloc([128, 32], BF16) for _ in range(2)]
            c0 = [AR.alloc([128, 1], F32) for _ in range(2)]
            raw = [AR.alloc([128, S], BF16) for _ in range(2)]
            h1T = AR.alloc([128, 512], BF16)
            for ti, (w1d, w2d, ped) in enumerate([(w1_k, w2_k, pe_k), (w1_v, w2_v, pe_v)]):
                dma("gpsimd", w1b[ti], w1d.rearrange("(l d) e -> d l e", d=128), (), [("w1b", ti)], f"w1b{ti}")
                dma("gpsimd", w2b[ti], w2d, (), [("w2b", ti)], f"w2b{ti}")
                dma("sync", pe_f[ti], ped, (), [("pe_f", ti)], f"pef{ti}")
                tr(ps[:, 0, 0:32], pe_f[ti], identF[0:32, 0:32], [("pe_f", ti), "identF"], [("ps", 0)])
                vcopy(peT[ti], ps[:, 0, 0:32], [("ps", 0)], [("peT", ti)])
                for l in range(32):
                    mm(ps[:, 1, 0:1], w1b[ti][:, l, :], peT[ti][:, l:l + 1], l == 0, l == 31,
                       [("w1b", ti), ("peT", ti)], [("ps", 1)])
                vcopy(c0[ti], ps[:, 1, 0:1], [("ps", 1)], [("c0", ti)])
            vmemset(vcc[:, :, :, 128:130], 1.0, ["vcc1"])
            it = 0
            for ti in range(2):
                for hk in range(2):
                    sl = it % 2
                    it += 1
                    dma("sync", raw[sl], KTs[0 if ti == 0 else 3, hk, :, :], (), [("raw", sl)], f"raw{sl}")
                    r16 = raw[sl].rearrange("p (n s) -> p n s", s=16)
                    pb = 2 + sl
                    for l in range(32):
                        rhs = r16[:, 0:511, l] if l < 16 else r16[:, 1:512, l - 16]
                        mm(ps[:, pb, 0:511], w1b[ti][:, l, :], rhs, l == 0, l == 31, [("w1b", ti), ("raw", sl)], [("ps", pb)])
                    vmemset(h1T[:, 511:512], 0.0, ["h1T"])
                    act(h1T[:, 0:511], ps[:, pb, 0:511], AF.Silu, [("ps", pb), ("c0", ti)], ["h1T"], bias=c0[ti])
                    if ti == 0:
                        mm(bank(4), w2b[0], h1T, True, True, [("w2b", 0), "h1T"], [("ps", 4)])
                        vcopy(kcT[:, hk, :], bank(4), [("ps", 4)], ["kcT"])
                    else:
                        for nb in range(4):
                            mm(ps[:, 5, nb * 128:(nb + 1) * 128], h1T[:, nb * 128:(nb + 1) * 128], w2b[1], True, True,
                               [("w2b", 1), "h1T"], [("ps", 5)])
                        vcopy(vcc[:, :, hk, 0:128], ps[:, 5, :].rearrange("p (n x) -> p n x", n=4), [("ps", 5)], ["vcc"])
            if DEBUG:
                dma("sync", dbg_kc, kcT[:].rearrange("p a b -> p (a b)"), ["kcT"], ["dbg_kc"], "dbg1")
                dma("sync", dbg_vc, vcc[:].rearrange("p a b c -> p (a b c)"), ["vcc", "vcc1"], ["dbg_vc"], "dbg2")
            P.barrier()
            if KSTOP == "A2":
                break

            AR.reset()
            gmix = AR.alloc([128, 2048], F32)
            gconv = AR.alloc([128, 1024], F32)
            wcv = AR.alloc([128, 3, 1024], F32)
            shf = AR.alloc([128, 2, 128], F32)
            hfix = AR.alloc([32, 2, 128], F32)
            hmask = AR.alloc([32, 16], F32)
            uh = [AR.alloc([32, 256], F32) for _ in range(2)]
            xs = [AR.alloc([128, 2048], F32) for _ in range(2)]
            xb = AR.alloc([128, 2048], BF16)
            aTo = AR.alloc([128, 16, 1024], BF16)
            wch = [AR.alloc([128, 16, 256], BF16) for _ in range(6)]
            wgt = AR.alloc([128, 16, 24], BF16)
            ycb = AR.alloc([128, 8, 1024], BF16)
            cso = AR.alloc([128, 8, 2, 64], F32)
            ccs = [AR.alloc([128, 256], F32) for _ in range(2)]
            uu = [AR.alloc([128, 256], F32) for _ in range(2)]
            aa = [AR.alloc([128, 256], F32) for _ in range(2)]
            tb_ = [AR.alloc([128, 256], F32) for _ in range(2)]
            qr1 = AR.alloc([128, 2, 64], F32)
            qr2 = AR.alloc([128, 2, 64], F32)
            qrope = [AR.alloc([128, 2, 2, 64], BF16) for _ in range(2)]
            qts = [AR.alloc([128, 2, 128], BF16) for _ in range(2)]
            ycn = AR.alloc([128, 1024], BF16)
            ymT = [AR.alloc([128, 8, 128], BF16) for _ in range(2)]
            ssq = AR.alloc([128, 8, 4], F32)

            bc_load(gmix, g_mix, "gmix", "c_gmix")
            bc_load(gconv, g_conv, "gconv", "c_gconv")
            for kk in range(3):
                bc_load(wcv[:, kk, :], conv_w[kk, :], ("wcv", kk), f"c_wcv{kk}")
            dma("sync", shf, Sh_d.rearrange("p (a b) -> p a b", a=2), (), ["shf"], "c_sh")
            dma("sync", hfix, Hh_d[:, 0:256].rearrange("p (a b) -> p a b", a=2), (), ["hfix"], "c_hh")
            dma("sync", hmask, Hh_d[:, 256:272], (), ["hmask"], "c_hm")
            wcnt = [0]

            def next_w(src_cols, ncols=256):
                i = wcnt[0] % 6
                wcnt[0] += 1
                load_w(wch[i][:, :, 0:ncols], src_cols, ("wch", i), f"wch{i}")
                return wch[i], ("wch", i)

            for hf in range(2):
                if hf == 0:
                    dma("sync", xs[1][0:32, :], xhalo, (), [("xs", 1)], "xs1")
                    norm_transpose(xs[1][0:32, :], ("xs", 1), gmix, "gmix", xb, "xb", 0, aT_halo[:], "aT_halo", 0, np_=32)
                for mi in range(8):
                    m = hf * 8 + mi
                    sl = mi % 2
                    dma("sync", xs[sl], xown[m * 128:(m + 1) * 128, :], (), [("xs", sl)], f"xs{sl}")
                    norm_transpose(xs[sl], ("xs", sl), gmix, "gmix", xb, "xb", 0 if sl == 0 else 6,
                                   aTo[:, :, mi * 128:(mi + 1) * 128], ("aTo", mi), 0)
                dma("sync", cso, cs_own[hf * 1024:(hf + 1) * 1024, :].rearrange("(m p) (a b) -> p m a b", p=128, a=2),
                    (), ["cso"], "cso")
                for cg in range(4):
                    c0_, c1_ = cg * 256, (cg + 1) * 256
                    Wb, kWb = next_w(w_in[:, c0_:c1_])
                    Wc, kWc = next_w(w_in[:, 1024 + c0_:1024 + c1_])
                    Wh, kWh = next_w(w_in[:, 2048 + c0_:2048 + c1_])
                    if hf == 0:
                        for k in range(16):
                            mm(ps[0:32, 2, 0:256], aT_halo[:, k, :], Wc[:, k, :], k == 0, k == 15, ["aT_halo", kWc], [("ps", 2)])
                        for k in range(16):
                            mm(ps[0:32, 2, 256:512], aT_halo[:, k, :], Wh[:, k, :], k == 0, k == 15, ["aT_halo", kWh], [("ps", 2)])
                        act(ccs[0][0:32, :], ps[0:32, 2, 0:256], AF.Copy, [("ps", 2)], [("ccs", 0)])
                        vtt(u_halo[:, c0_:c1_], ps[0:32, 2, 256:512], ccs[0][0:32, :], ALU.mult, [("ps", 2), ("ccs", 0)], ["u_halo"])
                    for mi in range(8):
                        m = hf * 8 + mi
                        sl = mi % 2
                        bA, bB, bC = (2, 3, 4) if sl == 0 else (5, 6, 7)
                        at = aTo[:, :, mi * 128:(mi + 1) * 128]
                        for k in range(16):
                            mm(ps[:, bA, 0:256], at[:, k, :], Wc[:, k, :], k == 0, k == 15, [("aTo", mi), kWc], [("ps", bA)])
                        for k in range(16):
                            mm(ps[:, bA, 256:512], at[:, k, :], Wh[:, k, :], k == 0, k == 15, [("aTo", mi), kWh], [("ps", bA)])
                        for k in range(16):
                            mm(ps[:, bB, 0:256], at[:, k, :], Wb[:, k, :], k == 0, k == 15, [("aTo", mi), kWb], [("ps", bB)])
                        act(ccs[sl], ps[:, bA, 0:256], AF.Copy, [("ps", bA)], [("ccs", sl)])
                        vtt(uu[sl], ps[:, bA, 256:512], ccs[sl], ALU.mult, [("ps", bA), ("ccs", sl)], [("uu", sl)])
                        vts(uh[sl], u_halo[:, c0_:c1_], hmask[:, m:m + 1], None, ALU.mult, None, ["u_halo", "hmask"], [("uh", sl)])
                        mm(ps[:, bC, 0:256], shf[:, 0, :], uu[sl], True, False, ["shf", ("uu", sl)], [("ps", bC)])
                        mm(ps[:, bC, 0:256], hfix[:, 0, :], uh[sl], False, True, ["hfix", ("uh", sl)], [("ps", bC)])
                        mm(ps[:, bC, 256:512], shf[:, 1, :], uu[sl], True, False, ["shf", ("uu", sl)], [("ps", bC)])
                        mm(ps[:, bC, 256:512], hfix[:, 1, :], uh[sl], False, True, ["hfix", ("uh", sl)], [("ps", bC)])
                        vtt(aa[sl], uu[sl], wcv[:, 2, c0_:c1_], ALU.mult, [("uu", sl), ("wcv", 2)], [("aa", sl)])
                        vtt(tb_[sl], ps[:, bC, 0:256], wcv[:, 1, c0_:c1_], ALU.mult, [("ps", bC), ("wcv", 1)], [("tb", sl)])
                        vtt(aa[sl], aa[sl], tb_[sl], ALU.add, [("aa", sl), ("tb", sl)], [("aa", sl)])
                        vtt(tb_[sl], ps[:, bC, 256:512], wcv[:, 0, c0_:c1_], ALU.mult, [("ps", bC), ("wcv", 0)], [("tb", sl)])
                        vtt(aa[sl], aa[sl], tb_[sl], ALU.add, [("aa", sl), ("tb", sl)], [("aa", sl)])
                        vtt(ycb[:, mi, c0_:c1_], ps[:, bB, 0:256], aa[sl], ALU.mult, [("ps", bB), ("aa", sl)], [("ycb", mi, cg)])
                        act(junk[:, 0:256], ycb[:, mi, c0_:c1_], AF.Square, [("ycb", mi, cg)], ["junk", ("ssq", mi, cg)],
                            accum_out=ssq[:, mi, cg:cg + 1])
                for qc in range(4):
                    Wq, kWq = next_w(w_in[:, 3072 + qc * 256:3072 + (qc + 1) * 256])
                    for mi in range(8):
                        m = hf * 8 + mi
                        sl = mi % 2
                        bq = 2 if sl == 0 else 5
                        at = aTo[:, :, mi * 128:(mi + 1) * 128]
                        for k in range(16):
                            mm(ps[:, bq, 0:256], at[:, k, :], Wq[:, k, :], k == 0, k == 15, [("aTo", mi), kWq], [("ps", bq)])
                        z = ps[:, bq, 0:256].rearrange("p (h f x) -> p h f x", h=2, f=2)
                        cosb = cso[:, mi, 0, :].unsqueeze(1).to_broadcast([128, 2, 64])
                        sinb = cso[:, mi, 1, :].unsqueeze(1).to_broadcast([128, 2, 64])
                        kz = [("ps", bq), "cso"]
                        vtt(qr1, z[:, :, 0, :], cosb, ALU.mult, kz, ["qr1"])
                        vtt(qr2, z[:, :, 1, :], sinb, ALU.mult, kz, ["qr2"])
                        vtt(qrope[sl][:, :, 0, :], qr1, qr2, ALU.subtract, ["qr1", "qr2"], [("qrope0", sl)])
                        vtt(qr1, z[:, :, 1, :], cosb, ALU.mult, kz, ["qr1"])
                        vtt(qr2, z[:, :, 0, :], sinb, ALU.mult, kz, ["qr2"])
                        vtt(qrope[sl][:, :, 1, :], qr1, qr2, ALU.add, ["qr1", "qr2"], [("qrope1", sl)])
                        bt = 3 if sl == 0 else 6
                        ptq = bank_bf(bt)[:, 0, 0:256].rearrange("p (h t) -> p h t", h=2)
                        for hh in range(2):
                            tr(ptq[:, hh, :], qrope[sl][:, hh, :, :].rearrange("p f x -> p (f x)"), identB[:],
                               [("qrope0", sl), ("qrope1", sl), "identB"], [("ps", bt)])
                        act(qts[sl], ptq, AF.Copy, [("ps", bt)], [("qts", sl)])
                        dma("sync", qTs[m, :, qc * 2:qc * 2 + 2, :], qts[sl], [("qts", sl)], [("qTs", m, qc)], f"qts{sl}")
                load_w(wgt, w_in[:, 5632:5656], "wgt", "wgt")
                for mi in range(8):
                    m = hf * 8 + mi
                    bq = 4 if mi % 2 == 0 else 7
                    at = aTo[:, :, mi * 128:(mi + 1) * 128]
                    for k in range(16):
                        mm(ps[:, bq, 0:24], at[:, k, :], wgt[:, k, :], k == 0, k == 15, [("aTo", mi), "wgt"], [("ps", bq)])
                    act(gates[:, m, :], ps[:, bq, 0:24], AF.Sigmoid, [("ps", bq)], [("gates", m)])
                for mi in range(8):
                    m = hf * 8 + mi
                    sl = mi % 2
                    P.op("vector", lambda e, mi=mi: e.tensor_reduce(out=stat[:, 8:9], in_=ssq[:, mi, :], axis=AX.X, op=ALU.add),
                         [("ssq", mi, c) for c in range(4)], [("stat", 8)])
                    act(stat[:, 9:10], stat[:, 8:9], AF.Sqrt, [("stat", 8)], [("stat", 9)], scale=1.0 / 1024, bias=eps_t[:])
                    vrecip(stat[:, 10:11], stat[:, 9:10], [("stat", 9)], [("stat", 10)])
                    vstt(ycn, ycb[:, mi, :], stat[:, 10:11], gconv, ALU.mult, ALU.mult,
                         [("ycb", mi, c) for c in range(4)] + [("stat", 10), "gconv"], ["ycn"])
                    bt = 3 if sl == 0 else 6
                    pty = bank_bf(bt).rearrange("p a (c t) -> p (a c) t", t=128)
                    for c in range(8):
                        tr(pty[:, c, :], ycn[:, c * 128:(c + 1) * 128], identB[:], ["ycn", "identB"], [("ps", bt)])
                    act(ymT[sl], pty, AF.Copy, [("ps", bt)], [("ymT", sl)])
                    dma("sync", ymixT[m, :, 0:8, :], ymT[sl], [("ymT", sl)], [("ymixT", m, 0)], f"ymT{sl}")
            if DEBUG:
                dma("sync", dbg_gates, gates[:].rearrange("p a b -> p (a b)"), [("gates", m) for m in range(16)], ["dbg_g"], "dbg3")
            P.barrier()
            if KSTOP == "0":
                break

            AR.reset()
            ksT = AR.alloc([128, 2, S], BF16)
            vsA = AR.alloc([128, 64, 2, 130], BF16)
            Eall = AR.alloc([128, 64, 128], BF16)
            cbs = AR.alloc([128, 4, 4, 128], BF16)
            cbw = AR.alloc([128, 8, 4, 128], BF16)
            cbs_f = AR.alloc([128, 8, 128], F32)
            gattn = AR.alloc([128, 1024], F32)
            sfz = AR.alloc([128, 9], F32)
            suz = AR.alloc([128, 9], F32)
            qT = [AR.alloc([128, 8, 128], BF16) for _ in range(2)]
            kwT = [AR.alloc([128, 2, 1024], BF16) for _ in range(2)]
            vwA = [AR.alloc([128, 8, 2, 130], BF16) for _ in range(2)]
            cbc_f = [AR.alloc([128, 2, 128], F32) for _ in range(2)]
            cbc = [AR.alloc([128, 2, 4, 128], BF16) for _ in range(2)]
            cbT = [AR.alloc([128, 64], F32) for _ in range(2)]
            ee = [AR.alloc([128, 512], F32) for _ in range(2)]
            psum_ = AR.alloc([128, 512], F32)
            imp = AR.alloc([128, 128], F32)
            imp2 = AR.alloc([128, 128], F32)
            m8 = AR.alloc([128, 16], F32)
            selb = AR.alloc([128, 128], F32)
            sbT = [AR.alloc([128, 4, 128], BF16) for _ in range(2)]
            pT = [AR.alloc([128, 512], BF16) for _ in range(3)]
            yat = AR.alloc([128, 8, 128], F32)
            ytmp = AR.alloc([128, 8, 128], F32)
            coef = AR.alloc([128, 8], F32)
            dden = AR.alloc([128, 8], F32)
            yan = AR.alloc([128, 1024], BF16)
            ymT2 = [AR.alloc([128, 8, 128], BF16) for _ in range(2)]
            Dh = AR.alloc([128, 8], F32)

            for hk in range(2):
                dma("sync", ksT[:, hk, :], KTs[1, hk, :, :], (), [("ksT", hk)], f"ksT{hk}")
            for qd in range(4):
                dma("sync", vsA[:, qd * 16:(qd + 1) * 16, :, :].rearrange("p b h c -> p b (h c)"),
                    VSc[0, qd * 2048:(qd + 1) * 2048, :, :].rearrange("(b p) h c -> p b (h c)", p=128), (), [("vsA", qd)], f"vsA{qd}")
            for qd in range(8):
                dma("gpsimd", Eall[:, qd * 8:(qd + 1) * 8, :], E_all_d[:, qd * 1024:(qd + 1) * 1024].rearrange("p (a b) -> p a b", a=8),
                    (), [("Eall", qd // 2)], f"Eall{qd}")
            dma("sync", cbs_f[:, 0:4, :], cb_sel_d.rearrange("p (a b) -> p a b", a=4), (), ["cbs_f"], "c_cbs")
            vcopy(cbs, cbs_f[:, 0:4, :].unsqueeze(2).to_broadcast([128, 4, 4, 128]), ["cbs_f"], ["cbs"])
            dma("sync", cbs_f, cb_win_d.rearrange("p (a b) -> p a b", a=8), ["cbs_f"], ["cbs_f"], "c_cbs")
            vcopy(cbw, cbs_f.unsqueeze(2).to_broadcast([128, 8, 4, 128]), ["cbs_f"], ["cbw"])
            bc_load(gattn, g_attn, "gattn", "c_gattn")
            dma("sync", sfz, self_force_d, (), ["sfz"], "c_sfz")
            dma("sync", suz, self_future_d, (), ["suz"], "c_suz")
            vmemset(imp, -1.0, ["imp"])

            sctr = [0]
            pctr = [0]
            S_BANKS = [0, 1]
            O_BANK0 = 2

            def o_view(h):
                return ps[:, O_BANK0 + h // 2, (h % 2) * 256:(h % 2) * 256 + 129]

            def attend(m, hk, visits, o_started):
                qsl = m % 2
                qrhs = qT[qsl][:, 4 * hk:4 * hk + 4, :].rearrange("p g q -> p (g q)")
                def scores(vi):
                    kT_ap, kkeys, v_ap, vkeys, biases = visits[vi]
                    sb_ = S_BANKS[sctr[0] % 2]
                    sctr[0] += 1
                    nb = len(biases)
                    mm(bank(sb_), kT_ap, qrhs, True, nb == 0, kkeys + [("qT", qsl)], [("ps", sb_)])
                    for bi, (bl, br, bk) in enumerate(biases):
                        mm(bank(sb_), bl, br, False, bi == nb - 1, bk, [("ps", sb_)])
                    return sb_

                cur = scores(0)
                for vi in range(len(visits)):
                    kT_ap, kkeys, v_ap, vkeys, biases = visits[vi]
                    last = vi == len(visits) - 1
                    nxt = scores(vi + 1) if not last else None
                    sb_ = cur
                    pi = pctr[0] % 3
                    pctr[0] += 1
                    act(pT[pi], bank(sb_), AF.Exp, [("ps", sb_)], [("pT", pi)], scale=SCALE)
                    for g_ in range(4):
                        h = 4 * hk + g_
                        bnk = O_BANK0 + h // 2
                        st = bnk not in o_started
                        o_started.add(bnk)
                        mm(o_view(h), pT[pi][:, g_ * 128:(g_ + 1) * 128], v_ap, st, last, [("pT", pi)] + vkeys,
                           [("ps", bnk)], skip=True)
                    cur = nxt

            def finalize(m, br, first):
                ov = ps[:, O_BANK0:O_BANK0 + 4, :].rearrange("p b (h x) -> p (b h) x", h=2)
                okeys = [("ps", O_BANK0 + b_) for b_ in range(4)]
                vts(dden, ov[:, :, 128], 1e-30, None, ALU.max, None, okeys, ["dden"])
                vrecip(dden, dden, ["dden"], ["dden"])
                gv = gates[:, m, :].rearrange("p (h b) -> p h b", b=3)[:, :, br]
                vtt(coef, dden, gv, ALU.mult, ["dden", ("gates", m)], ["coef"])
                cb_ = coef[:, :].unsqueeze(2).to_broadcast([128, 8, 128])
                if first:
                    vtt(yat, ov[:, :, 0:128], cb_, ALU.mult, okeys + ["coef"], ["yat"])
                else:
                    vtt(ytmp, ov[:, :, 0:128], cb_, ALU.mult, okeys + ["coef"], ["ytmp"])
                    vtt(yat, yat, ytmp, ALU.add, ["yat", "ytmp"], ["yat"])

            def issue_loads(m):
                qsl = m % 2
                dma("sync", qT[qsl], qTs[m], (), [("qT", qsl)], f"qT{qsl}")
                t0 = 512 * (m - 1)
                for hk in range(2):
                    if m == 0:
                        dma("sync", kwT[qsl][:, hk, 512:1024], KTs[2, hk, :, 0:512], (), [("kwT", qsl)], f"kwT{qsl}")
                    else:
                        dma("sync", kwT[qsl][:, hk, :], KTs[2, hk, :, t0:t0 + 1024], (), [("kwT", qsl)], f"kwT{qsl}")
                if m == 0:
                    dma("sync", vwA[qsl][:, 4:8, :, :].rearrange("p b h c -> p b (h c)"),
                        VSc[1, 0:512, :, :].rearrange("(b p) h c -> p b (h c)", p=128), (), [("vwA", qsl)], f"vwA{qsl}")
                else:
                    dma("sync", vwA[qsl].rearrange("p b h c -> p b (h c)"),
                        VSc[1, t0:t0 + 1024, :, :].rearrange("(b p) h c -> p b (h c)", p=128), (), [("vwA", qsl)], f"vwA{qsl}")
                dma("sync", cbc_f[qsl], cb_cmp_d[m].rearrange("p (a b) -> p a b", a=2), (), [("cbc_f", qsl)], f"cbcf{qsl}")
                dma("sync", cbT[qsl], cbT_cmp_d[m], (), [("cbT", qsl)], f"cbT{qsl}")

            issue_loads(0)
            for m in range(16):
                qsl = m % 2
                nwin0 = 4 if m == 0 else 0
                if m + 1 < 16:
                    issue_loads(m + 1)
                vcopy(cbc[qsl], cbc_f[qsl].unsqueeze(2).to_broadcast([128, 2, 4, 128]), [("cbc_f", qsl)], [("cbc", qsl)])
                Nm = 32 * (m + 1)
                ns = 8 * (m + 1)
                NBc = m // 4 + 1
                zlo = max(0, 32 * m - 32)
                zc0 = zlo - (32 * m - 32)
                for hk in range(2):
                    for g_ in range(4):
                        h = 4 * hk + g_
                        tbk = 6 + (h % 2)
                        esl = h % 2
                        mm(ps[:, tbk, 0:Nm], qT[qsl][:, h, :], kcT[:, hk, 0:Nm], True, True, [("qT", qsl), "kcT"], [("ps", tbk)])
                        vtt(ps[:, tbk, zlo:Nm], ps[:, tbk, zlo:Nm], cbT[qsl][:, zc0:64], ALU.add,
                            [("ps", tbk), ("cbT", qsl)], [("ps", tbk)])
                        act(ee[esl][:, 0:Nm], ps[:, tbk, 0:Nm], AF.Exp, [("ps", tbk)], [("ee", esl), ("Dh", h)],
                            scale=SCALE, accum_out=Dh[:, h:h + 1])
                        vts(Dh[:, h:h + 1], Dh[:, h:h + 1], 1e-30, None, ALU.max, None, [("Dh", h)], [("Dh", h)])
                        vrecip(Dh[:, h:h + 1], Dh[:, h:h + 1], [("Dh", h)], [("Dh", h)])
                        if g_ == 0:
                            vts(psum_[:, 0:Nm], ee[esl][:, 0:Nm], Dh[:, h:h + 1], None, ALU.mult, None,
                                [("ee", esl), ("Dh", h)], ["psum"])
                        else:
                            vstt(psum_[:, 0:Nm], ee[esl][:, 0:Nm], Dh[:, h:h + 1], psum_[:, 0:Nm], ALU.mult, ALU.add,
                                 [("ee", esl), ("Dh", h), "psum"], ["psum"])
                    P.op("vector", lambda e, Nm=Nm, ns=ns: e.tensor_reduce(
                        out=imp[:, 0:ns], in_=psum_[:, 0:Nm].rearrange("p (s f) -> p s f", f=4), axis=AX.X, op=ALU.add),
                        ["psum"], ["imp"])
                    vtt(imp[:, 1:ns], imp[:, 1:ns], psum_[:, 0:Nm].rearrange("p (s f) -> p s f", f=4)[:, 0:ns - 1, 3], ALU.add,
                        ["imp", "psum"], ["imp"])
                    vmemset(imp[:, 0:1], 10.0, ["imp"])
                    if m == 0:
                        zs, zt = imp[:, 0:8], slice(1, 9)
                    else:
                        zs, zt = imp[:, 8 * m - 1:8 * m + 8], slice(0, 9)
                    vtt(zs, zs, sfz[:, zt], ALU.max, ["imp", "sfz"], ["imp"])
                    vtt(zs, zs, suz[:, zt], ALU.min, ["imp", "suz"], ["imp"])
                    P.op("vector", lambda e: e.max(out=m8[:, 0:8], in_=imp[:, :]), ["imp"], ["m8a"])
                    P.op("vector", lambda e: e.match_replace(out=imp2[:, :], in_to_replace=m8[:, 0:8], in_values=imp[:, :],
                                                             imm_value=-2.0), ["imp", "m8a"], ["imp2"])
                    P.op("vector", lambda e: e.max(out=m8[:, 8:16], in_=imp2[:, :]), ["imp2"], ["m8b"])
                    vts(selb, imp, m8[:, 15:16], 1.0, ALU.is_ge, ALU.subtract, ["imp", "m8b"], ["selb"])
                    tr(ps[:, 6, 0:128], selb, identF[:], ["selb", "identF"], [("ps", 6)])
                    act(sbT[hk], ps[:, 6, 0:128].unsqueeze(1).to_broadcast([128, 4, 128]), AF.Copy, [("ps", 6)], [("sbT", hk)],
                        scale=-NEG)
                idB = identB[:]
                o_started = set()
                for hk in range(2):
                    visits = []
                    for nb in range(NBc):
                        biases = []
                        w_ = nb - (NBc - 2)
                        if w_ >= 0:
                            biases.append((idB, cbc[qsl][:, w_, :, :].rearrange("p g q -> p (g q)"), ["identB", ("cbc", qsl)]))
                        visits.append((kcT[:, hk, nb * 128:(nb + 1) * 128], ["kcT"], vcc[:, nb, hk, 0:129], ["vcc", "vcc1"], biases))
                    attend(m, hk, visits, o_started)
                finalize(m, 0, True)
                o_started = set()
                for hk in range(2):
                    visits = []
                    for jj in range(nwin0, 8):
                        biases = [(idB, cbw[:, jj, :, :].rearrange("p g q -> p (g q)"), ["identB", "cbw"])]
                        visits.append((kwT[qsl][:, hk, jj * 128:(jj + 1) * 128], [("kwT", qsl)], vwA[qsl][:, jj, hk, 0:129],
                                       [("vwA", qsl)], biases))
                    attend(m, hk, visits, o_started)
                finalize(m, 2, False)
                o_started = set()
                for hk in range(2):
                    visits = []
                    for kb in range(4 * m + 4):
                        biases = [(Eall[:, kb, :], sbT[hk].rearrange("p g q -> p (g q)"), [("Eall", kb // 16), ("sbT", hk)])]
                        if kb >= 4 * m:
                            biases.append((idB, cbs[:, kb - 4 * m, :, :].rearrange("p g q -> p (g q)"), ["identB", "cbs"]))
                        visits.append((ksT[:, hk, kb * 128:(kb + 1) * 128], [("ksT", hk)], vsA[:, kb, hk, 0:129],
                                       [("vsA", kb // 16)], biases))
                    attend(m, hk, visits, o_started)
                finalize(m, 1, False)
                yflat = yat.rearrange("p h x -> p (h x)")
                rs, krs = rms_rstd(yflat, 1024, 16, "yat")
                vstt(yan, yflat, rs, gattn, ALU.mult, ALU.mult, ["yat", krs, "gattn"], ["yan"])
                pty = bank_bf(7).rearrange("p a (c t) -> p (a c) t", t=128)
                for c in range(8):
                    tr(pty[:, c, :], yan[:, c * 128:(c + 1) * 128], identB[:], ["yan", "identB"], [("ps", 7)])
                act(ymT2[qsl], pty, AF.Copy, [("ps", 7)], [("ymT2", qsl)])
                dma("sync", ymixT[m, :, 8:16, :], ymT2[qsl], [("ymT2", qsl)], [("ymixT", m, 1)], f"ymT2{qsl}")
            P.barrier()
            if KSTOP == "B":
                break

            AR.reset()
            gbuf = AR.alloc([128, 2048], F32)
            ymx = [AR.alloc([128, 16, 128], BF16) for _ in range(4)]
            hh_ = AR.alloc([128, 4, 2048], F32)
            xb = AR.alloc([128, 2048], BF16)
            fT = AR.alloc([128, 16, 512], BF16)
            actT = AR.alloc([128, 44, 512], BF16)
            wst = [AR.alloc([128, 16, 256], BF16) for _ in range(3)]
            stg = [AR.alloc([128, 2816], F32) for _ in range(2)]
            wdn = [AR.alloc([128, 11, 256], BF16) for _ in range(2)]
            sg = [AR.alloc([128, 512], F32) for _ in range(2)]
            wc2 = [0]

            cst = [0]

            def load_cast(dst, src, kdst, a, n):
                i = cst[0] % 2
                cst[0] += 1
                st = stg[i][:, 0:a * n].rearrange("p (a n) -> p a n", a=a)
                dma("sync", st, src, (), [("stg", i)], f"stg{i}")
                if i == 0:
                    vcopy(dst, st, [("stg", i)], [kdst])
                else:
                    act(dst, st, AF.Copy, [("stg", i)], [kdst])

            def next_w2(src_cols):
                i = wc2[0] % 3
                wc2[0] += 1
                sv = src_cols.rearrange("(kc p) n -> p kc n", p=128)
                for hf_ in range(2):
                    load_cast(wst[i][:, hf_ * 8:(hf_ + 1) * 8, :], sv[:, hf_ * 8:(hf_ + 1) * 8, :], ("wst", i), 8, 256)
                return wst[i], ("wst", i)

            dctr = [0]
            octr = [0]
            for tt in range(4):
                for tb in range(4):
                    m = tt * 4 + tb
                    dma("sync", hh_[:, tb, :], xown[m * 128:(m + 1) * 128, :], (), [("hh", tb)], f"hh{tb}")
                    dma("sync", ymx[tb], ymixT[m], (), [("ymx", tb)], f"ymx{tb}")
                nxt_o = next_w2(w_out[:, 0:256])
                for oc in range(8):
                    Wo, kWo = nxt_o
                    if oc + 1 < 8:
                        nxt_o = next_w2(w_out[:, (oc + 1) * 256:(oc + 2) * 256])
                    for tb in range(4):
                        m = tt * 4 + tb
                        sl = tb
                        ob = octr[0] % 2
                        octr[0] += 1
                        for k in range(16):
                            mm(ps[:, ob, 0:256], ymx[sl][:, k, :], Wo[:, k, :], k == 0, k == 15, [("ymx", sl), kWo], [("ps", ob)])
                        vtt(hh_[:, tb, oc * 256:(oc + 1) * 256], ps[:, ob, 0:256], hh_[:, tb, oc * 256:(oc + 1) * 256], ALU.add,
                            [("ps", ob), ("hh", tb)], [("hh", tb)])
                bc_load(gbuf, g_ffn, "gbuf", "c_gbuf")
                for tb in range(4):
                    norm_transpose(hh_[:, tb, :], ("hh", tb), gbuf, "gbuf", xb, "xb", 2 if tb % 2 == 0 else 4,
                                   fT[:, :, tb * 128:(tb + 1) * 128], ("fT", tb), 20)
                fkeys = [("fT", tb) for tb in range(4)]
                chunks = []
                for hp in range(22):
                    chunks.append(w_gate[:, hp * 256:(hp + 1) * 256])
                    chunks.append(w_up[:, hp * 256:(hp + 1) * 256])
                nxt_w = next_w2(chunks[0])
                for n_ in range(44):
                    W_, kW_ = nxt_w
                    if n_ + 1 < 44:
                        nxt_w = next_w2(chunks[n_ + 1])
                    hp = n_ // 2
                    base = 0 if hp % 2 == 0 else 4
                    if n_ % 2 == 0:
                        for c2 in range(2):
                            bk_ = base + 2 * c2
                            for k in range(16):
                                mm(bank(bk_), W_[:, k, c2 * 128:(c2 + 1) * 128], fT[:, k, :], k == 0, k == 15,
                                   [kW_] + fkeys, [("ps", bk_)])
                    else:
                        for c2 in range(2):
                            bk_ = base + 2 * c2 + 1
                            for k in range(16):
                                mm(bank(bk_), W_[:, k, c2 * 128:(c2 + 1) * 128], fT[:, k, :], k == 0, k == 15,
                                   [kW_] + fkeys, [("ps", bk_)])
                        for c2 in range(2):
                            hc = hp * 2 + c2
                            gb = base + 2 * c2
                            act(sg[c2], bank(gb), AF.Silu, [("ps", gb)], [("sg", c2)])
                            vtt(actT[:, hc, :], bank(gb + 1), sg[c2], ALU.mult, [("ps", gb + 1), ("sg", c2)], [("actT", hc)])
                wdv = w_down.rearrange("(hc p) n -> p hc n", p=128)
                for cg in range(8):
                    for hq in range(4):
                        di = dctr[0] % 2
                        dctr[0] += 1
                        load_cast(wdn[di], wdv[:, hq * 11:(hq + 1) * 11, cg * 256:(cg + 1) * 256], ("wdn", di), 11, 256)
                        for tb in range(4):
                            for c in range(11):
                                hc = hq * 11 + c
                                mm(ps[:, 4 + tb, 0:256], actT[:, hc, tb * 128:(tb + 1) * 128], wdn[di][:, c, :], hc == 0, hc == 43,
                                   [("actT", hc), ("wdn", di)], [("ps", 4 + tb)])
                    for tb in range(4):
                        vtt(hh_[:, tb, cg * 256:(cg + 1) * 256], ps[:, 4 + tb, 0:256], hh_[:, tb, cg * 256:(cg + 1) * 256], ALU.add,
                            [("ps", 4 + tb), ("hh", tb)], [("hh", tb)])
                bc_load(gbuf, g_fin, "gbuf", "c_gbuf")
                for tb in range(4):
                    m = tt * 4 + tb
                    rs, krs = rms_rstd(hh_[:, tb, :], 2048, 24, ("hh", tb))
                    vstt(hh_[:, tb, :], hh_[:, tb, :], rs, gbuf, ALU.mult, ALU.mult, [("hh", tb), krs, "gbuf"], [("hh", tb)])
                    dma("sync", y[m * 128:(m + 1) * 128, :], hh_[:, tb, :], [("hh", tb)], [("y", m)], f"yout{tb}")
            o_ = P.op("sync", lambda e: e.nop(), [("y", m) for m in range(16)], ())
            o_.is_nop = True
            P.barrier()
        P.emit(nc, ctx)
    return nc


def _tables(j):
    f32 = np.float32
    k = np.arange(128)[:, None]
    q = np.arange(128)[None, :]
    cb_sel = np.zeros((128, 4, 128), f32)
    for jj in range(4):
        if jj < j:
            v = np.ones((128, 128), bool)
        elif jj == j:
            v = k <= q
        else:
            v = np.zeros((128, 128), bool)
        cb_sel[:, jj, :] = np.where(v, 0.0, NEG)
    cb_win = np.zeros((128, 8, 128), f32)
    for jj in range(8):
        delta = 128 * (j + 4 - jj) + q - k
        cb_win[:, jj, :] = np.where((delta >= 0) & (delta < 512), 0.0, NEG)
    cb_cmp = np.zeros((16, 128, 2, 128), f32)
    cbT_cmp = np.zeros((16, 128, 64), f32)
    for m in range(16):
        t = 128 * (4 * m + j) + np.arange(128)
        NBc = m // 4 + 1
        for w in range(2):
            nb = NBc - 2 + w
            n = 128 * nb + np.arange(128)
            valid = (16 * n[:, None] + 31 <= t[None, :]) & (n[:, None] >= 0)
            cb_cmp[m, :, w, :] = np.where(valid, 0.0, NEG)
        n = 32 * m - 32 + np.arange(64)
        valid = 16 * n[None, :] + 31 <= t[:, None]
        cbT_cmp[m] = np.where(valid, 0.0, NEG)
    qq = np.arange(128)[:, None]
    c = np.arange(9)[None, :]
    rel = c - 1 - 2 * j - (qq >= 64)
    sel_force = np.where((rel == 0) | (rel == -1), 10.0, -1e9).astype(f32)
    sel_future = np.where(rel > 0, -1.0, 1e9).astype(f32)
    return dict(cb_sel=cb_sel.reshape(128, 512), cb_win=cb_win.reshape(128, 1024),
                cb_cmp=cb_cmp.reshape(16, 128, 256), cbT_cmp=cbT_cmp, sel_force=sel_force, sel_future=sel_future)


def _shared_tables():
    f32 = np.float32
    half = 64
    inv = 1.0 / (10000.0 ** (np.arange(half, dtype=np.float32) / half))
    ang = np.arange(S, dtype=np.float32)[:, None] * inv[None, :]
    cs_all = np.concatenate([np.cos(ang), np.sin(ang)], axis=1).astype(f32)
    s = np.arange(128)[:, None, None]
    kb = np.arange(64)[None, :, None]
    kk = np.arange(128)[None, None, :]
    E_all = (s == 2 * kb + kk // 64).astype(f32).reshape(128, 64 * 128)
    kr = np.arange(128)[:, None]
    mc = np.arange(128)[None, :]
    Sh = np.stack([(kr == mc - 1), (kr == mc - 2)], axis=1).astype(f32).reshape(128, 256)
    Hh = np.zeros((32, 272), f32)
    r = np.arange(32)
    Hh[r % 2 == 1, 0] = 1.0
    Hh[r % 2 == 0, 128 + 0] = 1.0
    Hh[r % 2 == 1, 128 + 1] = 1.0
    for m in range(16):
        Hh[2 * m:2 * m + 2, 256 + m] = 1.0
    return dict(cs_all=cs_all, E_all=E_all, Sh=Sh, Hh=Hh, ident=np.eye(128, dtype=f32))


_NC_CACHE = {}


def kernel(**inputs):
    x = np.asarray(inputs["x"], dtype=np.float32)
    sh = _shared_tables()
    base = {
        "w_in": np.ascontiguousarray(inputs["w_in"][0]),
        "w_out": np.ascontiguousarray(inputs["w_out"][0]),
        "w_gate": np.ascontiguousarray(inputs["w_gate"][0]),
        "w_up": np.ascontiguousarray(inputs["w_up"][0]),
        "w_down": np.ascontiguousarray(inputs["w_down"][0]),
        "conv_w": np.ascontiguousarray(inputs["conv_w"][0]),
        "cmp_pe_k": np.ascontiguousarray(inputs["cmp_pe_k"][0]),
        "cmp_w1_k": np.ascontiguousarray(inputs["cmp_w1_k"][0]),
        "cmp_w2_k": np.ascontiguousarray(inputs["cmp_w2_k"][0]),
        "cmp_pe_v": np.ascontiguousarray(inputs["cmp_pe_v"][0]),
        "cmp_w1_v": np.ascontiguousarray(inputs["cmp_w1_v"][0]),
        "cmp_w2_v": np.ascontiguousarray(inputs["cmp_w2_v"][0]),
        "norm_mix": np.ascontiguousarray(inputs["norm_mix"][0]),
        "norm_conv_out": np.ascontiguousarray(inputs["norm_conv_out"][0]),
        "norm_attn_out": np.ascontiguousarray(inputs["norm_attn_out"][0]),
        "norm_ffn": np.ascontiguousarray(inputs["norm_ffn"][0]),
        "norm_final": np.ascontiguousarray(inputs["norm_final"]),
    }
    base = {k: np.asarray(v, dtype=np.float32) for k, v in base.items()}
    base.update(sh)
    in_maps = []
    for c in range(8):
        b, j = c // 4, c % 4
        blocks = [4 * m + j for m in range(16)]
        xb_ = x[b]
        xown = np.concatenate([xb_[128 * qb:128 * qb + 128] for qb in blocks], axis=0)
        xhalo = np.zeros((32, D), np.float32)
        for m, qb in enumerate(blocks):
            if qb > 0:
                xhalo[2 * m:2 * m + 2] = xb_[128 * qb - 2:128 * qb]
        cs_own = np.concatenate([sh["cs_all"][128 * qb:128 * qb + 128] for qb in blocks], axis=0)
        im = dict(base)
        im.update(_tables(j))
        im.update(xall=np.ascontiguousarray(xb_[:KS]), xown=np.ascontiguousarray(xown), xhalo=xhalo,
                  cs_own=np.ascontiguousarray(cs_own))
        im["cs_all"] = np.ascontiguousarray(im["cs_all"][:KS])
        if SHRINK_KEEP is not None:
            im = {k: (v if k in SHRINK_KEEP else np.zeros((1, 1), np.float32)) for k, v in im.items()}
        in_maps.append(im)
    if "nc" not in _NC_CACHE:
        _NC_CACHE["nc"] = build_nc()
    nc = _NC_CACHE["nc"]
    res = run_bass_kernel_spmd(nc, in_maps[:KCORES], core_ids=list(range(KCORES)))
    out = np.zeros((2, S, D), np.float32)
    for c in range(KCORES):
        b, j = c // 4, c % 4
        yv = res.results[c]["y"]
        for m in range(16):
            qb = 4 * m + j
            out[b, 128 * qb:128 * qb + 128] = yv[128 * m:128 * m + 128]
    if DEBUG:
        kernel.last_results = res.results
    return out
```
